# Optimizing a Trainium2 kernel written in Bass

```python
import math
import jax, jax.numpy as jnp
from jax import lax
import numpy as np

D_MODEL = 1024
BATCH = 4
SEQ = 8192
DEPTH = 1

CHUNK = 64
N_HEADS_A = 4
HEAD_DIM_A = 64
WIDTH_A = N_HEADS_A * 2 * HEAD_DIM_A
Q_BLOCK = 128
N_HEADS_B = 8
HEAD_DIM_B = 64
WIDTH_B = N_HEADS_B * HEAD_DIM_B
N_PREV_CHUNKS = 8
REL_CLIP = 128
IN_COLS = 3 * WIDTH_A + 3 * WIDTH_B + 2 * D_MODEL
N_EXPERTS = 32
TOP_K = 4
D_FF = 1024
SWIGLU_ALPHA = 1.702
SWIGLU_LIMIT = 7.0
EXPERT_BLOCK = 128
DEEPNORM_ALPHA = (2 * DEPTH) ** 0.25
DEEPNORM_BETA = (8 * DEPTH) ** -0.25
LN_EPS = 1e-5

kernel_name = "hybrid_diffattn_chunkband_moe_deepnorm"


def layer_norm(x, g, b):
    xf = x.astype(jnp.float32)
    mu = jnp.mean(xf, axis=-1, keepdims=True)
    var = jnp.mean(jnp.square(xf - mu), axis=-1, keepdims=True)
    y = (xf - mu) * lax.rsqrt(var + LN_EPS) * g.astype(jnp.float32) + b.astype(jnp.float32)
    return y.astype(x.dtype)


def rms_norm(x, w):
    xf = x.astype(jnp.float32)
    y = xf * lax.rsqrt(jnp.mean(jnp.square(xf), axis=-1, keepdims=True) + LN_EPS) * w.astype(jnp.float32)
    return y.astype(x.dtype)


def alibi_slopes(n):
    return jnp.asarray(np.array([2.0 ** (-8.0 * (i + 1) / n) for i in range(n)], dtype=np.float32))


def diff_attention(q, k, v, lam, lam_init, subln_w):
    B, S = q.shape[0], q.shape[1]
    scale = HEAD_DIM_A ** -0.5
    q = q.transpose(0, 2, 1, 3, 4) * scale
    k = k.transpose(0, 2, 1, 3, 4)
    v = v.transpose(0, 2, 1, 3)
    n_qb = S // Q_BLOCK
    qb = q.reshape(B, N_HEADS_A, n_qb, Q_BLOCK, 2, HEAD_DIM_A).transpose(2, 0, 1, 3, 4, 5)
    kpos = jnp.arange(S, dtype=jnp.int32)
    kchunk = kpos // CHUNK
    slopes = alibi_slopes(N_HEADS_A)

    def one_block(args):
        q_blk, i = args
        qpos = i * Q_BLOCK + jnp.arange(Q_BLOCK, dtype=jnp.int32)
        s = jnp.einsum('bhqcd,bhkcd->cbhqk', q_blk, k).astype(jnp.float32)
        dist = jnp.abs(qpos[:, None] - kpos[None, :]).astype(jnp.float32)
        bias = -slopes[:, None, None] * dist
        allowed = kchunk[None, :] <= (qpos // CHUNK)[:, None]
        p = jax.nn.softmax(jnp.where(allowed, s + bias, -jnp.inf), axis=-1)
        a = p[0] - lam * p[1]
        return jnp.einsum('bhqk,bhkd->bhqd', a.astype(v.dtype), v)

    o = lax.map(one_block, (qb, jnp.arange(n_qb, dtype=jnp.int32)))
    o = o.transpose(1, 0, 3, 2, 4).reshape(B, S, N_HEADS_A, 2 * HEAD_DIM_A)
    o = rms_norm(o, subln_w) * (1.0 - lam_init)
    return o.reshape(B, S, WIDTH_A)


def chunk_band_attention(q, k, v, rel_bias):
    B, S = q.shape[0], q.shape[1]
    n_chunks = S // CHUNK
    pad_len = N_PREV_CHUNKS * CHUNK
    band = (N_PREV_CHUNKS + 1) * CHUNK
    scale = HEAD_DIM_B ** -0.5
    q = q.transpose(0, 2, 1, 3) * scale
    kp = jnp.pad(k.transpose(0, 2, 1, 3), ((0, 0), (0, 0), (pad_len, 0), (0, 0)))
    vp = jnp.pad(v.transpose(0, 2, 1, 3), ((0, 0), (0, 0), (pad_len, 0), (0, 0)))
    qc = q.reshape(B, N_HEADS_B, n_chunks, CHUNK, HEAD_DIM_B).transpose(2, 0, 1, 3, 4)
    qi = jnp.arange(CHUNK, dtype=jnp.int32)
    kj = jnp.arange(band, dtype=jnp.int32)
    rel = (kj[None, :] - pad_len) - qi[:, None]
    bias = rel_bias.astype(jnp.float32)[:, jnp.clip(rel, -REL_CLIP, REL_CLIP) + REL_CLIP]

    def one_chunk(args):
        q_blk, c = args
        start = c * CHUNK
        kb = lax.dynamic_slice_in_dim(kp, start, band, axis=2)
        vb = lax.dynamic_slice_in_dim(vp, start, band, axis=2)
        s = jnp.einsum('bhqd,bhkd->bhqk', q_blk, kb).astype(jnp.float32) + bias
        valid = (start - pad_len + kj) >= 0
        p = jax.nn.softmax(jnp.where(valid, s, -jnp.inf), axis=-1)
        return jnp.einsum('bhqk,bhkd->bhqd', p.astype(vb.dtype), vb)

    o = lax.map(one_chunk, (qc, jnp.arange(n_chunks, dtype=jnp.int32)))
    return o.transpose(1, 0, 3, 2, 4).reshape(B, S, WIDTH_B)


def gated_mixer(h, w_in, b_gate, lam_q1, lam_k1, lam_q2, lam_k2, subln_w, rel_bias,
                w_branch_a, w_branch_b, w_out, lam_init):
    B, S, D = h.shape
    proj = h @ w_in
    o0 = 0
    qa = proj[..., o0:o0 + WIDTH_A].reshape(B, S, N_HEADS_A, 2, HEAD_DIM_A); o0 += WIDTH_A
    ka = proj[..., o0:o0 + WIDTH_A].reshape(B, S, N_HEADS_A, 2, HEAD_DIM_A); o0 += WIDTH_A
    va = proj[..., o0:o0 + WIDTH_A].reshape(B, S, N_HEADS_A, 2 * HEAD_DIM_A); o0 += WIDTH_A
    qb = proj[..., o0:o0 + WIDTH_B].reshape(B, S, N_HEADS_B, HEAD_DIM_B); o0 += WIDTH_B
    kb = proj[..., o0:o0 + WIDTH_B].reshape(B, S, N_HEADS_B, HEAD_DIM_B); o0 += WIDTH_B
    vb = proj[..., o0:o0 + WIDTH_B].reshape(B, S, N_HEADS_B, HEAD_DIM_B); o0 += WIDTH_B
    gates = jax.nn.sigmoid(proj[..., o0:] + b_gate).reshape(B, S, 2, D)

    lam = (jnp.exp(jnp.sum(lam_q1.astype(jnp.float32) * lam_k1.astype(jnp.float32)))
           - jnp.exp(jnp.sum(lam_q2.astype(jnp.float32) * lam_k2.astype(jnp.float32))) + lam_init)
    out_a = diff_attention(qa, ka, va, lam, lam_init, subln_w)
    out_b = chunk_band_attention(qb, kb, vb, rel_bias)
    merged = gates[:, :, 0] * (out_a @ w_branch_a) + gates[:, :, 1] * (out_b @ w_branch_b)
    return merged @ w_out


def moe_ffn(h, w_router, b_router, w_exp_in, b_exp_in, w_exp_out, b_exp_out):
    B, S, D = h.shape
    t = h.reshape(-1, D)
    n_tok = t.shape[0]
    logits = (t @ w_router + b_router).astype(jnp.float32)
    top_val, top_idx = lax.top_k(logits, TOP_K)
    gate_w = jax.nn.softmax(top_val, axis=-1).astype(h.dtype)

    n_slots = n_tok * TOP_K
    e_flat = top_idx.reshape(-1).astype(jnp.int32)
    tok_flat = jnp.arange(n_slots, dtype=jnp.int32) // TOP_K
    order = jnp.argsort(e_flat)
    e_sorted = e_flat[order]
    counts = jnp.bincount(e_flat, length=N_EXPERTS).astype(jnp.int32)
    padded = (counts + EXPERT_BLOCK - 1) // EXPERT_BLOCK * EXPERT_BLOCK
    start = jnp.cumsum(counts) - counts
    pend = jnp.cumsum(padded)
    pstart = pend - padded
    rank = jnp.arange(n_slots, dtype=jnp.int32) - start[e_sorted]
    dest_sorted = pstart[e_sorted] + rank
    buf_len = (-(-n_slots // EXPERT_BLOCK)) * EXPERT_BLOCK + N_EXPERTS * EXPERT_BLOCK
    n_blocks = buf_len // EXPERT_BLOCK
    buf_tok = jnp.full((buf_len,), n_tok, jnp.int32).at[dest_sorted].set(tok_flat[order])
    dest = jnp.zeros((n_slots,), jnp.int32).at[order].set(dest_sorted)
    block_start = jnp.arange(n_blocks, dtype=jnp.int32) * EXPERT_BLOCK
    block_exp = jnp.minimum(jnp.searchsorted(pend, block_start, side='right'), N_EXPERTS - 1).astype(jnp.int32)
    t_pad = jnp.concatenate([t, jnp.zeros((1, D), t.dtype)], axis=0)

    def expert_block(args):
        tok_idx, e = args
        xb = t_pad[tok_idx]
        hb = xb @ w_exp_in[e] + b_exp_in[e]
        g = jnp.minimum(hb[:, :D_FF], SWIGLU_LIMIT)
        u = jnp.clip(hb[:, D_FF:], -SWIGLU_LIMIT, SWIGLU_LIMIT)
        a = g * jax.nn.sigmoid(SWIGLU_ALPHA * g) * (u + 1.0)
        return a @ w_exp_out[e] + b_exp_out[e]

    y_buf = lax.map(expert_block, (buf_tok.reshape(n_blocks, EXPERT_BLOCK), block_exp)).reshape(buf_len, D)
    y_slot = y_buf[dest].reshape(n_tok, TOP_K, D)
    y = jnp.einsum('nk,nkd->nd', gate_w, y_slot)
    return y.reshape(B, S, D)


def setup_inputs(seed: int = 0) -> dict:
    key = jax.random.key(seed)
    ks = jax.random.split(key, 24)
    L, D, E, F = DEPTH, D_MODEL, N_EXPERTS, D_FF
    nrm = lambda k, shape: jax.random.normal(k, shape, jnp.float32)
    col_scale = np.ones((IN_COLS,), np.float32)
    col_scale[2 * WIDTH_A:3 * WIDTH_A] = DEEPNORM_BETA
    col_scale[3 * WIDTH_A + 2 * WIDTH_B:3 * WIDTH_A + 3 * WIDTH_B] = DEEPNORM_BETA
    return {
        "x": nrm(ks[0], (BATCH, SEQ, D)),
        "ln_in_g": 1.0 + 0.05 * nrm(ks[1], (D,)),
        "ln_in_b": 0.02 * nrm(ks[2], (D,)),
        "w_in": nrm(ks[3], (L, D, IN_COLS)) * (D ** -0.5) * jnp.asarray(col_scale),
        "b_gate": 0.02 * nrm(ks[4], (L, 2 * D)),
        "lambda_q1": 0.1 * nrm(ks[5], (L, HEAD_DIM_A)),
        "lambda_k1": 0.1 * nrm(ks[6], (L, HEAD_DIM_A)),
        "lambda_q2": 0.1 * nrm(ks[7], (L, HEAD_DIM_A)),
        "lambda_k2": 0.1 * nrm(ks[8], (L, HEAD_DIM_A)),
        "subln_w": 1.0 + 0.05 * nrm(ks[9], (L, 2 * HEAD_DIM_A)),
        "rel_bias": 0.1 * nrm(ks[10], (L, N_HEADS_B, 2 * REL_CLIP + 1)),
        "w_branch_a": nrm(ks[11], (L, WIDTH_A, D)) * (WIDTH_A ** -0.5) * DEEPNORM_BETA,
        "w_branch_b": nrm(ks[12], (L, WIDTH_B, D)) * (WIDTH_B ** -0.5) * DEEPNORM_BETA,
        "w_out": nrm(ks[13], (L, D, D)) * (D ** -0.5) * DEEPNORM_BETA,
        "ln1_g": 1.0 + 0.05 * nrm(ks[14], (L, D)),
        "ln1_b": 0.02 * nrm(ks[15], (L, D)),
        "w_router": nrm(ks[16], (L, D, E)) * (D ** -0.5),
        "b_router": 0.01 * nrm(ks[17], (L, E)),
        "w_exp_in": nrm(ks[18], (L, E, D, 2 * F)) * (D ** -0.5) * DEEPNORM_BETA,
        "b_exp_in": 0.01 * nrm(ks[19], (L, E, 2 * F)),
        "w_exp_out": nrm(ks[20], (L, E, F, D)) * (F ** -0.5) * DEEPNORM_BETA,
        "b_exp_out": 0.01 * nrm(ks[21], (L, E, D)),
        "ln2_g": 1.0 + 0.05 * nrm(ks[22], (L, D)),
        "ln2_b": 0.02 * nrm(ks[23], (L, D)),
    }


def reference(x, ln_in_g, ln_in_b, w_in, b_gate, lambda_q1, lambda_k1, lambda_q2, lambda_k2,
              subln_w, rel_bias, w_branch_a, w_branch_b, w_out, ln1_g, ln1_b,
              w_router, b_router, w_exp_in, b_exp_in, w_exp_out, b_exp_out, ln2_g, ln2_b):
    h = layer_norm(x, ln_in_g, ln_in_b)
    for l in range(DEPTH):
        lam_init = 0.8 - 0.6 * math.exp(-0.3 * l)
        m = gated_mixer(h, w_in[l], b_gate[l], lambda_q1[l], lambda_k1[l], lambda_q2[l], lambda_k2[l],
                        subln_w[l], rel_bias[l], w_branch_a[l], w_branch_b[l], w_out[l], lam_init)
        h = layer_norm(DEEPNORM_ALPHA * h + m, ln1_g[l], ln1_b[l])
        f = moe_ffn(h, w_router[l], b_router[l], w_exp_in[l], b_exp_in[l], w_exp_out[l], b_exp_out[l])
        h = layer_norm(DEEPNORM_ALPHA * h + f, ln2_g[l], ln2_b[l])
    return h
```

```python
import numpy as np
from contextlib import ExitStack
import concourse.bass as bass
import concourse.mybir as mybir
from concourse.bass_utils import run_bass_kernel_spmd

F32 = mybir.dt.float32
BF16 = mybir.dt.bfloat16
I32 = mybir.dt.int32
U32 = mybir.dt.uint32
ALU = mybir.AluOpType
AF = mybir.ActivationFunctionType
AX = mybir.AxisListType

D = 1024
NBLK = 64
NOWN = 32
NTOK = NOWN * 128
ALPHA = 2.0 ** 0.25
EPS = 1e-5
NEG = -30000.0
LAM_INIT = 0.8 - 0.6
SLOPES = [2.0 ** (-8.0 * (i + 1) / 4) for i in range(4)]
A_WINDOW = [3, 9, 33, 10 ** 6]
E_CAP = 704
NEXP = 32

SAME_ENG_SYNC = True
PA_DBG = {}


class Op:
    __slots__ = ("eng", "name", "args", "kwargs", "reads", "writes", "dma", "key", "deps", "token")


class Sched:
    def __init__(self, nc, stack):
        self.nc = nc
        self.eng = {"pe": nc.tensor, "act": nc.scalar, "dve": nc.vector, "pool": nc.gpsimd, "sp": nc.sync}
        self.esem = {e: stack.enter_context(nc.semaphore("es_" + e)) for e in self.eng}
        self.ecount = {e: 0 for e in self.eng}
        self.dsem = {}
        self.scount = {}
        self.sem_pool = []
        self.all_sems = []
        self.sem_kind = {}
        self.stack = stack
        self.waited = {e: {} for e in self.eng}
        self.ops = []
        self.n_emitted = 0
        self.group_keys = set()

    def add(self, eng, name, *args, reads=(), writes=(), dma=False, key=None, **kwargs):
        op = Op()
        op.eng, op.name, op.args, op.kwargs = eng, name, args, kwargs
        op.reads, op.writes, op.dma, op.key = tuple(reads), tuple(writes), dma, key
        op.token = None
        self.ops.append(op)
        return op

    def dma(self, eng, out, in_, reads, writes, key=None, group=False, **kw):
        if key is None:
            key = writes[0]
        key = key + ("@sw" if eng == "pool" else "@hw")
        if group:
            self.group_keys.add(key)
        return self.add(eng, "dma_start", reads=reads, writes=writes, dma=True, key=key, out=out, in_=in_, **kw)

    def _dsem(self, key):
        if key not in self.dsem:
            kind = key[-3:]
            cand = [x for x in self.sem_pool if self.sem_kind[x] == kind]
            if cand:
                sem = cand[-1]
                self.sem_pool.remove(sem)
            else:
                sem = self.stack.enter_context(self.nc.semaphore("ds_%d" % len(self.all_sems)))
                self.all_sems.append(sem)
                self.scount[sem] = 0
                self.sem_kind[sem] = key[-3:]
            self.dsem[key] = sem
        return self.dsem[key]

    def flush(self):
        ops = self.ops
        self.ops = []
        n = len(ops)
        last_w = {}
        readers = {}
        for i, op in enumerate(ops):
            deps = set()
            for r in op.reads:
                if r in last_w:
                    deps.add(last_w[r])
            for w in op.writes:
                if w in last_w:
                    deps.add(last_w[w])
                rd = readers.get(w)
                if rd:
                    deps.update(rd.values())
            deps.discard(i)
            kept = []
            for j in deps:
                pj = ops[j]
                if (not pj.dma) and (not op.dma) and pj.eng == op.eng:
                    if op.eng == "pe" or not SAME_ENG_SYNC:
                        continue
                kept.append(j)
            op.deps = sorted(kept)
            for r in op.reads:
                d = readers.setdefault(r, {})
                d[("d%d" % i) if op.dma else op.eng] = i
            for w in op.writes:
                last_w[w] = i
                readers[w] = {}
        needs = [False] * n
        for op in ops:
            for j in op.deps:
                needs[j] = True
        last_on = {}
        for i, op in enumerate(ops):
            if not op.dma:
                last_on[op.eng] = i
        for i in last_on.values():
            needs[i] = True
        for i, op in enumerate(ops):
            E = op.eng
            e = self.eng[E]
            wt = self.waited[E]
            for j in op.deps:
                sem, val = ops[j].token
                if ops[j].dma and ops[j].key in self.group_keys:
                    val = self.scount[sem]
                if wt.get(sem, 0) < val:
                    e.wait_ge(sem, val)
                    wt[sem] = val
            try:
                ins = getattr(e, op.name)(*op.args, **op.kwargs)
            except Exception:
                print("EMIT FAIL", E, op.name, op.writes, [str(a)[:120] for a in op.args], {k: str(v)[:120] for k, v in op.kwargs.items()})
                raise
            if op.dma:
                sem = self._dsem(op.key)
                self.scount[sem] += 16
                ins.then_inc(sem, 16)
                op.token = (sem, self.scount[sem])
            elif needs[i]:
                self.ecount[E] += 1
                ins.then_inc(self.esem[E], 1)
                op.token = (self.esem[E], self.ecount[E])
        self.n_emitted += n
        for E, e in self.eng.items():
            wt = self.waited[E]
            for F in self.eng:
                if F != E and self.ecount[F] > wt.get(self.esem[F], 0):
                    e.wait_ge(self.esem[F], self.ecount[F])
                    wt[self.esem[F]] = self.ecount[F]
            for sem in self.all_sems:
                if self.scount[sem] > wt.get(sem, 0):
                    e.wait_ge(sem, self.scount[sem])
                    wt[sem] = self.scount[sem]
        self.sem_pool.extend(self.dsem.values())
        self.dsem = {}


def _dram(nc, name, shape, dt, dbg):
    return nc.dram_tensor(name, list(shape), dt, kind=("ExternalOutput" if dbg else "Internal")).ap()


def build_program(stop_after=None, dbg=()):
    nc = bass.Bass("TRN2", target_bir_lowering=False)
    inp = {}

    def din(name, shape, dt=F32):
        inp[name] = nc.dram_tensor(name, list(shape), dt, kind="ExternalInput").ap()
        return inp[name]

    xall = din("xall", [NBLK * 128, D])
    lnin = din("lnin", [2, D])
    w_in = din("w_in", [D, 5120])
    ident_in = din("ident", [128, 128])
    din("abias", [128, 2 * 4 * 64])
    din("adiag", [128, 4 * 2 * 128])
    din("lamv", [4, 64])
    din("subln", [1, 128])
    din("btab", [128, 8 * 5 * 128])
    din("parmask", [128, 1])
    out = nc.dram_tensor("out", [NTOK, D], F32, kind="ExternalOutput").ap()
    din("b_gate", [128, 16])
    din("w_ba", [512, D])
    din("w_bb", [512, D])
    din("w_out", [D, D])
    din("ln1", [2, D])
    din("w_router", [D, NEXP])
    din("b_router", [1, NEXP])
    din("rconst", [128, 128 + 128 + 32 + 32])
    H1 = _dram(nc, "H1", [NTOK, D], F32, "H1" in dbg)
    YS = _dram(nc, "YS", [NEXP * E_CAP, D], F32, "YS" in dbg)
    din("w_e1", [NEXP, D, 2048])
    din("w_e2", [NEXP, D, D])
    din("b_e1", [128, NEXP * 16])
    din("b_e2", [NEXP, D])
    din("ln2", [2, D])
    XS = _dram(nc, "XS", [NEXP * E_CAP, D], BF16, "XS" in dbg)
    OA = _dram(nc, "OA", [NTOK, 512], BF16, "OA" in dbg)
    OB = _dram(nc, "OB", [NTOK, 512], BF16, "OB" in dbg)

    KAT = _dram(nc, "KAT", [4, 128, NBLK * 128], BF16, "KAT" in dbg)
    KBT = _dram(nc, "KBT", [4, 128, NBLK * 128], BF16, "KBT" in dbg)
    QAT = _dram(nc, "QAT", [4, 128, NTOK], BF16, "QAT" in dbg)
    QBT = _dram(nc, "QBT", [4, 128, NTOK], BF16, "QBT" in dbg)
    VA = _dram(nc, "VA", [NBLK, 128, 4 * 129], BF16, "VA" in dbg)
    VB = _dram(nc, "VB", [NBLK, 128, 8 * 65], BF16, "VB" in dbg)

    with ExitStack() as stack:
        S = Sched(nc, stack)
        psum = [stack.enter_context(nc.psum_tensor("ps%d" % i, [128, 512], F32)) for i in range(8)]
        ident = stack.enter_context(nc.sbuf_tensor("identb", [128, 128], BF16))
        S.dma("pool", ident[:], ident_in[:, :], reads=[], writes=["ident"], key="init", group=True)

        if "skip0" not in dbg:
            phase0(nc, S, psum, ident, xall, lnin, w_in, KAT, KBT, QAT, QBT, VA, VB)
        if stop_after == 0:
            return nc, S
        if "skipA" not in dbg:
            phaseA(nc, S, psum, inp, KAT, QAT, VA, OA)
        if stop_after == 1:
            return nc, S
        if "skipB" not in dbg:
            phaseB(nc, S, psum, inp, KBT, QBT, VB, OB)
        if stop_after == 2:
            return nc, S
        pers = {
            "dest": stack.enter_context(nc.sbuf_tensor("dest_i", [128, NOWN, 4], I32)),
            "gate": stack.enter_context(nc.sbuf_tensor("gates4", [128, NOWN, 4], F32)),
        }
        if "DESTD" in dbg:
            pers["DESTD"] = _dram(nc, "DESTD", [128, NOWN * 4], I32, True)
            pers["GATED"] = _dram(nc, "GATED", [128, NOWN * 4], F32, True)
        if "skipM" not in dbg:
            phaseM(nc, S, psum, ident, inp, xall, lnin, w_in, OA, OB, H1, XS, pers)
        if stop_after == 3:
            return nc, S
        if "skipX" not in dbg:
            phaseX(nc, S, psum, ident, inp, XS, YS)
        if stop_after == 4:
            return nc, S
        phaseF(nc, S, psum, inp, H1, YS, out, pers)
    return nc, S


def phase0(nc, S, psum, ident, xall, lnin, w_in, KAT, KBT, QAT, QBT, VA, VB):
    with ExitStack() as st:
        sb = lambda name, shape, dt: st.enter_context(nc.sbuf_tensor(name, list(shape), dt))
        gb = sb("p0_gb", [128, 2, D], F32)
        wkv = sb("p0_w", [128, 8, 3072], BF16)
        xt = [sb("p0_x%d" % i, [128, D], F32) for i in range(2)]
        xn = [sb("p0_xn%d" % i, [128, D], F32) for i in range(2)]
        hb = [sb("p0_hb%d" % i, [128, D], BF16) for i in range(2)]
        hT = [sb("p0_hT%d" % i, [128, 8, 512], BF16) for i in range(2)]
        st6 = [sb("p0_st%d" % i, [128, 2, 6], F32) for i in range(2)]
        mv = [sb("p0_mv%d" % i, [128, 8], F32) for i in range(2)]
        for i in range(2):
            ln_consts(S, mv[i], "p0s%d" % i)
        kq = [sb("p0_kq%d" % i, [128, 16, 512], BF16) for i in range(2)]
        vas = [sb("p0_va%d" % i, [128, 4, 129], BF16) for i in range(2)]
        vbs = [sb("p0_vb%d" % i, [128, 8, 65], BF16) for i in range(2)]

        S.dma("sp", gb[:, 0, :], lnin[0:1, :].partition_broadcast(128), reads=[], writes=["gb0"], key="init", group=True)
        S.dma("sp", gb[:, 1, :], lnin[1:2, :].partition_broadcast(128), reads=[], writes=["gb1"], key="init", group=True)
        wv = w_in[:, 0:3072].rearrange("(k p) c -> p k c", p=128)
        for k in range(8):
            S.dma("pool", wkv[:, k, :], wv[:, k, :], reads=[], writes=["wkv%d" % k], key="init", group=True)
        for i in range(2):
            S.add("pool", "memset", vas[i][:, :, 128:129], 1.0, reads=[], writes=["vas1_%d" % i])
            S.add("pool", "memset", vbs[i][:, :, 64:65], 1.0, reads=[], writes=["vbs1_%d" % i])
        wk_all = ["wkv%d" % k for k in range(8)]

        tp_banks = [psum[0], psum[1]]
        kq_banks = [psum[2], psum[3], psum[4]]
        v_banks = [psum[5], psum[6], psum[7]]
        vcnt = 0
        kcnt = 0
        def stage_a(kx):
            b2 = kx % 2
            X, XN, HB = xt[b2], xn[b2], hb[b2]
            rx, rxn, rhb = "x%d" % b2, "xn%d" % b2, "hb%d" % b2
            S.dma("sp", X[:], xall[kx * 128:(kx + 1) * 128, :], reads=[], writes=[rx])
            layer_norm_tile(S, X, rx, XN, rxn, HB, rhb, st6[b2], mv[b2], "p0s%d" % b2, gb, ["gb0", "gb1"])

        stage_a(0)
        for kx in range(NBLK):
            b2 = kx % 2
            u, blk = divmod(kx, 4)
            g2 = u % 2
            X, XN, HB = xt[b2], xn[b2], hb[b2]
            rx, rxn, rhb = "x%d" % b2, "xn%d" % b2, "hb%d" % b2
            if kx + 1 < NBLK:
                stage_a(kx + 1)
            tpb = tp_banks[b2]
            tpv = tpb[:].bitcast(BF16)
            rtp = "tp%d" % b2
            for k in range(8):
                S.add("pe", "transpose", tpv[:, k * 128:(k + 1) * 128], HB[:, k * 128:(k + 1) * 128], ident[:],
                      reads=[rhb, "ident"], writes=[rtp])
            rhT = "hT%d_%d" % (g2, blk)
            S.add("act", "copy", hT[g2][:, :, blk * 128:(blk + 1) * 128],
                  tpv.rearrange("p (k t) -> p k t", k=8), reads=[rtp], writes=[rhT])
            for which, (c0, stg, rs, dst, nh, dv) in enumerate(
                    [(1024, vas[b2], "vas_%d" % b2, VA, 4, 128), (2560, vbs[b2], "vbs_%d" % b2, VB, 8, 64)]):
                vb = v_banks[vcnt % 3]
                rv = "vps%d" % (vcnt % 3)
                vcnt += 1
                for kc in range(8):
                    S.add("pe", "matmul", vb[:], hT[g2][:, kc, blk * 128:(blk + 1) * 128], wkv[:, kc, c0:c0 + 512],
                          start=(kc == 0), stop=(kc == 7), reads=[rhT, "wkv%d" % kc], writes=[rv])
                eng = "act" if which == 0 else "dve"
                if eng == "act":
                    S.add("act", "copy", stg[:, :, 0:dv], vb[:].rearrange("p (h d) -> p h d", h=nh),
                          reads=[rv], writes=[rs])
                else:
                    S.add("dve", "tensor_copy", stg[:, :, 0:dv], vb[:].rearrange("p (h d) -> p h d", h=nh),
                          reads=[rv], writes=[rs])
                S.dma("sp", dst[kx], stg[:].rearrange("p h d -> p (h d)"),
                      reads=[rs, ("vas1_%d" if which == 0 else "vbs1_%d") % b2], writes=["dram_v%d_%d" % (which, kx)],
                      key="vst%d_%d" % (which, b2))
            if blk == 3:
                KQ = kq[g2]
                rgrp = ["hT%d_%d" % (g2, t) for t in range(4)]
                for ci in range(16):
                    kind, m = divmod(ci, 4)
                    c0 = [512, 2048, 0, 1536][kind] + 128 * m
                    pb = kq_banks[kcnt % 3]
                    rp = "kqps%d" % (kcnt % 3)
                    kcnt += 1
                    isq = kind >= 2
                    N = 256 if isq else 512
                    for kc in range(8):
                        if isq:
                            rhs = hT[g2][:, kc, :].rearrange("p (a b t) -> p a b t", a=2, b=2)[:, :, 1, :]
                            o = pb[:, 0:256].rearrange("p (a t) -> p a t", a=2)
                        else:
                            rhs = hT[g2][:, kc, :]
                            o = pb[:, :]
                        S.add("pe", "matmul", o, wkv[:, kc, c0:c0 + 128], rhs, start=(kc == 0), stop=(kc == 7),
                              reads=rgrp + ["wkv%d" % kc], writes=[rp])
                    rk = "kq%d_%d" % (g2, ci)
                    if isq:
                        S.add("act", "activation", KQ[:, ci, 0:N], pb[:, 0:N], AF.Copy, scale=0.125,
                              reads=[rp], writes=[rk])
                    else:
                        S.add("dve", "tensor_copy", KQ[:, ci, 0:N], pb[:, 0:N], reads=[rp], writes=[rk])
                    dst = [KAT, KBT, QAT, QBT][kind]
                    S.dma("sp" if ci % 2 == 0 else "pool", dst[m][:, u * N:(u + 1) * N], KQ[:, ci, 0:N], reads=[rk],
                          writes=["dram_kq%d_%d_%d" % (kind, m, u)], key="kqst%d_%d" % (g2, ci))
        S.flush()


def layer_norm_tile(S, X, rx, XN, rxn, OUT, rout, st6, mv, rs, gb, rgb):
    for c in range(2):
        S.add("dve", "bn_stats", st6[:, c, :], X[:, c * 512:(c + 1) * 512], reads=[rx], writes=[rs + "a"])
    S.add("dve", "bn_aggr", mv[:, 0:2], st6[:].rearrange("p a b -> p (a b)"), reads=[rs + "a"], writes=[rs + "b"])
    S.add("dve", "scalar_tensor_tensor", XN[:], X[:], mv[:, 0:1], gb[:, 0, :], ALU.subtract, ALU.mult,
          reads=[rx, rs + "b", rgb[0]], writes=[rxn])
    S.add("pool", "tensor_scalar", mv[:, 2:3], mv[:, 1:2], EPS, None, ALU.add, reads=[rs + "b"], writes=[rs + "c"])
    S.add("pool", "tensor_tensor", mv[:, 3:4], mv[:, 2:3], mv[:, 4:5], ALU.pow, reads=[rs + "c", rs + "k"], writes=[rs + "d"])
    S.add("dve", "scalar_tensor_tensor", OUT[:], XN[:], mv[:, 3:4], gb[:, 1, :], ALU.mult, ALU.add,
          reads=[rxn, rs + "d", rgb[1]], writes=[rout])


def ln_consts(S, mv, rs):
    S.add("pool", "memset", mv[:, 4:5], -0.5, reads=[], writes=[rs + "k"])

def phaseA(nc, S, psum, inp, KAT, QAT, VA, OA):
    with ExitStack() as st:
        sb = lambda name, shape, dt: st.enter_context(nc.sbuf_tensor(name, list(shape), dt))
        vall = sb("pa_v", [128, NBLK, 4 * 129], BF16)
        kt = [sb("pa_k%d" % i, [128, NBLK * 128], BF16) for i in range(2)]
        qt = [sb("pa_q%d" % i, [128, 2, NTOK], BF16) for i in range(2)]
        oa = sb("pa_oa", [128, NOWN, 512], BF16)
        abias = sb("pa_ab", [128, 2, 4, 64], F32)
        adiag = sb("pa_ad", [128, 4, 256], F32)
        lamt = sb("pa_lam", [128, 4, 64], F32)
        lams = sb("pa_lams", [128, 8], F32)
        subw = sb("pa_sw", [128, 128], F32)
        NPT = 4
        pt = [sb("pa_pt%d" % i, [128, 512], BF16) for i in range(NPT)]
        dtmp = [sb("pa_dt%d" % i, [128, 512], F32) for i in range(2)]
        fin = [sb("pa_fin%d" % i, [128, 8], F32) for i in range(2)]
        t0 = [sb("pa_t0%d" % i, [128, 128], F32) for i in range(2)]
        av = [sb("pa_a%d" % i, [128, 128], F32) for i in range(2)]
        junk = sb("pa_junk", [128, 128], F32)
        zt = sb("pa_zt", [1, 512], BF16)
        S.add("pool", "memset", zt[:], 0.0, reads=[], writes=["zt"])

        for i in range(2):
            S.add("pool", "memset", qt[i][0:64, 1, :], 0.0, reads=[], writes=["qtz%d" % i])
            S.add("pool", "memset", qt[i][64:128, 0, :], 0.0, reads=[], writes=["qtz%d" % i])
        for c in range(4):
            S.dma("sp" if c % 2 == 0 else "pool", vall[:, c * 16:(c + 1) * 16, :],
                  VA[c * 16:(c + 1) * 16].rearrange("k p d -> p k d"), reads=[], writes=["vall%d" % c],
                  key="initA", group=True)
        S.dma("sp", abias[:].rearrange("p a b c -> p (a b c)"), inp["abias"][:, :], reads=[], writes=["abias"],
              key="initA", group=True)
        S.dma("sp", adiag[:].rearrange("p a b -> p (a b)"), inp["adiag"][:, :], reads=[], writes=["adiag"],
              key="initA", group=True)
        for v in range(4):
            S.dma("sp", lamt[:, v, :], inp["lamv"][v:v + 1, :].partition_broadcast(128), reads=[], writes=["lamt"],
                  key="initA", group=True)
        S.dma("sp", subw[:], inp["subln"][0:1, :].partition_broadcast(128), reads=[], writes=["subw"],
              key="initA", group=True)
        for v in range(2):
            S.add("dve", "tensor_tensor", lamt[:, 2 * v, :], lamt[:, 2 * v, :], lamt[:, 2 * v + 1, :], ALU.mult,
                  reads=["lamt"], writes=["lamt"])
            S.add("dve", "reduce_sum", lams[:, v:v + 1], lamt[:, 2 * v, :], AX.X, reads=["lamt"], writes=["lams"])
        S.add("act", "activation", lams[:, 2:4], lams[:, 0:2], AF.Exp, reads=["lams"], writes=["lams"])
        S.add("dve", "tensor_tensor", lams[:, 4:5], lams[:, 3:4], lams[:, 2:3], ALU.subtract,
              reads=["lams"], writes=["lams"])
        S.add("dve", "tensor_scalar", lams[:, 4:5], lams[:, 4:5], -LAM_INIT, None, ALU.add,
              reads=["lams"], writes=["lams"])
        S.add("dve", "tensor_scalar", subw[:], subw[:], 1.0 - LAM_INIT, None, ALU.mult, reads=["subw"], writes=["subw"])

        s_banks = [psum[0], psum[1], psum[2], psum[3]]
        o_banks = [psum[4], psum[5], psum[6], psum[7]]
        LOOK = 2
        steps = []
        for h in range(PA_DBG.get("heads", 4)):
            for ia in range(0, PA_DBG.get("qblocks", NOWN), 2):
                rmax = [min(A_WINDOW[h], 2 * (ia + s_) + 1) for s_ in range(2)]
                for rel in range(max(rmax), -1, -1):
                    slots = []
                    for s_ in range(2):
                        i = ia + s_
                        if rel <= rmax[s_]:
                            slots.append((s_, i, 2 * i + 1 - rel, rel == rmax[s_]))
                    steps.append((h, rel, slots))
        loaded = set()
        deferred = []
        pcnt = {}

        def head_load(h):
            KT, QT = kt[h % 2], qt[h % 2]
            rk, rq = "kt%d" % (h % 2), "qt%d" % (h % 2)
            for c in range(4):
                S.dma("sp" if c % 2 == 0 else "pool", KT[:, c * 2048:(c + 1) * 2048], KAT[h][:, c * 2048:(c + 1) * 2048],
                      reads=[], writes=[rk + "_%d" % c], key=rk, group=True)
            S.dma("sp", QT[0:64, 0, :], QAT[h][0:64, :], reads=[], writes=[rq + "a"])
            S.dma("pool", QT[64:128, 1, :], QAT[h][64:128, :], reads=[], writes=[rq + "b"])

        def emit_s(sidx):
            h, rel, slots = steps[sidx]
            if h not in loaded:
                loaded.add(h)
                head_load(h)
            KT, QT = kt[h % 2], qt[h % 2]
            rk, rq = "kt%d" % (h % 2), "qt%d" % (h % 2)
            sbk = s_banks[sidx % 4]
            rs = "sps%d" % (sidx % 4)
            P = pt[sidx % NPT]
            rp = "pt%d" % (sidx % NPT)
            for (s_, i, kx, first) in slots:
                for c in range(2):
                    col = s_ * 256 + c * 128
                    S.add("pe", "matmul", sbk[:, col:col + 128], KT[:, kx * 128:(kx + 1) * 128],
                          QT[:, c, i * 128:(i + 1) * 128], start=True, stop=True,
                          reads=[rk + "_%d" % (kx // 16), rq + "a", rq + "b", "qtz%d" % (h % 2)], writes=[rs])
            lo = slots[0][0] * 256
            hi = slots[-1][0] * 256 + 256
            if rel == 0:
                DT = dtmp[sidx % 2]
                rd = "dtmp%d" % (sidx % 2)
                n2 = (hi - lo) // 256
                S.add("dve", "tensor_tensor", DT[:, lo:hi].rearrange("p (a b) -> p a b", a=n2),
                      sbk[:, lo:hi].rearrange("p (a b) -> p a b", a=n2),
                      adiag[:, h, :].unsqueeze(1).to_broadcast([128, n2, 256]), ALU.add, reads=[rs, "adiag"], writes=[rd])
                S.add("act", "activation", P[:, lo:hi], DT[:, lo:hi], AF.Exp, reads=[rd], writes=[rp])
            elif any(kx == 0 for (_, _, kx, _) in slots):
                for (s_, i, kx, first) in slots:
                    z = 1 if kx == 0 else 0
                    S.add("act", "activation", P[:, s_ * 256:s_ * 256 + 256], sbk[:, s_ * 256:s_ * 256 + 256], AF.Exp,
                          bias=abias[:, z, h, rel:rel + 1], reads=[rs, "abias"], writes=[rp])
            else:
                S.add("act", "activation", P[:, lo:hi], sbk[:, lo:hi], AF.Exp, bias=abias[:, 0, h, rel:rel + 1],
                      reads=[rs, "abias"], writes=[rp])

        def emit_av(sidx):
            h, rel, slots = steps[sidx]
            P = pt[sidx % NPT]
            rp = "pt%d" % (sidx % NPT)
            for (s_, i, kx, first) in slots:
                key = (h, i)
                if key not in pcnt:
                    pcnt[key] = len(pcnt)
                qn = pcnt[key]
                ob = o_banks[qn % 4]
                ro = "ops%d" % (qn % 4)
                if first:
                    S.add("pe", "matmul", ob[:, 0:258], zt[0:1, 0:128], zt[0:1, 0:258], start=True, stop=False,
                          reads=["zt"], writes=[ro])
                for c in range(2):
                    col = s_ * 256 + c * 128
                    S.add("pe", "matmul", ob[:, c * 129:(c + 1) * 129], P[:, col:col + 128],
                          vall[:, kx, h * 129:(h + 1) * 129], start=False, stop=(rel == 0 and c == 1),
                          reads=[rp, "vall%d" % (kx // 16)], writes=[ro])
                if rel == 0:
                    deferred.append((sidx + 3, h, i, qn))

        def emit_fin(h, i, qn):
            ob = o_banks[qn % 4]
            ro = "ops%d" % (qn % 4)
            f2 = qn % 2
            F, T0, A = fin[f2], t0[f2], av[f2]
            rf = "fin%d" % f2
            S.add("dve", "reciprocal", F[:, 0:2], ob[:, 0:258].rearrange("p (c d) -> p c d", c=2)[:, :, 128],
                  reads=[ro], writes=[rf])
            S.add("dve", "tensor_tensor", F[:, 2:3], F[:, 1:2], lams[:, 4:5], ALU.mult, reads=[rf, "lams"], writes=[rf])
            S.add("act", "activation", T0[:], ob[:, 0:128], AF.Copy, scale=F[:, 0:1], reads=[ro, rf], writes=[rf + "t"])
            S.add("dve", "scalar_tensor_tensor", A[:], ob[:, 129:257], F[:, 2:3], T0[:], ALU.mult, ALU.add,
                  reads=[ro, rf, rf + "t"], writes=[rf + "a"])
            S.add("dve", "scalar_tensor_tensor", junk[:], A[:], 1.0, A[:], ALU.mult, ALU.mult, accum_out=F[:, 3:4],
                  reads=[rf + "a"], writes=["junk", rf + "s"])
            S.add("dve", "tensor_scalar", F[:, 3:4], F[:, 3:4], 1.0 / 128, EPS, ALU.mult, ALU.add,
                  reads=[rf + "s"], writes=[rf + "s"])
            S.add("act", "activation", F[:, 4:5], F[:, 3:4], AF.Ln, reads=[rf + "s"], writes=[rf + "l"])
            S.add("act", "activation", F[:, 5:6], F[:, 4:5], AF.Exp, scale=-0.5, reads=[rf + "l"], writes=[rf + "r"])
            S.add("dve", "scalar_tensor_tensor", oa[:, i, h * 128:(h + 1) * 128], A[:], F[:, 5:6], subw[:],
                  ALU.mult, ALU.mult, reads=[rf + "a", rf + "r", "subw"], writes=["oa%d" % i])

        ns = len(steps)
        for sidx in range(ns + LOOK + 4):
            if sidx < ns:
                emit_s(sidx)
            if 0 <= sidx - LOOK < ns:
                emit_av(sidx - LOOK)
            while deferred and deferred[0][0] <= sidx:
                _, h_, i_, qn_ = deferred.pop(0)
                if not PA_DBG.get("nofin"):
                    emit_fin(h_, i_, qn_)
        assert not deferred
        for c in range(4):
            S.dma("sp" if c % 2 == 0 else "pool", OA[c * 1024:(c + 1) * 1024, :].rearrange("(i p) d -> p i d", p=128),
                  oa[:, c * 8:(c + 1) * 8, :], reads=["oa%d" % i for i in range(c * 8, c * 8 + 8)],
                  writes=["OA%d" % c], key="oast")
        S.flush()


def _const_tables(j, rel_bias):
    bk = np.arange(128, dtype=np.float32)
    parmask = np.full((128, 1), 0.0 if j == 1 else NEG, np.float32)
    abias = np.zeros((128, 2, 4, 64), np.float32)
    adiag = np.zeros((128, 4, 2, 128), np.float32)
    for h in range(4):
        sl = np.float32(SLOPES[h])
        for rel in range(64):
            abias[:, 0, h, rel] = sl * (bk - 128.0 * rel)
            abias[:, 1, h, rel] = sl * (bk - 128.0 * rel) + parmask[0, 0]
        bq = bk[None, :]
        bkk = bk[:, None]
        t = -sl * np.abs(bq - bkk) + sl * bq
        t = np.where((bkk // 64) > (bq // 64), NEG, t)
        adiag[:, h, 0, :] = t
        adiag[:, h, 1, :] = t
    kk = np.arange(128)[:, None, None]
    tb = np.arange(5)[None, :, None]
    q = np.arange(128)[None, None, :]
    rel = (tb - 4) * 128 + kk - q
    kch = 2 * (tb - 4) + kk // 64
    qch = q // 64
    valid = (kch - qch >= -8) & (kch - qch <= 0)
    idx = np.clip(rel, -128, 128) + 128
    btab = np.zeros((128, 8, 5, 128), np.float32)
    for hb in range(8):
        btab[:, hb] = np.where(valid, rel_bias[hb][idx], NEG)
    return parmask, abias.reshape(128, -1), adiag.reshape(128, -1), btab.reshape(128, -1)


def _core_inputs(core, inputs):
    b, j = divmod(core, 2)
    xb = np.asarray(inputs["x"][b], np.float32)
    if j == 0:
        xall = np.concatenate([np.zeros((128, D), np.float32), xb[:63 * 128]], 0)
    else:
        xall = xb
    parmask, abias, adiag, btab = _const_tables(j, np.asarray(inputs["rel_bias"][0], np.float32))
    m = {
        "xall": np.ascontiguousarray(xall),
        "lnin": np.stack([inputs["ln_in_g"], inputs["ln_in_b"]]).astype(np.float32),
        "w_in": np.ascontiguousarray(inputs["w_in"][0]),
        "ident": np.eye(128, dtype=np.float32),
        "abias": abias, "adiag": adiag, "btab": btab, "parmask": parmask,
        "lamv": np.stack([inputs["lambda_q1"][0], inputs["lambda_k1"][0], inputs["lambda_q2"][0],
                          inputs["lambda_k2"][0]]).astype(np.float32),
        "subln": np.asarray(inputs["subln_w"], np.float32).reshape(1, 128),
        "b_gate": np.ascontiguousarray(np.asarray(inputs["b_gate"][0], np.float32).reshape(16, 128).T),
        "w_ba": np.ascontiguousarray(inputs["w_branch_a"][0]), "w_bb": np.ascontiguousarray(inputs["w_branch_b"][0]),
        "w_out": np.ascontiguousarray(inputs["w_out"][0]),
        "ln1": np.stack([inputs["ln1_g"][0], inputs["ln1_b"][0]]).astype(np.float32),
        "w_router": np.ascontiguousarray(inputs["w_router"][0]),
        "b_router": np.asarray(inputs["b_router"][0], np.float32).reshape(1, NEXP),
        "rconst": _rconst(),
        "w_e1": np.asarray(inputs["w_exp_in"][0]), "w_e2": np.asarray(inputs["w_exp_out"][0]),
        "b_e1": np.ascontiguousarray(np.asarray(inputs["b_exp_in"][0], np.float32).reshape(NEXP, 16, 128)
                                     .transpose(2, 0, 1).reshape(128, NEXP * 16)),
        "b_e2": np.asarray(inputs["b_exp_out"][0], np.float32),
        "ln2": np.stack([inputs["ln2_g"][0], inputs["ln2_b"][0]]).astype(np.float32),
    }
    return m


def phaseB(nc, S, psum, inp, KBT, QBT, VB, OB):
    with ExitStack() as st:
        sb = lambda name, shape, dt: st.enter_context(nc.sbuf_tensor(name, list(shape), dt))
        vall = sb("pb_v", [128, NBLK, 8 * 65], BF16)
        kt = sb("pb_k", [128, 4, NBLK * 128], BF16)
        btab = sb("pb_bt", [128, 8, 5, 128], F32)
        pm = sb("pb_pm", [128, 1], F32)
        qp = [sb("pb_q%d" % i, [128, 8, 128], BF16) for i in range(2)]
        tmp = [sb("pb_t%d" % i, [128, 5, 128], F32) for i in range(2)]
        pt = [sb("pb_p%d" % i, [128, 5, 128], BF16) for i in range(2)]
        rc = [sb("pb_r%d" % i, [128, 8], F32) for i in range(2)]
        ob = [sb("pb_o%d" % i, [128, 8, 64], BF16) for i in range(2)]
        for c in range(4):
            S.dma("sp" if c % 2 == 0 else "pool", vall[:, c * 16:(c + 1) * 16, :],
                  VB[c * 16:(c + 1) * 16].rearrange("k p d -> p k d"), reads=[], writes=["vball"], key="initB", group=True)
            S.dma("pool" if c % 2 == 0 else "sp", kt[:, c, :], KBT[c], reads=[], writes=["kball"], key="initB", group=True)
        S.dma("sp", btab[:].rearrange("p a b c -> p (a b c)"), inp["btab"][:, :], reads=[], writes=["btab"],
              key="initB", group=True)
        S.dma("sp", pm[:], inp["parmask"][:, :], reads=[], writes=["pm"], key="initB", group=True)
        for i in range(2):
            for hb in range(8):
                lo = 64 * (1 - hb % 2)
                S.add("pool", "memset", qp[i][lo:lo + 64, hb, :], 0.0, reads=[], writes=["qbz%d" % i])
        s_banks = [(psum[0], psum[1]), (psum[2], psum[3])]
        o_banks = [(psum[4], psum[5]), (psum[6], psum[7])]
        steps = [(i, hb) for i in range(NOWN) for hb in range(8)]

        def q_load(i):
            Q = qp[i % 2]
            rq = "qb%d" % (i % 2)
            for m in range(4):
                for half in range(2):
                    S.dma("sp" if half == 0 else "pool", Q[64 * half:64 * half + 64, 2 * m + half, :],
                          QBT[m][64 * half:64 * half + 64, i * 128:(i + 1) * 128], reads=[], writes=[rq + "_%d" % (2 * m + half)],
                          key=rq, group=True)

        def emit_s(sidx):
            i, hb = steps[sidx]
            if hb == 0 and i + 1 < NOWN:
                q_load(i + 1)
            kxq = 2 * i + 1
            Q = qp[i % 2]
            rq = "qb%d" % (i % 2)
            tbs = [tb for tb in range(5) if kxq - 4 + tb >= 0]
            m = hb // 2
            sA, sB = s_banks[sidx % 2]
            rs = "sbps%d" % (sidx % 2)
            T, P = tmp[sidx % 2], pt[sidx % 2]
            rt, rp = "bt%d" % (sidx % 2), "bp%d" % (sidx % 2)
            for tb in tbs:
                kx = kxq - 4 + tb
                o = sA[:, tb * 128:(tb + 1) * 128] if tb < 4 else sB[:, 0:128]
                S.add("pe", "matmul", o, kt[:, m, kx * 128:(kx + 1) * 128], Q[:, hb, :], start=True, stop=True,
                      reads=["kball", rq + "_%d" % hb, "qbz%d" % (i % 2)], writes=[rs + ("a" if tb < 4 else "b")])
            lo = tbs[0]
            if lo < 4:
                S.add("dve", "tensor_tensor", T[:, lo:4, :], sA[:, lo * 128:512].rearrange("p (a b) -> p a b", b=128),
                      btab[:, hb, lo:4, :], ALU.add, reads=[rs + "a", "btab"], writes=[rt + "a"])
            S.add("dve", "tensor_tensor", T[:, 4, :], sB[:, 0:128], btab[:, hb, 4, :], ALU.add,
                  reads=[rs + "b", "btab"], writes=[rt + "b"])
            segs = []
            z = [tb for tb in tbs if kxq - 4 + tb == 0]
            if z:
                segs.append((z[0], z[0] + 1, True))
                if z[0] + 1 < 5:
                    segs.append((z[0] + 1, 5, False))
            else:
                segs.append((lo, 5, False))
            for (a_, b_, msk) in segs:
                kw = dict(bias=pm[:, 0:1]) if msk else {}
                S.add("act", "activation", P[:, a_:b_, :], T[:, a_:b_, :], AF.Exp, reads=[rt + "a", rt + "b", "pm"],
                      writes=[rp], **kw)

        def emit_av(sidx):
            i, hb = steps[sidx]
            kxq = 2 * i + 1
            tbs = [tb for tb in range(5) if kxq - 4 + tb >= 0]
            P = pt[sidx % 2]
            rp = "bp%d" % (sidx % 2)
            obk = o_banks[i % 2]
            ro = "obps%d" % (i % 2)
            ot = obk[hb // 4]
            oc = (hb % 4) * 65
            for tb in tbs:
                kx = kxq - 4 + tb
                S.add("pe", "matmul", ot[:, oc:oc + 65], P[:, tb, :], vall[:, kx, hb * 65:(hb + 1) * 65],
                      start=(tb == tbs[0]), stop=(tb == 4), reads=[rp, "vball"], writes=[ro + "_%d" % (hb // 4)])

        def emit_fin(i):
            obk = o_banks[i % 2]
            ro = "obps%d" % (i % 2)
            R, O = rc[i % 2], ob[i % 2]
            rr, rob = "brc%d" % (i % 2), "bob%d" % (i % 2)
            for g in range(2):
                ov = obk[g][:, 0:260].rearrange("p (h d) -> p h d", h=4)
                S.add("dve", "reciprocal", R[:, 4 * g:4 * g + 4], ov[:, :, 64], reads=[ro + "_%d" % g], writes=[rr])
                for hh in range(4):
                    hb = 4 * g + hh
                    S.add("act", "activation", O[:, hb, :], ov[:, hh, 0:64], AF.Copy, scale=R[:, hb:hb + 1],
                          reads=[ro + "_%d" % g, rr], writes=[rob])
            S.dma("sp", OB[i * 128:(i + 1) * 128, :], O[:].rearrange("p h d -> p (h d)"), reads=[rob], writes=["OB%d" % i],
                  key="obst%d" % (i % 2))

        q_load(0)
        ns = len(steps)
        fin_at = {}
        for sidx in range(ns + 4):
            if sidx < ns:
                emit_s(sidx)
            if 0 <= sidx - 1 < ns:
                emit_av(sidx - 1)
                i_, hb_ = steps[sidx - 1]
                if hb_ == 7:
                    fin_at[sidx + 2] = i_
            if sidx in fin_at:
                emit_fin(fin_at.pop(sidx))
        assert not fin_at
        S.flush()


def _rconst():
    t = np.arange(128)
    U = (t[:, None] < t[None, :]).astype(np.float32)
    ones = np.ones((128, 128), np.float32)
    iota = np.tile(np.arange(32, dtype=np.float32)[None, :], (128, 1))
    ecap = iota * E_CAP
    return np.concatenate([U, ones, iota, ecap], 1)


def phaseM(nc, S, psum, ident, inp, xall, lnin, w_in, OA, OB, H1, XS, pers):
    with ExitStack() as st:
        sb = lambda name, shape, dt: st.enter_context(nc.sbuf_tensor(name, list(shape), dt))
        wg = sb("pm_wg", [128, 8, 2048], BF16)
        wba = sb("pm_wba", [128, 4, D], BF16)
        wbb = sb("pm_wbb", [128, 4, D], BF16)
        wo = sb("pm_wo", [128, 8, D], BF16)
        wr = sb("pm_wr", [128, 8, NEXP], F32)
        gbin = sb("pm_gbin", [128, 2, D], F32)
        gb1 = sb("pm_gb1", [128, 2, D], F32)
        bg = sb("pm_bg", [128, 16], F32)
        brb = sb("pm_brb", [128, NEXP], F32)
        rcon = sb("pm_rc", [128, 320], F32)
        ub = sb("pm_ub", [128, 256], BF16)
        identf = sb("pm_idf", [128, 128], F32)
        carry = sb("pm_carry", [128, NEXP], F32)
        xt = [sb("pm_x%d" % i, [128, D], F32) for i in range(2)]
        xn = sb("pm_xn", [128, D], F32)
        hres_all = [sb("pm_hr%d" % i, [128, D], F32) for i in range(4)]
        hb = sb("pm_hb", [128, D], BF16)
        hT_all = [sb("pm_hT%d" % i, [128, 8, 256], BF16) for i in range(2)]
        oab = [sb("pm_oab%d" % i, [128, D], BF16) for i in range(2)]
        oT_all = [sb("pm_oT%d" % i, [128, 8, 256], BF16) for i in range(2)]
        gT = sb("pm_gT", [128, 16, 256], F32)
        mT = sb("pm_mT", [128, 8, 256], BF16)
        t1 = [sb("pm_t1%d" % i, [128, 256], F32) for i in range(2)]
        t2 = [sb("pm_t2%d" % i, [128, 256], F32) for i in range(2)]
        rr_all = [sb("pm_r%d" % i, [128, D], F32) for i in range(4)]
        h1b = [sb("pm_h1b%d" % i, [128, D], BF16) for i in range(2)]
        h1T = sb("pm_h1T", [128, 2, 8, 128], F32)
        st6 = sb("pm_st6", [128, 2, 6], F32)
        mv = sb("pm_mv", [128, 8], F32)
        ln_consts(S, mv, "pms")
        sm = [sb("pm_sm%d" % i, [128, 160], F32) for i in range(2)]
        idx8 = [sb("pm_ix%d" % i, [128, 8], U32) for i in range(2)]
        maskb = [sb("pm_mk%d" % i, [128, NEXP], BF16) for i in range(2)]
        junk = sb("pm_junk", [128, 4 * NEXP], F32)

        S.group_keys.update(["xsc0@sw", "xsc1@sw"])
        bc_reg = nc.gpsimd.alloc_register("xs_bc")
        nc.gpsimd.reg_mov(bc_reg, NEXP * E_CAP - 1)
        ini = dict(key="initM", group=True)
        wgv = w_in[:, 3072:5120].rearrange("(k p) c -> p k c", p=128)
        for k in range(8):
            S.dma("pool", wg[:, k, :], wgv[:, k, :], reads=[], writes=["wg"], **ini)
        S.dma("pool", wba[:], inp["w_ba"].rearrange("(k p) c -> p k c", p=128), reads=[], writes=["wba"], **ini)
        S.dma("pool", wbb[:], inp["w_bb"].rearrange("(k p) c -> p k c", p=128), reads=[], writes=["wbb"], **ini)
        S.dma("pool", wo[:], inp["w_out"].rearrange("(k p) c -> p k c", p=128), reads=[], writes=["wo"], **ini)
        S.dma("sp", wr[:], inp["w_router"].rearrange("(k p) c -> p k c", p=128), reads=[], writes=["wr"], **ini)
        S.dma("sp", gbin[:, 0, :], lnin[0:1, :].partition_broadcast(128), reads=[], writes=["gbin0"], **ini)
        S.dma("sp", gbin[:, 1, :], lnin[1:2, :].partition_broadcast(128), reads=[], writes=["gbin1"], **ini)
        S.dma("sp", gb1[:, 0, :], inp["ln1"][0:1, :].partition_broadcast(128), reads=[], writes=["gb10"], **ini)
        S.dma("sp", gb1[:, 1, :], inp["ln1"][1:2, :].partition_broadcast(128), reads=[], writes=["gb11"], **ini)
        S.dma("sp", bg[:], inp["b_gate"][:, :], reads=[], writes=["bg"], **ini)
        S.dma("sp", brb[:], inp["b_router"][0:1, :].partition_broadcast(128), reads=[], writes=["brb"], **ini)
        S.dma("sp", rcon[:], inp["rconst"][:, :], reads=[], writes=["rcon"], **ini)
        S.dma("pool", ub[:], inp["rconst"][:, 0:256], reads=[], writes=["ub"], **ini)
        S.dma("sp", identf[:], inp["ident"][:, :], reads=[], writes=["identf"], **ini)
        S.add("pool", "memset", carry[:], 0.0, reads=[], writes=["carry"])
        iota = rcon[:, 256:288]
        ecap = rcon[:, 288:320]

        tpb = psum[0]
        tpv = tpb[:].bitcast(BF16)
        g_banks = [psum[1], psum[2]]
        brA, brBk = psum[3], psum[4]
        m_banks = [psum[5], psum[6]]
        rb = psum[7]
        gcnt = 0
        NG = NOWN // 2

        def load(G):
            for t in range(2):
                i = 2 * G + t
                kx = 2 * i + 1
                S.dma("sp", xt[t][:], xall[kx * 128:(kx + 1) * 128, :], reads=[], writes=["mx%d" % t])
                S.dma("sp", oab[t][:, 0:512], OA[i * 128:(i + 1) * 128, :], reads=[], writes=["oab%da" % t])
                S.dma("sp", oab[t][:, 512:1024], OB[i * 128:(i + 1) * 128, :], reads=[], writes=["oab%db" % t])

        hbs = [hb, sb("pm_hb2", [128, D], BF16)]

        def stage_a(G, part="ab"):
            g2 = G % 2
            hres = hres_all[2 * g2:2 * g2 + 2]
            hT, oT = hT_all[g2], oT_all[g2]
            if "a" in part:
                load(G)
                for t in range(2):
                    layer_norm_tile(S, xt[t], "mx%d" % t, xna, "mxna", hres[t], "hres%d_%d" % (g2, t), st6a, mva, "pmsa", gbin,
                                    ["gbin0", "gbin1"])
                    S.add("pool", "tensor_copy", hbs[t][:], hres[t][:], reads=["hres%d_%d" % (g2, t)], writes=["mhb%d" % t])
            if "b" not in part:
                return
            for t in range(2):
                hb = hbs[t]
                for k in range(8):
                    S.add("pe", "transpose", tpv[:, k * 128:(k + 1) * 128], hb[:, k * 128:(k + 1) * 128], ident[:],
                          reads=["mhb%d" % t, "ident"], writes=["mtp"])
                S.add("act", "copy", hT[:, :, t * 128:(t + 1) * 128], tpv.rearrange("p (k t) -> p k t", k=8),
                      reads=["mtp"], writes=["mhT%d_%d" % (g2, t)])
                for k in range(8):
                    S.add("pe", "transpose", tpv[:, k * 128:(k + 1) * 128], oab[t][:, k * 128:(k + 1) * 128], ident[:],
                          reads=["oab%da" % t, "oab%db" % t, "ident"], writes=["mtp"])
                S.add("act", "copy", oT[:, :, t * 128:(t + 1) * 128], tpv.rearrange("p (k t) -> p k t", k=8),
                      reads=["mtp"], writes=["moT%d_%d" % (g2, t)])

        env = (rr_all, h1b, h1T, xn, st6, mv, gb1, H1, g_banks, rb, identf, wr, brb, sm, idx8, maskb, carry, ub, iota, ecap,
               junk, pers, XS, bc_reg)
        stage_b2 = lambda G_, part: _pm_stage_b2(S, G_, env, part)
        st6a = sb("pm_st6a", [128, 2, 6], F32)
        xna = sb("pm_xna", [128, D], F32)
        mva = sb("pm_mva", [128, 8], F32)
        ln_consts(S, mva, "pmsa")
        stage_a(0)
        for G in range(NG):
            g2 = G % 2
            hres = hres_all[2 * g2:2 * g2 + 2]
            hT, oT = hT_all[g2], oT_all[g2]
            if G >= 1:
                stage_b2(G - 1, "a")
            if G + 1 < NG:
                stage_a(G + 1, "a")
            rhT = ["mhT%d_0" % g2, "mhT%d_1" % g2]
            roT = ["moT%d_0" % g2, "moT%d_1" % g2]
            for gc in range(16):
                gbk = g_banks[gcnt % 2]
                rg = "gps%d" % (gcnt % 2)
                gcnt += 1
                for kc in range(8):
                    S.add("pe", "matmul", gbk[:, 0:256], wg[:, kc, gc * 128:(gc + 1) * 128], hT[:, kc, :],
                          start=(kc == 0), stop=(kc == 7), reads=rhT + ["wg"], writes=[rg])
                S.add("act", "activation", gT[:, gc, :], gbk[:, 0:256], AF.Sigmoid, bias=bg[:, gc:gc + 1],
                      reads=[rg, "bg"], writes=["gT%d" % gc])
            if G + 1 < NG:
                stage_a(G + 1, "b")
            if G >= 1:
                stage_b2(G - 1, "b")
                stage_b2(G - 1, "c")
            for oc in range(8):
                for kc in range(4):
                    S.add("pe", "matmul", brA[:, 0:256], wba[:, kc, oc * 128:(oc + 1) * 128], oT[:, kc, :],
                          start=(kc == 0), stop=(kc == 3), reads=roT + ["wba"], writes=["brA"])
                for kc in range(4):
                    S.add("pe", "matmul", brBk[:, 0:256], wbb[:, kc, oc * 128:(oc + 1) * 128], oT[:, 4 + kc, :],
                          start=(kc == 0), stop=(kc == 3), reads=roT + ["wbb"], writes=["brB"])
                T1, T2 = t1[oc % 2], t2[oc % 2]
                S.add("dve", "tensor_tensor", T1[:], brA[:, 0:256], gT[:, oc, :], ALU.mult,
                      reads=["brA", "gT%d" % oc], writes=["mt1%d" % (oc % 2)])
                S.add("dve", "tensor_tensor", T2[:], brBk[:, 0:256], gT[:, 8 + oc, :], ALU.mult,
                      reads=["brB", "gT%d" % (8 + oc)], writes=["mt2%d" % (oc % 2)])
                S.add("pool", "tensor_tensor", mT[:, oc, :], T1[:], T2[:], ALU.add,
                      reads=["mt1%d" % (oc % 2), "mt2%d" % (oc % 2)], writes=["mT%d" % oc])
            rmT = ["mT%d" % oc for oc in range(8)]
            if G >= 1:
                stage_b2(G - 1, "d")
            for t in range(2):
                for half in range(2):
                    for kc in range(8):
                        S.add("pe", "matmul", m_banks[half][:, :], mT[:, kc, t * 128:(t + 1) * 128],
                              wo[:, kc, half * 512:(half + 1) * 512], start=(kc == 0), stop=(kc == 7),
                              reads=rmT + ["wo"], writes=["mps%d" % half])
                R = rr_all[2 * g2 + t]
                for half in range(2):
                    S.add("dve", "scalar_tensor_tensor", R[:, half * 512:(half + 1) * 512],
                          hres[t][:, half * 512:(half + 1) * 512], ALPHA, m_banks[half][:, :], ALU.mult, ALU.add,
                          reads=["hres%d_%d" % (g2, t), "mps%d" % half], writes=["mr%d_%d" % (g2, t)])
        for part in "abcd":
            stage_b2(NG - 1, part)
        if "DESTD" in pers:
            S.dma("sp", pers["DESTD"][:, :], pers["dest"][:].rearrange("p a b -> p (a b)"),
                  reads=["dest%d" % i for i in range(NOWN)], writes=["DESTD"])
            S.dma("sp", pers["GATED"][:, :], pers["gate"][:].rearrange("p a b -> p (a b)"),
                  reads=["gate%d" % i for i in range(NOWN)], writes=["GATED"])
        S.flush()


def _pm_stage_b2(S, G, env, part):
    (rr_all, h1b, h1T, xn, st6, mv, gb1, H1, g_banks, rb, identf, wr, brb, sm, idx8, maskb, carry, ub, iota, ecap, junk,
     pers, XS, bc_reg) = env
    g2 = G % 2
    for t in range(2):
        i = 2 * G + t
        R, HB = rr_all[2 * g2 + t], h1b[t]
        H = R
        rh = "mr%d_%d" % (g2, t)
        SM, IX, MK = sm[t], idx8[t], maskb[t]
        rs = "msm%d" % t
        lg, top8, idxf, e4 = SM[:, 0:32], SM[:, 32:40], SM[:, 40:44], SM[:, 44:48]
        nmax, den, rden = SM[:, 48:49], SM[:, 49:50], SM[:, 50:51]
        posf, ovf, dfull = SM[:, 52:84], SM[:, 84:116], SM[:, 116:148]
        c0 = 96 * t
        if part == "a":
            layer_norm_tile(S, R, rh, xn, "mxn", H, rh, st6, mv, "pms", gb1, ["gb10", "gb11"])
            S.dma("sp", H1[i * 128:(i + 1) * 128, :], H[:], reads=[rh], writes=["H1_%d" % i], key="h1st%d" % t)
            S.add("pool", "tensor_copy", HB[:], H[:], reads=[rh], writes=["mh1b%d" % t])
        elif part == "b":
            for k in range(8):
                bank = g_banks[k // 4]
                S.add("pe", "transpose", bank[:, (k % 4) * 128:(k % 4 + 1) * 128], H[:, k * 128:(k + 1) * 128],
                      identf[:], reads=[rh, "identf"], writes=["gps%d" % (k // 4)])
            S.add("act", "copy", h1T[:, t, 0:4, :], g_banks[0][:, :].rearrange("p (k t) -> p k t", k=4), reads=["gps0"],
                  writes=["h1Ta%d" % t])
            S.add("act", "copy", h1T[:, t, 4:8, :], g_banks[1][:, :].rearrange("p (k t) -> p k t", k=4), reads=["gps1"],
                  writes=["h1Tb%d" % t])
        elif part == "c":
            for kc in range(8):
                S.add("pe", "matmul", rb[:, c0:c0 + 32], h1T[:, t, kc, :], wr[:, kc, :], start=(kc == 0), stop=(kc == 7),
                      reads=["h1Ta%d" % t, "h1Tb%d" % t, "wr"], writes=["rb"])
            S.add("dve", "tensor_tensor", lg, rb[:, c0:c0 + 32], brb[:], ALU.add, reads=["rb", "brb"], writes=[rs])
            S.add("dve", "max", top8, lg, reads=[rs], writes=[rs])
            S.add("dve", "max_index", IX[:], top8, lg, reads=[rs], writes=[rs + "i"])
            S.add("dve", "tensor_copy", idxf, IX[:, 0:4], reads=[rs + "i"], writes=[rs])
            S.add("dve", "tensor_scalar", nmax, top8[:, 0:1], -1.0, None, ALU.mult, reads=[rs], writes=[rs])
            S.add("act", "activation", e4, top8[:, 0:4], AF.Exp, bias=nmax, accum_out=den, reads=[rs], writes=[rs])
            S.add("dve", "tensor_scalar", MK[:], lg, top8[:, 3:4], None, ALU.is_ge, reads=[rs], writes=[rs + "m"])
            S.add("dve", "reciprocal", rden, den, reads=[rs], writes=[rs])
            S.add("dve", "tensor_scalar", pers["gate"][:, i, :], e4, rden, None, ALU.mult, reads=[rs],
                  writes=["gate%d" % i])
        elif part == "d":
            S.add("pe", "matmul", rb[:, c0 + 32:c0 + 64], ub[:, 0:128], MK[:], start=True, stop=True,
                  reads=[rs + "m", "ub"], writes=["rb"])
            S.add("pe", "matmul", rb[:, c0 + 64:c0 + 96], ub[:, 128:256], MK[:], start=True, stop=True,
                  reads=[rs + "m", "ub"], writes=["rb"])
            S.add("dve", "tensor_tensor", posf, rb[:, c0 + 32:c0 + 64], carry[:], ALU.add, reads=["rb", "carry"],
                  writes=[rs])
            S.add("dve", "tensor_tensor", carry[:], rb[:, c0 + 64:c0 + 96], carry[:], ALU.add, reads=["rb", "carry"],
                  writes=["carry"])
            S.add("dve", "tensor_scalar", ovf, posf, float(E_CAP), 1.0e7, ALU.is_ge, ALU.mult, reads=[rs], writes=[rs])
            S.add("dve", "tensor_tensor", dfull, posf, ecap, ALU.add, reads=[rs, "rcon"], writes=[rs])
            S.add("dve", "tensor_tensor", dfull, dfull, ovf, ALU.add, reads=[rs], writes=[rs])
            oh3 = junk[:, 0:128].rearrange("p (k e) -> p k e", k=4)
            S.add("dve", "tensor_tensor", oh3, iota.unsqueeze(1).to_broadcast([128, 4, 32]),
                  idxf.unsqueeze(2).to_broadcast([128, 4, 32]), ALU.is_equal, reads=[rs, "rcon"], writes=["mjunk"])
            S.add("dve", "tensor_tensor", oh3, oh3, dfull.unsqueeze(1).to_broadcast([128, 4, 32]), ALU.mult,
                  reads=[rs, "mjunk"], writes=["mjunk"])
            S.add("dve", "tensor_reduce", SM[:, 152:156], oh3, AX.X, ALU.add, reads=["mjunk"], writes=[rs + "d"])
            S.add("dve", "tensor_copy", pers["dest"][:, i, :], SM[:, 152:156], reads=[rs + "d"], writes=["dest%d" % i])
            S.add("dve", "tensor_scalar", SM[:, 156:160], SM[:, 152:156], 1.0e6, None, ALU.is_lt, reads=[rs + "d"],
                  writes=[rs + "g"])
            S.add("dve", "tensor_tensor", pers["gate"][:, i, :], pers["gate"][:, i, :], SM[:, 156:160], ALU.mult,
                  reads=[rs + "g", "gate%d" % i], writes=["gate%d" % i])
            for k in range(4):
                S.add("pool", "indirect_dma_start", reads=["dest%d" % i, "mh1b%d" % t], writes=["XS_%d_%d" % (i, k)],
                      dma=True, key="xsc%d@sw" % t,
                      out=XS[:, :], out_offset=bass.IndirectOffsetOnAxis(ap=pers["dest"][:, i, k:k + 1], axis=0),
                      in_=HB[:], in_offset=None, bounds_check=bc_reg, oob_is_err=False)


def phaseX(nc, S, psum, ident, inp, XS, YS):
    NB = (E_CAP + 127) // 128
    ntl = [(0, min(512, E_CAP))] + ([(512, E_CAP)] if E_CAP > 512 else [])
    with ExitStack() as st:
        sb = lambda name, shape, dt: st.enter_context(nc.sbuf_tensor(name, list(shape), dt))
        w1 = [sb("px_w1%d" % i, [128, 8, 2048], BF16) for i in range(2)]
        w2 = [sb("px_w2%d" % i, [128, 8, D], BF16) for i in range(2)]
        xs = [sb("px_xs%d" % i, [128, NB, D], BF16) for i in range(2)]
        xT = [sb("px_xT%d" % i, [128, 8, E_CAP], BF16) for i in range(2)]
        aT = sb("px_aT", [128, 8, E_CAP], BF16)
        b1 = sb("px_b1", [128, NEXP, 16], F32)
        b2 = [sb("px_b2%d" % i, [128, D], F32) for i in range(2)]
        gs = [sb("px_g%d" % i, [128, 512], F32) for i in range(3)]
        sg = [sb("px_sg%d" % i, [128, 512], F32) for i in range(3)]
        us = [sb("px_u%d" % i, [128, 512], F32) for i in range(3)]
        ys = [sb("px_y%d" % i, [128, D], F32) for i in range(2)]
        S.dma("sp", b1[:].rearrange("p e c -> p (e c)"), inp["b_e1"][:, :], reads=[], writes=["b1"], key="initX", group=True)
        b1p = sb("px_b1p", [128, NEXP, 16], F32)
        S.add("dve", "tensor_scalar", b1p[:], b1[:], 7.0, None, ALU.add, reads=["b1"], writes=["b1p"])
        tpb = psum[0]
        tpv = tpb[:].bitcast(BF16)
        h_banks = [psum[1], psum[2], psum[3]]
        y_banks = [(psum[4], psum[5]), (psum[6], psum[7])]
        hc = 0
        yc = 0
        ec = 0

        pending = []

        def load(e, defer):
            b = e % 2
            w1v = inp["w_e1"][e].rearrange("(k p) c -> p k c", p=128)
            w2v = inp["w_e2"][e].rearrange("(k p) c -> p k c", p=128)
            lst = []
            for k in range(8):
                lst.append(lambda k=k: S.dma("pool", w1[b][:, k, :], w1v[:, k, :], reads=[], writes=["w1_%d_%d" % (b, k)],
                                             key="w1_%d" % b, group=True))
            for k in range(0, 8, 2):
                lst.append(lambda k=k: S.dma("pool", w2[b][:, k:k + 2, :], w2v[:, k:k + 2, :], reads=[],
                                             writes=["w2_%d_%d" % (b, k // 2)], key="w2_%d" % b, group=True))
            nfull = E_CAP // 128
            S.dma("sp", xs[b][:, 0:nfull, :], XS[e * E_CAP:e * E_CAP + nfull * 128, :].rearrange("(n p) d -> p n d", p=128),
                  reads=[], writes=["xs%d" % b])
            if E_CAP % 128:
                S.dma("sp", xs[b][0:E_CAP % 128, nfull, :], XS[e * E_CAP + nfull * 128:(e + 1) * E_CAP, :], reads=[],
                      writes=["xs%dr" % b])
            S.dma("sp", b2[b][:], inp["b_e2"][e:e + 1, :].partition_broadcast(128), reads=[], writes=["b2_%d" % b])
            if defer:
                pending.extend(lst)
            else:
                for f in lst:
                    f()

        load(0, False)
        for e in range(NEXP):
            b = e % 2
            if e + 1 < NEXP:
                load(e + 1, True)
            W1, W2, XSb, XT = w1[b], w2[b], xs[b], xT[b]
            rw1 = ["w1_%d_%d" % (b, k) for k in range(8)]
            rw2 = ["w2_%d_%d" % (b, k) for k in range(4)]

            def xpose(ee):
                bb = ee % 2
                for n in range(NB):
                    rows = min(128, E_CAP - n * 128)
                    for k in range(8):
                        S.add("pe", "transpose", tpv[:, k * 128:k * 128 + rows], xs[bb][0:rows, n, k * 128:(k + 1) * 128],
                              ident[0:rows, 0:rows], reads=["xs%d" % bb, "xs%dr" % bb, "ident"], writes=["xtp"])
                    S.add("act", "copy", xT[bb][:, :, n * 128:n * 128 + rows],
                          tpv.rearrange("p (k t) -> p k t", k=8)[:, :, 0:rows], reads=["xtp"], writes=["xT%d_%d" % (bb, n)])

            if e == 0:
                xpose(0)
            rxT = ["xT%d_%d" % (b, n) for n in range(NB)]
            for fc in range(8):
                for (n0, n1) in ntl:
                    N = n1 - n0
                    pg = h_banks[hc % 3]
                    rpg = "hps%d" % (hc % 3)
                    hc += 1
                    pu = h_banks[hc % 3]
                    rpu = "hps%d" % (hc % 3)
                    hc += 1
                    for kc in range(8):
                        S.add("pe", "matmul", pg[:, 0:N], W1[:, kc, fc * 128:(fc + 1) * 128], XT[:, kc, n0:n1],
                              start=(kc == 0), stop=(kc == 7), reads=rxT + [rw1[kc]], writes=[rpg])
                    for kc in range(8):
                        S.add("pe", "matmul", pu[:, 0:N], W1[:, kc, 1024 + fc * 128:1024 + (fc + 1) * 128], XT[:, kc, n0:n1],
                              start=(kc == 0), stop=(kc == 7), reads=rxT + [rw1[kc]], writes=[rpu])
                    q = ec % 3
                    ec += 1
                    Gs, Sg, Us = gs[q], sg[q], us[q]
                    S.add("dve", "tensor_scalar", Gs[:, 0:N], pg[:, 0:N], b1[:, e, fc:fc + 1], 7.0, ALU.add, ALU.min,
                          reads=[rpg, "b1"], writes=["xg%d" % q])
                    S.add("act", "activation", Us[:, 0:N], pu[:, 0:N], AF.Relu, bias=b1p[:, e, 8 + fc:9 + fc],
                          reads=[rpu, "b1p"], writes=["xu%d" % q])
                    S.add("act", "activation", Sg[:, 0:N], Gs[:, 0:N], AF.Gelu_apprx_sigmoid, reads=["xg%d" % q],
                          writes=["xsg%d" % q])
                    S.add("dve", "tensor_scalar", Us[:, 0:N], Us[:, 0:N], 14.0, -6.0, ALU.min, ALU.add,
                          reads=["xu%d" % q], writes=["xu%d" % q])
                    S.add("dve", "tensor_tensor", aT[:, fc, n0:n1], Sg[:, 0:N], Us[:, 0:N], ALU.mult,
                          reads=["xsg%d" % q, "xu%d" % q], writes=["aT%d_%d" % (fc, n0)])
                    if pending:
                        pending.pop(0)()
            while pending:
                pending.pop(0)()
            if e + 1 < NEXP:
                xpose(e + 1)
            raT = ["aT%d_%d" % (fc, n0) for fc in range(8) for (n0, _) in ntl]
            for n in range(NB):
                yb = y_banks[yc % 2]
                ry = "yps%d" % (yc % 2)
                Y = ys[yc % 2]
                rys = "ysb%d" % (yc % 2)
                yc += 1
                rows = min(128, E_CAP - n * 128)
                for half in range(2):
                    for kc in range(8):
                        S.add("pe", "matmul", yb[half][0:rows, :], aT[:, kc, n * 128:n * 128 + rows],
                              W2[:, kc, half * 512:(half + 1) * 512], start=(kc == 0), stop=(kc == 7),
                              reads=raT + [rw2[kc // 2]], writes=[ry + "_%d" % half])
                for half in range(2):
                    S.add("dve", "tensor_tensor", Y[0:rows, half * 512:(half + 1) * 512], yb[half][0:rows, :],
                          b2[b][0:rows, half * 512:(half + 1) * 512], ALU.add, reads=[ry + "_%d" % half, "b2_%d" % b],
                          writes=[rys])
                r0 = e * E_CAP + n * 128
                S.dma("sp", YS[r0:r0 + rows, :], Y[0:rows, :], reads=[rys], writes=["YS_%d_%d" % (e, n)], key=rys)
        S.flush()


def phaseF(nc, S, psum, inp, H1, YS, out, pers):
    with ExitStack() as st:
        sb = lambda name, shape, dt: st.enter_context(nc.sbuf_tensor(name, list(shape), dt))
        gb2 = sb("pf_gb2", [128, 2, D], F32)
        h1 = [sb("pf_h%d" % i, [128, D], F32) for i in range(2)]
        yk = [[sb("pf_y%d_%d" % (i, k), [128, D], F32) for k in range(4)] for i in range(2)]
        acc = [sb("pf_acc%d" % i, [128, D], F32) for i in range(2)]
        xn = sb("pf_xn", [128, D], F32)
        ot = [sb("pf_o%d" % i, [128, D], F32) for i in range(2)]
        st6 = sb("pf_st6", [128, 2, 6], F32)
        mv = sb("pf_mv", [128, 8], F32)
        ln_consts(S, mv, "pfs")
        bc_reg = nc.gpsimd.alloc_register("ys_bc")
        nc.gpsimd.reg_mov(bc_reg, NEXP * E_CAP - 1)
        S.dma("sp", gb2[:, 0, :], inp["ln2"][0:1, :].partition_broadcast(128), reads=[], writes=["gb20"], key="initF", group=True)
        S.dma("sp", gb2[:, 1, :], inp["ln2"][1:2, :].partition_broadcast(128), reads=[], writes=["gb21"], key="initF", group=True)
        def fetch(i):
            b = i % 2
            S.dma("sp", h1[b][:], H1[i * 128:(i + 1) * 128, :], reads=[], writes=["fh%d" % b])
            for k in range(4):
                S.add("pool", "indirect_dma_start", reads=[], writes=["fy%d_%d" % (b, k)], dma=True, key="fy%d_%d@sw" % (b, k),
                      out=yk[b][k][:], out_offset=None, in_=YS[:, :],
                      in_offset=bass.IndirectOffsetOnAxis(ap=pers["dest"][:, i, k:k + 1], axis=0),
                      bounds_check=bc_reg, oob_is_err=False)

        for b_ in range(2):
            for k in range(4):
                S.add("dve", "memset", yk[b_][k][:], 0.0, reads=[], writes=["fy%d_%d" % (b_, k)])
        fetch(0)
        for i in range(NOWN):
            b = i % 2
            if i + 1 < NOWN:
                fetch(i + 1)
            A = acc[b]
            ra = "facc%d" % b
            S.add("act", "activation", A[:], yk[b][0][:], AF.Copy, scale=pers["gate"][:, i, 0:1],
                  reads=["fy%d_0" % b], writes=[ra])
            S.add("dve", "scalar_tensor_tensor", A[:], h1[b][:], ALPHA, A[:], ALU.mult, ALU.add,
                  reads=[ra, "fh%d" % b], writes=[ra])
            for k in range(1, 4):
                S.add("dve", "scalar_tensor_tensor", A[:], yk[b][k][:], pers["gate"][:, i, k:k + 1], A[:], ALU.mult, ALU.add,
                      reads=["fy%d_%d" % (b, k), ra], writes=[ra])
            layer_norm_tile(S, A, ra, xn, "fxn", ot[b], "fo%d" % b, st6, mv, "pfs", gb2, ["gb20", "gb21"])
            S.dma("sp", out[i * 128:(i + 1) * 128, :], ot[b][:], reads=["fo%d" % b], writes=["out%d" % i], key="ost%d" % b)
        S.flush()


def kernel(**inputs):
    nc, S = build_program()
    in_maps = [_core_inputs(c, inputs) for c in range(8)]
    res = run_bass_kernel_spmd(nc, in_maps, core_ids=list(range(8)))
    outp = np.zeros((4, 64, 128, D), np.float32)
    for c in range(8):
        b, j = divmod(c, 2)
        outp[b, j::2] = np.asarray(res.results[c]["out"]).reshape(NOWN, 128, D)
    return outp.reshape(4, 8192, D)
```

```python
import numpy as np
from contextlib import ExitStack
import concourse.bass as bass
import concourse.mybir as mybir
from concourse.bass_utils import run_bass_kernel_spmd

F32 = mybir.dt.float32
BF16 = mybir.dt.bfloat16
I32 = mybir.dt.int32
U32 = mybir.dt.uint32
ALU = mybir.AluOpType
AF = mybir.ActivationFunctionType
AX = mybir.AxisListType

D = 1024
NBLK = 64
NOWN = 32
NTOK = NOWN * 128
ALPHA = 2.0 ** 0.25
EPS = 1e-5
NEG = -30000.0
LAM_INIT = 0.8 - 0.6
SLOPES = [2.0 ** (-8.0 * (i + 1) / 4) for i in range(4)]
A_WINDOW = [3, 9, 33, 10 ** 6]
E_CAP = 704
NEXP = 32

SAME_ENG_SYNC = True
PA_DBG = {}


class Op:
    __slots__ = ("eng", "name", "args", "kwargs", "reads", "writes", "dma", "key", "deps", "token")


class Sched:
    def __init__(self, nc, stack):
        self.nc = nc
        self.eng = {"pe": nc.tensor, "act": nc.scalar, "dve": nc.vector, "pool": nc.gpsimd, "sp": nc.sync}
        self.esem = {e: stack.enter_context(nc.semaphore("es_" + e)) for e in self.eng}
        self.ecount = {e: 0 for e in self.eng}
        self.dsem = {}
        self.scount = {}
        self.sem_pool = []
        self.all_sems = []
        self.sem_kind = {}
        self.stack = stack
        self.waited = {e: {} for e in self.eng}
        self.ops = []
        self.n_emitted = 0
        self.group_keys = set()

    def add(self, eng, name, *args, reads=(), writes=(), dma=False, key=None, **kwargs):
        op = Op()
        op.eng, op.name, op.args, op.kwargs = eng, name, args, kwargs
        op.reads, op.writes, op.dma, op.key = tuple(reads), tuple(writes), dma, key
        op.token = None
        self.ops.append(op)
        return op

    def dma(self, eng, out, in_, reads, writes, key=None, group=False, **kw):
        if key is None:
            key = writes[0]
        key = key + ("@sw" if eng == "pool" else "@hw")
        if group:
            self.group_keys.add(key)
        return self.add(eng, "dma_start", reads=reads, writes=writes, dma=True, key=key, out=out, in_=in_, **kw)

    def _dsem(self, key):
        if key not in self.dsem:
            kind = key[-3:]
            cand = [x for x in self.sem_pool if self.sem_kind[x] == kind]
            if cand:
                sem = cand[-1]
                self.sem_pool.remove(sem)
            else:
                sem = self.stack.enter_context(self.nc.semaphore("ds_%d" % len(self.all_sems)))
                self.all_sems.append(sem)
                self.scount[sem] = 0
                self.sem_kind[sem] = key[-3:]
            self.dsem[key] = sem
        return self.dsem[key]

    def flush(self):
        ops = self.ops
        self.ops = []
        n = len(ops)
        last_w = {}
        readers = {}
        for i, op in enumerate(ops):
            deps = set()
            for r in op.reads:
                if r in last_w:
                    deps.add(last_w[r])
            for w in op.writes:
                if w in last_w:
                    deps.add(last_w[w])
                rd = readers.get(w)
                if rd:
                    deps.update(rd.values())
            deps.discard(i)
            kept = []
            for j in deps:
                pj = ops[j]
                if (not pj.dma) and (not op.dma) and pj.eng == op.eng:
                    if op.eng == "pe" or not SAME_ENG_SYNC:
                        continue
                kept.append(j)
            op.deps = sorted(kept)
            for r in op.reads:
                d = readers.setdefault(r, {})
                d[("d%d" % i) if op.dma else op.eng] = i
            for w in op.writes:
                last_w[w] = i
                readers[w] = {}
        needs = [False] * n
        for op in ops:
            for j in op.deps:
                needs[j] = True
        last_on = {}
        for i, op in enumerate(ops):
            if not op.dma:
                last_on[op.eng] = i
        for i in last_on.values():
            needs[i] = True
        for i, op in enumerate(ops):
            E = op.eng
            e = self.eng[E]
            wt = self.waited[E]
            for j in op.deps:
                sem, val = ops[j].token
                if ops[j].dma and ops[j].key in self.group_keys:
                    val = self.scount[sem]
                if wt.get(sem, 0) < val:
                    e.wait_ge(sem, val)
                    wt[sem] = val
            try:
                ins = getattr(e, op.name)(*op.args, **op.kwargs)
            except Exception:
                print("EMIT FAIL", E, op.name, op.writes, [str(a)[:120] for a in op.args], {k: str(v)[:120] for k, v in op.kwargs.items()})
                raise
            if op.dma:
                sem = self._dsem(op.key)
                self.scount[sem] += 16
                ins.then_inc(sem, 16)
                op.token = (sem, self.scount[sem])
            elif needs[i]:
                self.ecount[E] += 1
                ins.then_inc(self.esem[E], 1)
                op.token = (self.esem[E], self.ecount[E])
        self.n_emitted += n
        for E, e in self.eng.items():
            wt = self.waited[E]
            for F in self.eng:
                if F != E and self.ecount[F] > wt.get(self.esem[F], 0):
                    e.wait_ge(self.esem[F], self.ecount[F])
                    wt[self.esem[F]] = self.ecount[F]
            for sem in self.all_sems:
                if self.scount[sem] > wt.get(sem, 0):
                    e.wait_ge(sem, self.scount[sem])
                    wt[sem] = self.scount[sem]
        self.sem_pool.extend(self.dsem.values())
        self.dsem = {}


def _dram(nc, name, shape, dt, dbg):
    return nc.dram_tensor(name, list(shape), dt, kind=("ExternalOutput" if dbg else "Internal")).ap()


def build_program(stop_after=None, dbg=()):
    nc = bass.Bass("TRN2", target_bir_lowering=False)
    inp = {}

    def din(name, shape, dt=F32):
        inp[name] = nc.dram_tensor(name, list(shape), dt, kind="ExternalInput").ap()
        return inp[name]

    xall = din("xall", [NBLK * 128, D])
    lnin = din("lnin", [2, D])
    w_in = din("w_in", [D, 5120])
    ident_in = din("ident", [128, 128])
    din("abias", [128, 2 * 4 * 64])
    din("adiag", [128, 4 * 2 * 128])
    din("lamv", [4, 64])
    din("subln", [1, 128])
    din("btab", [128, 8 * 5 * 128])
    din("parmask", [128, 1])
    out = nc.dram_tensor("out", [NTOK, D], F32, kind="ExternalOutput").ap()
    din("b_gate", [128, 16])
    din("w_ba", [512, D])
    din("w_bb", [512, D])
    din("w_out", [D, D])
    din("ln1", [2, D])
    din("w_router", [D, NEXP])
    din("b_router", [1, NEXP])
    din("rconst", [128, 128 + 128 + 32 + 32])
    H1 = _dram(nc, "H1", [NTOK, D], F32, "H1" in dbg)
    YS = _dram(nc, "YS", [NEXP * E_CAP, D], F32, "YS" in dbg)
    din("w_e1", [NEXP, D, 2048])
    din("w_e2", [NEXP, D, D])
    din("b_e1", [128, NEXP * 16])
    din("b_e2", [NEXP, D])
    din("ln2", [2, D])
    XS = _dram(nc, "XS", [NEXP * E_CAP, D], BF16, "XS" in dbg)
    OA = _dram(nc, "OA", [NTOK, 512], BF16, "OA" in dbg)
    OB = _dram(nc, "OB", [NTOK, 512], BF16, "OB" in dbg)

    KAT = _dram(nc, "KAT", [4, 128, NBLK * 128], BF16, "KAT" in dbg)
    KBT = _dram(nc, "KBT", [4, 128, NBLK * 128], BF16, "KBT" in dbg)
    QAT = _dram(nc, "QAT", [4, 128, NTOK], BF16, "QAT" in dbg)
    QBT = _dram(nc, "QBT", [4, 128, NTOK], BF16, "QBT" in dbg)
    VA = _dram(nc, "VA", [NBLK, 128, 4 * 129], BF16, "VA" in dbg)
    VB = _dram(nc, "VB", [NBLK, 128, 8 * 65], BF16, "VB" in dbg)

    with ExitStack() as stack:
        S = Sched(nc, stack)
        psum = [stack.enter_context(nc.psum_tensor("ps%d" % i, [128, 512], F32)) for i in range(8)]
        ident = stack.enter_context(nc.sbuf_tensor("identb", [128, 128], BF16))
        S.dma("pool", ident[:], ident_in[:, :], reads=[], writes=["ident"], key="init", group=True)

        if "skip0" not in dbg:
            phase0(nc, S, psum, ident, xall, lnin, w_in, KAT, KBT, QAT, QBT, VA, VB)
        if stop_after == 0:
            return nc, S
        if "skipA" not in dbg:
            phaseA(nc, S, psum, inp, KAT, QAT, VA, OA)
        if stop_after == 1:
            return nc, S
        if "skipB" not in dbg:
            phaseB(nc, S, psum, inp, KBT, QBT, VB, OB)
        if stop_after == 2:
            return nc, S
        pers = {
            "dest": stack.enter_context(nc.sbuf_tensor("dest_i", [128, NOWN, 4], I32)),
            "gate": stack.enter_context(nc.sbuf_tensor("gates4", [128, NOWN, 4], F32)),
        }
        if "DESTD" in dbg:
            pers["DESTD"] = _dram(nc, "DESTD", [128, NOWN * 4], I32, True)
            pers["GATED"] = _dram(nc, "GATED", [128, NOWN * 4], F32, True)
        if "skipM" not in dbg:
            phaseM(nc, S, psum, ident, inp, xall, lnin, w_in, OA, OB, H1, XS, pers)
        if stop_after == 3:
            return nc, S
        if "skipX" not in dbg:
            phaseX(nc, S, psum, ident, inp, XS, YS)
        if stop_after == 4:
            return nc, S
        phaseF(nc, S, psum, inp, H1, YS, out, pers)
    return nc, S


def phase0(nc, S, psum, ident, xall, lnin, w_in, KAT, KBT, QAT, QBT, VA, VB):
    with ExitStack() as st:
        sb = lambda name, shape, dt: st.enter_context(nc.sbuf_tensor(name, list(shape), dt))
        gb = sb("p0_gb", [128, 2, D], F32)
        wkv = sb("p0_w", [128, 8, 3072], BF16)
        xt = [sb("p0_x%d" % i, [128, D], F32) for i in range(2)]
        xn = [sb("p0_xn%d" % i, [128, D], F32) for i in range(2)]
        hb = [sb("p0_hb%d" % i, [128, D], BF16) for i in range(2)]
        hT = [sb("p0_hT%d" % i, [128, 8, 512], BF16) for i in range(2)]
        st6 = [sb("p0_st%d" % i, [128, 2, 6], F32) for i in range(2)]
        mv = [sb("p0_mv%d" % i, [128, 8], F32) for i in range(2)]
        for i in range(2):
            ln_consts(S, mv[i], "p0s%d" % i)
        kq = [sb("p0_kq%d" % i, [128, 16, 512], BF16) for i in range(2)]
        vas = [sb("p0_va%d" % i, [128, 4, 129], BF16) for i in range(2)]
        vbs = [sb("p0_vb%d" % i, [128, 8, 65], BF16) for i in range(2)]

        S.dma("sp", gb[:, 0, :], lnin[0:1, :].partition_broadcast(128), reads=[], writes=["gb0"], key="init", group=True)
        S.dma("sp", gb[:, 1, :], lnin[1:2, :].partition_broadcast(128), reads=[], writes=["gb1"], key="init", group=True)
        wv = w_in[:, 0:3072].rearrange("(k p) c -> p k c", p=128)
        for k in range(8):
            S.dma("pool", wkv[:, k, :], wv[:, k, :], reads=[], writes=["wkv%d" % k], key="init", group=True)
        for i in range(2):
            S.add("pool", "memset", vas[i][:, :, 128:129], 1.0, reads=[], writes=["vas1_%d" % i])
            S.add("pool", "memset", vbs[i][:, :, 64:65], 1.0, reads=[], writes=["vbs1_%d" % i])
        wk_all = ["wkv%d" % k for k in range(8)]

        tp_banks = [psum[0], psum[1]]
        kq_banks = [psum[2], psum[3], psum[4]]
        v_banks = [psum[5], psum[6], psum[7]]
        vcnt = 0
        kcnt = 0
        def stage_a(kx):
            b2 = kx % 2
            X, XN, HB = xt[b2], xn[b2], hb[b2]
            rx, rxn, rhb = "x%d" % b2, "xn%d" % b2, "hb%d" % b2
            S.dma("sp", X[:], xall[kx * 128:(kx + 1) * 128, :], reads=[], writes=[rx])
            layer_norm_tile(S, X, rx, XN, rxn, HB, rhb, st6[b2], mv[b2], "p0s%d" % b2, gb, ["gb0", "gb1"])

        stage_a(0)
        for kx in range(NBLK):
            b2 = kx % 2
            u, blk = divmod(kx, 4)
            g2 = u % 2
            X, XN, HB = xt[b2], xn[b2], hb[b2]
            rx, rxn, rhb = "x%d" % b2, "xn%d" % b2, "hb%d" % b2
            if kx + 1 < NBLK:
                stage_a(kx + 1)
            tpb = tp_banks[b2]
            tpv = tpb[:].bitcast(BF16)
            rtp = "tp%d" % b2
            for k in range(8):
                S.add("pe", "transpose", tpv[:, k * 128:(k + 1) * 128], HB[:, k * 128:(k + 1) * 128], ident[:],
                      reads=[rhb, "ident"], writes=[rtp])
            rhT = "hT%d_%d" % (g2, blk)
            S.add("act", "copy", hT[g2][:, :, blk * 128:(blk + 1) * 128],
                  tpv.rearrange("p (k t) -> p k t", k=8), reads=[rtp], writes=[rhT])
            for which, (c0, stg, rs, dst, nh, dv) in enumerate(
                    [(1024, vas[b2], "vas_%d" % b2, VA, 4, 128), (2560, vbs[b2], "vbs_%d" % b2, VB, 8, 64)]):
                vb = v_banks[vcnt % 3]
                rv = "vps%d" % (vcnt % 3)
                vcnt += 1
                for kc in range(8):
                    S.add("pe", "matmul", vb[:], hT[g2][:, kc, blk * 128:(blk + 1) * 128], wkv[:, kc, c0:c0 + 512],
                          start=(kc == 0), stop=(kc == 7), reads=[rhT, "wkv%d" % kc], writes=[rv])
                eng = "act" if which == 0 else "dve"
                if eng == "act":
                    S.add("act", "copy", stg[:, :, 0:dv], vb[:].rearrange("p (h d) -> p h d", h=nh),
                          reads=[rv], writes=[rs])
                else:
                    S.add("dve", "tensor_copy", stg[:, :, 0:dv], vb[:].rearrange("p (h d) -> p h d", h=nh),
                          reads=[rv], writes=[rs])
                S.dma("sp", dst[kx], stg[:].rearrange("p h d -> p (h d)"),
                      reads=[rs, ("vas1_%d" if which == 0 else "vbs1_%d") % b2], writes=["dram_v%d_%d" % (which, kx)],
                      key="vst%d_%d" % (which, b2))
            if blk == 3:
                KQ = kq[g2]
                rgrp = ["hT%d_%d" % (g2, t) for t in range(4)]
                for ci in range(16):
                    kind, m = divmod(ci, 4)
                    c0 = [512, 2048, 0, 1536][kind] + 128 * m
                    pb = kq_banks[kcnt % 3]
                    rp = "kqps%d" % (kcnt % 3)
                    kcnt += 1
                    isq = kind >= 2
                    N = 256 if isq else 512
                    for kc in range(8):
                        if isq:
                            rhs = hT[g2][:, kc, :].rearrange("p (a b t) -> p a b t", a=2, b=2)[:, :, 1, :]
                            o = pb[:, 0:256].rearrange("p (a t) -> p a t", a=2)
                        else:
                            rhs = hT[g2][:, kc, :]
                            o = pb[:, :]
                        S.add("pe", "matmul", o, wkv[:, kc, c0:c0 + 128], rhs, start=(kc == 0), stop=(kc == 7),
                              reads=rgrp + ["wkv%d" % kc], writes=[rp])
                    rk = "kq%d_%d" % (g2, ci)
                    if isq:
                        S.add("act", "activation", KQ[:, ci, 0:N], pb[:, 0:N], AF.Copy, scale=0.125,
                              reads=[rp], writes=[rk])
                    else:
                        S.add("dve", "tensor_copy", KQ[:, ci, 0:N], pb[:, 0:N], reads=[rp], writes=[rk])
                    dst = [KAT, KBT, QAT, QBT][kind]
                    S.dma("sp" if ci % 2 == 0 else "pool", dst[m][:, u * N:(u + 1) * N], KQ[:, ci, 0:N], reads=[rk],
                          writes=["dram_kq%d_%d_%d" % (kind, m, u)], key="kqst%d_%d" % (g2, ci))
        S.flush()


def layer_norm_tile(S, X, rx, XN, rxn, OUT, rout, st6, mv, rs, gb, rgb):
    for c in range(2):
        S.add("dve", "bn_stats", st6[:, c, :], X[:, c * 512:(c + 1) * 512], reads=[rx], writes=[rs + "a"])
    S.add("dve", "bn_aggr", mv[:, 0:2], st6[:].rearrange("p a b -> p (a b)"), reads=[rs + "a"], writes=[rs + "b"])
    S.add("dve", "scalar_tensor_tensor", XN[:], X[:], mv[:, 0:1], gb[:, 0, :], ALU.subtract, ALU.mult,
          reads=[rx, rs + "b", rgb[0]], writes=[rxn])
    S.add("pool", "tensor_scalar", mv[:, 2:3], mv[:, 1:2], EPS, None, ALU.add, reads=[rs + "b"], writes=[rs + "c"])
    S.add("pool", "tensor_tensor", mv[:, 3:4], mv[:, 2:3], mv[:, 4:5], ALU.pow, reads=[rs + "c", rs + "k"], writes=[rs + "d"])
    S.add("dve", "scalar_tensor_tensor", OUT[:], XN[:], mv[:, 3:4], gb[:, 1, :], ALU.mult, ALU.add,
          reads=[rxn, rs + "d", rgb[1]], writes=[rout])


def ln_consts(S, mv, rs):
    S.add("pool", "memset", mv[:, 4:5], -0.5, reads=[], writes=[rs + "k"])

def phaseA(nc, S, psum, inp, KAT, QAT, VA, OA):
    with ExitStack() as st:
        sb = lambda name, shape, dt: st.enter_context(nc.sbuf_tensor(name, list(shape), dt))
        vall = sb("pa_v", [128, NBLK, 4 * 129], BF16)
        kt = [sb("pa_k%d" % i, [128, NBLK * 128], BF16) for i in range(2)]
        qt = [sb("pa_q%d" % i, [128, 2, NTOK], BF16) for i in range(2)]
        oa = sb("pa_oa", [128, NOWN, 512], BF16)
        abias = sb("pa_ab", [128, 2, 4, 64], F32)
        adiag = sb("pa_ad", [128, 4, 256], F32)
        lamt = sb("pa_lam", [128, 4, 64], F32)
        lams = sb("pa_lams", [128, 8], F32)
        subw = sb("pa_sw", [128, 128], F32)
        NPT = 4
        pt = [sb("pa_pt%d" % i, [128, 512], BF16) for i in range(NPT)]
        dtmp = [sb("pa_dt%d" % i, [128, 512], F32) for i in range(2)]
        fin = [sb("pa_fin%d" % i, [128, 8], F32) for i in range(2)]
        t0 = [sb("pa_t0%d" % i, [128, 128], F32) for i in range(2)]
        av = [sb("pa_a%d" % i, [128, 128], F32) for i in range(2)]
        junk = sb("pa_junk", [128, 128], F32)
        for f_ in fin:
            S.add("pool", "memset", f_[:, 6:7], -0.5, reads=[], writes=["fink"])
        zt = sb("pa_zt", [1, 512], BF16)
        S.add("pool", "memset", zt[:], 0.0, reads=[], writes=["zt"])

        for i in range(2):
            S.add("pool", "memset", qt[i][0:64, 1, :], 0.0, reads=[], writes=["qtz%d" % i])
            S.add("pool", "memset", qt[i][64:128, 0, :], 0.0, reads=[], writes=["qtz%d" % i])
        for c in range(4):
            S.dma("sp" if c % 2 == 0 else "pool", vall[:, c * 16:(c + 1) * 16, :],
                  VA[c * 16:(c + 1) * 16].rearrange("k p d -> p k d"), reads=[], writes=["vall%d" % c],
                  key="initA", group=True)
        S.dma("sp", abias[:].rearrange("p a b c -> p (a b c)"), inp["abias"][:, :], reads=[], writes=["abias"],
              key="initA", group=True)
        S.dma("sp", adiag[:].rearrange("p a b -> p (a b)"), inp["adiag"][:, :], reads=[], writes=["adiag"],
              key="initA", group=True)
        for v in range(4):
            S.dma("sp", lamt[:, v, :], inp["lamv"][v:v + 1, :].partition_broadcast(128), reads=[], writes=["lamt"],
                  key="initA", group=True)
        S.dma("sp", subw[:], inp["subln"][0:1, :].partition_broadcast(128), reads=[], writes=["subw"],
              key="initA", group=True)
        for v in range(2):
            S.add("dve", "tensor_tensor", lamt[:, 2 * v, :], lamt[:, 2 * v, :], lamt[:, 2 * v + 1, :], ALU.mult,
                  reads=["lamt"], writes=["lamt"])
            S.add("dve", "reduce_sum", lams[:, v:v + 1], lamt[:, 2 * v, :], AX.X, reads=["lamt"], writes=["lams"])
        S.add("act", "activation", lams[:, 2:4], lams[:, 0:2], AF.Exp, reads=["lams"], writes=["lams"])
        S.add("dve", "tensor_tensor", lams[:, 4:5], lams[:, 3:4], lams[:, 2:3], ALU.subtract,
              reads=["lams"], writes=["lams"])
        S.add("dve", "tensor_scalar", lams[:, 4:5], lams[:, 4:5], -LAM_INIT, None, ALU.add,
              reads=["lams"], writes=["lams"])
        S.add("dve", "tensor_scalar", subw[:], subw[:], 1.0 - LAM_INIT, None, ALU.mult, reads=["subw"], writes=["subw"])

        s_banks = [psum[0], psum[1], psum[2], psum[3]]
        o_banks = [psum[4], psum[5], psum[6], psum[7]]
        LOOK = 2
        steps = []
        for h in range(PA_DBG.get("heads", 4)):
            for ia in range(0, PA_DBG.get("qblocks", NOWN), 2):
                rmax = [min(A_WINDOW[h], 2 * (ia + s_) + 1) for s_ in range(2)]
                for rel in range(max(rmax), -1, -1):
                    slots = []
                    for s_ in range(2):
                        i = ia + s_
                        if rel <= rmax[s_]:
                            slots.append((s_, i, 2 * i + 1 - rel, rel == rmax[s_]))
                    steps.append((h, rel, slots))
        loaded = set()
        deferred = []
        pcnt = {}

        def head_load(h):
            KT, QT = kt[h % 2], qt[h % 2]
            rk, rq = "kt%d" % (h % 2), "qt%d" % (h % 2)
            for c in range(4):
                S.dma("sp" if c % 2 == 0 else "pool", KT[:, c * 2048:(c + 1) * 2048], KAT[h][:, c * 2048:(c + 1) * 2048],
                      reads=[], writes=[rk + "_%d" % c], key=rk, group=True)
            S.dma("sp", QT[0:64, 0, :], QAT[h][0:64, :], reads=[], writes=[rq + "a"])
            S.dma("pool", QT[64:128, 1, :], QAT[h][64:128, :], reads=[], writes=[rq + "b"])

        def emit_s(sidx):
            h, rel, slots = steps[sidx]
            if h not in loaded:
                loaded.add(h)
                head_load(h)
            KT, QT = kt[h % 2], qt[h % 2]
            rk, rq = "kt%d" % (h % 2), "qt%d" % (h % 2)
            sbk = s_banks[sidx % 4]
            rs = "sps%d" % (sidx % 4)
            P = pt[sidx % NPT]
            rp = "pt%d" % (sidx % NPT)
            for (s_, i, kx, first) in slots:
                for c in range(2):
                    col = s_ * 256 + c * 128
                    S.add("pe", "matmul", sbk[:, col:col + 128], KT[:, kx * 128:(kx + 1) * 128],
                          QT[:, c, i * 128:(i + 1) * 128], start=True, stop=True,
                          reads=[rk + "_%d" % (kx // 16), rq + "a", rq + "b", "qtz%d" % (h % 2)], writes=[rs])
            lo = slots[0][0] * 256
            hi = slots[-1][0] * 256 + 256
            if rel == 0:
                DT = dtmp[sidx % 2]
                rd = "dtmp%d" % (sidx % 2)
                n2 = (hi - lo) // 256
                S.add("dve", "tensor_tensor", DT[:, lo:hi].rearrange("p (a b) -> p a b", a=n2),
                      sbk[:, lo:hi].rearrange("p (a b) -> p a b", a=n2),
                      adiag[:, h, :].unsqueeze(1).to_broadcast([128, n2, 256]), ALU.add, reads=[rs, "adiag"], writes=[rd])
                S.add("act", "activation", P[:, lo:hi], DT[:, lo:hi], AF.Exp, reads=[rd], writes=[rp])
            elif any(kx == 0 for (_, _, kx, _) in slots):
                for (s_, i, kx, first) in slots:
                    z = 1 if kx == 0 else 0
                    S.add("act", "activation", P[:, s_ * 256:s_ * 256 + 256], sbk[:, s_ * 256:s_ * 256 + 256], AF.Exp,
                          bias=abias[:, z, h, rel:rel + 1], reads=[rs, "abias"], writes=[rp])
            else:
                S.add("act", "activation", P[:, lo:hi], sbk[:, lo:hi], AF.Exp, bias=abias[:, 0, h, rel:rel + 1],
                      reads=[rs, "abias"], writes=[rp])

        def emit_av(sidx):
            h, rel, slots = steps[sidx]
            P = pt[sidx % NPT]
            rp = "pt%d" % (sidx % NPT)
            for (s_, i, kx, first) in slots:
                key = (h, i)
                if key not in pcnt:
                    pcnt[key] = len(pcnt)
                qn = pcnt[key]
                ob = o_banks[qn % 4]
                ro = "ops%d" % (qn % 4)
                if first:
                    S.add("pe", "matmul", ob[:, 0:258], zt[0:1, 0:128], zt[0:1, 0:258], start=True, stop=False,
                          reads=["zt"], writes=[ro])
                for c in range(2):
                    col = s_ * 256 + c * 128
                    S.add("pe", "matmul", ob[:, c * 129:(c + 1) * 129], P[:, col:col + 128],
                          vall[:, kx, h * 129:(h + 1) * 129], start=False, stop=(rel == 0 and c == 1),
                          reads=[rp, "vall%d" % (kx // 16)], writes=[ro])
                if rel == 0:
                    deferred.append((sidx + 3, h, i, qn))

        def emit_fin(h, i, qn):
            ob = o_banks[qn % 4]
            ro = "ops%d" % (qn % 4)
            f2 = qn % 2
            F, T0, A = fin[f2], t0[f2], av[f2]
            rf = "fin%d" % f2
            S.add("dve", "reciprocal", F[:, 0:2], ob[:, 0:258].rearrange("p (c d) -> p c d", c=2)[:, :, 128],
                  reads=[ro], writes=[rf])
            S.add("dve", "tensor_tensor", F[:, 2:3], F[:, 1:2], lams[:, 4:5], ALU.mult, reads=[rf, "lams"], writes=[rf])
            S.add("dve", "tensor_scalar", T0[:], ob[:, 0:128], F[:, 0:1], None, ALU.mult, reads=[ro, rf], writes=[rf + "t"])
            S.add("dve", "scalar_tensor_tensor", A[:], ob[:, 129:257], F[:, 2:3], T0[:], ALU.mult, ALU.add,
                  reads=[ro, rf, rf + "t"], writes=[rf + "a"])
            S.add("dve", "scalar_tensor_tensor", junk[:], A[:], 1.0, A[:], ALU.mult, ALU.mult, accum_out=F[:, 3:4],
                  reads=[rf + "a"], writes=["junk", rf + "s"])
            S.add("dve", "tensor_scalar", F[:, 3:4], F[:, 3:4], 1.0 / 128, EPS, ALU.mult, ALU.add,
                  reads=[rf + "s"], writes=[rf + "s"])
            S.add("pool", "tensor_tensor", F[:, 5:6], F[:, 3:4], F[:, 6:7], ALU.pow, reads=[rf + "s", "fink"], writes=[rf + "r"])
            S.add("dve", "scalar_tensor_tensor", oa[:, i, h * 128:(h + 1) * 128], A[:], F[:, 5:6], subw[:],
                  ALU.mult, ALU.mult, reads=[rf + "a", rf + "r", "subw"], writes=["oa%d" % i])

        ns = len(steps)
        for sidx in range(ns + LOOK + 4):
            if sidx < ns:
                emit_s(sidx)
            if 0 <= sidx - LOOK < ns:
                emit_av(sidx - LOOK)
            while deferred and deferred[0][0] <= sidx:
                _, h_, i_, qn_ = deferred.pop(0)
                if not PA_DBG.get("nofin"):
                    emit_fin(h_, i_, qn_)
        assert not deferred
        for c in range(4):
            S.dma("sp" if c % 2 == 0 else "pool", OA[c * 1024:(c + 1) * 1024, :].rearrange("(i p) d -> p i d", p=128),
                  oa[:, c * 8:(c + 1) * 8, :], reads=["oa%d" % i for i in range(c * 8, c * 8 + 8)],
                  writes=["OA%d" % c], key="oast")
        S.flush()


def _const_tables(j, rel_bias):
    bk = np.arange(128, dtype=np.float32)
    parmask = np.full((128, 1), 0.0 if j == 1 else NEG, np.float32)
    abias = np.zeros((128, 2, 4, 64), np.float32)
    adiag = np.zeros((128, 4, 2, 128), np.float32)
    for h in range(4):
        sl = np.float32(SLOPES[h])
        for rel in range(64):
            abias[:, 0, h, rel] = sl * (bk - 128.0 * rel)
            abias[:, 1, h, rel] = sl * (bk - 128.0 * rel) + parmask[0, 0]
        bq = bk[None, :]
        bkk = bk[:, None]
        t = -sl * np.abs(bq - bkk) + sl * bq
        t = np.where((bkk // 64) > (bq // 64), NEG, t)
        adiag[:, h, 0, :] = t
        adiag[:, h, 1, :] = t
    kk = np.arange(128)[:, None, None]
    tb = np.arange(5)[None, :, None]
    q = np.arange(128)[None, None, :]
    rel = (tb - 4) * 128 + kk - q
    kch = 2 * (tb - 4) + kk // 64
    qch = q // 64
    valid = (kch - qch >= -8) & (kch - qch <= 0)
    idx = np.clip(rel, -128, 128) + 128
    btab = np.zeros((128, 8, 5, 128), np.float32)
    for hb in range(8):
        btab[:, hb] = np.where(valid, rel_bias[hb][idx], NEG)
    return parmask, abias.reshape(128, -1), adiag.reshape(128, -1), btab.reshape(128, -1)


def _core_inputs(core, inputs):
    b, j = divmod(core, 2)
    xb = np.asarray(inputs["x"][b], np.float32)
    if j == 0:
        xall = np.concatenate([np.zeros((128, D), np.float32), xb[:63 * 128]], 0)
    else:
        xall = xb
    parmask, abias, adiag, btab = _const_tables(j, np.asarray(inputs["rel_bias"][0], np.float32))
    m = {
        "xall": np.ascontiguousarray(xall),
        "lnin": np.stack([inputs["ln_in_g"], inputs["ln_in_b"]]).astype(np.float32),
        "w_in": np.ascontiguousarray(inputs["w_in"][0]),
        "ident": np.eye(128, dtype=np.float32),
        "abias": abias, "adiag": adiag, "btab": btab, "parmask": parmask,
        "lamv": np.stack([inputs["lambda_q1"][0], inputs["lambda_k1"][0], inputs["lambda_q2"][0],
                          inputs["lambda_k2"][0]]).astype(np.float32),
        "subln": np.asarray(inputs["subln_w"], np.float32).reshape(1, 128),
        "b_gate": np.ascontiguousarray(np.asarray(inputs["b_gate"][0], np.float32).reshape(16, 128).T),
        "w_ba": np.ascontiguousarray(inputs["w_branch_a"][0]), "w_bb": np.ascontiguousarray(inputs["w_branch_b"][0]),
        "w_out": np.ascontiguousarray(inputs["w_out"][0]),
        "ln1": np.stack([inputs["ln1_g"][0], inputs["ln1_b"][0]]).astype(np.float32),
        "w_router": np.ascontiguousarray(inputs["w_router"][0]),
        "b_router": np.asarray(inputs["b_router"][0], np.float32).reshape(1, NEXP),
        "rconst": _rconst(),
        "w_e1": np.asarray(inputs["w_exp_in"][0]), "w_e2": np.asarray(inputs["w_exp_out"][0]),
        "b_e1": np.ascontiguousarray(np.asarray(inputs["b_exp_in"][0], np.float32).reshape(NEXP, 16, 128)
                                     .transpose(2, 0, 1).reshape(128, NEXP * 16)),
        "b_e2": np.asarray(inputs["b_exp_out"][0], np.float32),
        "ln2": np.stack([inputs["ln2_g"][0], inputs["ln2_b"][0]]).astype(np.float32),
    }
    return m


def phaseB(nc, S, psum, inp, KBT, QBT, VB, OB):
    with ExitStack() as st:
        sb = lambda name, shape, dt: st.enter_context(nc.sbuf_tensor(name, list(shape), dt))
        vall = sb("pb_v", [128, NBLK, 8 * 65], BF16)
        kt = sb("pb_k", [128, 4, NBLK * 128], BF16)
        btab = sb("pb_bt", [128, 8, 5, 128], F32)
        pm = sb("pb_pm", [128, 1], F32)
        qp = [sb("pb_q%d" % i, [128, 8, 128], BF16) for i in range(2)]
        tmp = [sb("pb_t%d" % i, [128, 5, 128], F32) for i in range(2)]
        pt = [sb("pb_p%d" % i, [128, 5, 128], BF16) for i in range(2)]
        rc = [sb("pb_r%d" % i, [128, 8], F32) for i in range(2)]
        ob = [sb("pb_o%d" % i, [128, 8, 64], BF16) for i in range(2)]
        for c in range(4):
            S.dma("sp" if c % 2 == 0 else "pool", vall[:, c * 16:(c + 1) * 16, :],
                  VB[c * 16:(c + 1) * 16].rearrange("k p d -> p k d"), reads=[], writes=["vball"], key="initB", group=True)
            S.dma("pool" if c % 2 == 0 else "sp", kt[:, c, :], KBT[c], reads=[], writes=["kball"], key="initB", group=True)
        S.dma("sp", btab[:].rearrange("p a b c -> p (a b c)"), inp["btab"][:, :], reads=[], writes=["btab"],
              key="initB", group=True)
        S.dma("sp", pm[:], inp["parmask"][:, :], reads=[], writes=["pm"], key="initB", group=True)
        for i in range(2):
            for hb in range(8):
                lo = 64 * (1 - hb % 2)
                S.add("pool", "memset", qp[i][lo:lo + 64, hb, :], 0.0, reads=[], writes=["qbz%d" % i])
        s_banks = [(psum[0], psum[1]), (psum[2], psum[3])]
        o_banks = [(psum[4], psum[5]), (psum[6], psum[7])]
        steps = [(i, hb) for i in range(NOWN) for hb in range(8)]

        def q_load(i):
            Q = qp[i % 2]
            rq = "qb%d" % (i % 2)
            for m in range(4):
                for half in range(2):
                    S.dma("sp" if half == 0 else "pool", Q[64 * half:64 * half + 64, 2 * m + half, :],
                          QBT[m][64 * half:64 * half + 64, i * 128:(i + 1) * 128], reads=[], writes=[rq + "_%d" % (2 * m + half)],
                          key=rq, group=True)

        def emit_s(sidx):
            i, hb = steps[sidx]
            if hb == 0 and i + 1 < NOWN:
                q_load(i + 1)
            kxq = 2 * i + 1
            Q = qp[i % 2]
            rq = "qb%d" % (i % 2)
            tbs = [tb for tb in range(5) if kxq - 4 + tb >= 0]
            m = hb // 2
            sA, sB = s_banks[sidx % 2]
            rs = "sbps%d" % (sidx % 2)
            T, P = tmp[sidx % 2], pt[sidx % 2]
            rt, rp = "bt%d" % (sidx % 2), "bp%d" % (sidx % 2)
            for tb in tbs:
                kx = kxq - 4 + tb
                o = sA[:, tb * 128:(tb + 1) * 128] if tb < 4 else sB[:, 0:128]
                S.add("pe", "matmul", o, kt[:, m, kx * 128:(kx + 1) * 128], Q[:, hb, :], start=True, stop=True,
                      reads=["kball", rq + "_%d" % hb, "qbz%d" % (i % 2)], writes=[rs + ("a" if tb < 4 else "b")])
            lo = tbs[0]
            if lo < 4:
                S.add("dve", "tensor_tensor", T[:, lo:4, :], sA[:, lo * 128:512].rearrange("p (a b) -> p a b", b=128),
                      btab[:, hb, lo:4, :], ALU.add, reads=[rs + "a", "btab"], writes=[rt + "a"])
            S.add("dve", "tensor_tensor", T[:, 4, :], sB[:, 0:128], btab[:, hb, 4, :], ALU.add,
                  reads=[rs + "b", "btab"], writes=[rt + "b"])
            segs = []
            z = [tb for tb in tbs if kxq - 4 + tb == 0]
            if z:
                segs.append((z[0], z[0] + 1, True))
                if z[0] + 1 < 5:
                    segs.append((z[0] + 1, 5, False))
            else:
                segs.append((lo, 5, False))
            for (a_, b_, msk) in segs:
                kw = dict(bias=pm[:, 0:1]) if msk else {}
                S.add("act", "activation", P[:, a_:b_, :], T[:, a_:b_, :], AF.Exp, reads=[rt + "a", rt + "b", "pm"],
                      writes=[rp], **kw)

        def emit_av(sidx):
            i, hb = steps[sidx]
            kxq = 2 * i + 1
            tbs = [tb for tb in range(5) if kxq - 4 + tb >= 0]
            P = pt[sidx % 2]
            rp = "bp%d" % (sidx % 2)
            obk = o_banks[i % 2]
            ro = "obps%d" % (i % 2)
            ot = obk[hb // 4]
            oc = (hb % 4) * 65
            for tb in tbs:
                kx = kxq - 4 + tb
                S.add("pe", "matmul", ot[:, oc:oc + 65], P[:, tb, :], vall[:, kx, hb * 65:(hb + 1) * 65],
                      start=(tb == tbs[0]), stop=(tb == 4), reads=[rp, "vball"], writes=[ro + "_%d" % (hb // 4)])

        def emit_fin(i):
            obk = o_banks[i % 2]
            ro = "obps%d" % (i % 2)
            R, O = rc[i % 2], ob[i % 2]
            rr, rob = "brc%d" % (i % 2), "bob%d" % (i % 2)
            for g in range(2):
                ov = obk[g][:, 0:260].rearrange("p (h d) -> p h d", h=4)
                S.add("dve", "reciprocal", R[:, 4 * g:4 * g + 4], ov[:, :, 64], reads=[ro + "_%d" % g], writes=[rr])
                S.add("dve", "tensor_tensor", O[:, 4 * g:4 * g + 4, :], ov[:, :, 0:64],
                      R[:, 4 * g:4 * g + 4].unsqueeze(2).to_broadcast([128, 4, 64]), ALU.mult,
                      reads=[ro + "_%d" % g, rr], writes=[rob])
            S.dma("sp", OB[i * 128:(i + 1) * 128, :], O[:].rearrange("p h d -> p (h d)"), reads=[rob], writes=["OB%d" % i],
                  key="obst%d" % (i % 2))

        q_load(0)
        ns = len(steps)
        fin_at = {}
        for sidx in range(ns + 4):
            if sidx < ns:
                emit_s(sidx)
            if 0 <= sidx - 1 < ns:
                emit_av(sidx - 1)
                i_, hb_ = steps[sidx - 1]
                if hb_ == 7:
                    fin_at[sidx + 2] = i_
            if sidx in fin_at:
                emit_fin(fin_at.pop(sidx))
        assert not fin_at
        S.flush()


def _rconst():
    t = np.arange(128)
    U = (t[:, None] < t[None, :]).astype(np.float32)
    ones = np.ones((128, 128), np.float32)
    iota = np.tile(np.arange(32, dtype=np.float32)[None, :], (128, 1))
    ecap = iota * E_CAP
    return np.concatenate([U, ones, iota, ecap], 1)


def phaseM(nc, S, psum, ident, inp, xall, lnin, w_in, OA, OB, H1, XS, pers):
    with ExitStack() as st:
        sb = lambda name, shape, dt: st.enter_context(nc.sbuf_tensor(name, list(shape), dt))
        wg = sb("pm_wg", [128, 8, 2048], BF16)
        wba = sb("pm_wba", [128, 4, D], BF16)
        wbb = sb("pm_wbb", [128, 4, D], BF16)
        wo = sb("pm_wo", [128, 8, D], BF16)
        wr = sb("pm_wr", [128, 8, NEXP], F32)
        gbin = sb("pm_gbin", [128, 2, D], F32)
        gb1 = sb("pm_gb1", [128, 2, D], F32)
        bg = sb("pm_bg", [128, 16], F32)
        brb = sb("pm_brb", [128, NEXP], F32)
        rcon = sb("pm_rc", [128, 320], F32)
        ub = sb("pm_ub", [128, 256], BF16)
        identf = sb("pm_idf", [128, 128], F32)
        carry = sb("pm_carry", [128, NEXP], F32)
        xt = [sb("pm_x%d" % i, [128, D], F32) for i in range(2)]
        xn = sb("pm_xn", [128, D], F32)
        hres_all = [sb("pm_hr%d" % i, [128, D], F32) for i in range(4)]
        hb = sb("pm_hb", [128, D], BF16)
        hT_all = [sb("pm_hT%d" % i, [128, 8, 256], BF16) for i in range(2)]
        oab = [sb("pm_oab%d" % i, [128, D], BF16) for i in range(2)]
        oT_all = [sb("pm_oT%d" % i, [128, 8, 256], BF16) for i in range(2)]
        gT = sb("pm_gT", [128, 16, 256], F32)
        mT = sb("pm_mT", [128, 8, 256], BF16)
        t1 = [sb("pm_t1%d" % i, [128, 256], F32) for i in range(2)]
        t2 = [sb("pm_t2%d" % i, [128, 256], F32) for i in range(2)]
        rr_all = [sb("pm_r%d" % i, [128, D], F32) for i in range(4)]
        h1b = [sb("pm_h1b%d" % i, [128, D], BF16) for i in range(2)]
        h1T = sb("pm_h1T", [128, 2, 8, 128], F32)
        st6 = sb("pm_st6", [128, 2, 6], F32)
        mv = sb("pm_mv", [128, 8], F32)
        ln_consts(S, mv, "pms")
        sm = [sb("pm_sm%d" % i, [128, 160], F32) for i in range(2)]
        idx8 = [sb("pm_ix%d" % i, [128, 8], U32) for i in range(2)]
        maskb = [sb("pm_mk%d" % i, [128, NEXP], BF16) for i in range(2)]
        junk = sb("pm_junk", [128, 4 * NEXP], F32)

        S.group_keys.update(["xsc0@sw", "xsc1@sw"])
        bc_reg = nc.gpsimd.alloc_register("xs_bc")
        nc.gpsimd.reg_mov(bc_reg, NEXP * E_CAP - 1)
        ini = dict(key="initM", group=True)
        wgv = w_in[:, 3072:5120].rearrange("(k p) c -> p k c", p=128)
        for k in range(8):
            S.dma("pool", wg[:, k, :], wgv[:, k, :], reads=[], writes=["wg"], **ini)
        S.dma("pool", wba[:], inp["w_ba"].rearrange("(k p) c -> p k c", p=128), reads=[], writes=["wba"], **ini)
        S.dma("pool", wbb[:], inp["w_bb"].rearrange("(k p) c -> p k c", p=128), reads=[], writes=["wbb"], **ini)
        S.dma("pool", wo[:], inp["w_out"].rearrange("(k p) c -> p k c", p=128), reads=[], writes=["wo"], **ini)
        S.dma("sp", wr[:], inp["w_router"].rearrange("(k p) c -> p k c", p=128), reads=[], writes=["wr"], **ini)
        S.dma("sp", gbin[:, 0, :], lnin[0:1, :].partition_broadcast(128), reads=[], writes=["gbin0"], **ini)
        S.dma("sp", gbin[:, 1, :], lnin[1:2, :].partition_broadcast(128), reads=[], writes=["gbin1"], **ini)
        S.dma("sp", gb1[:, 0, :], inp["ln1"][0:1, :].partition_broadcast(128), reads=[], writes=["gb10"], **ini)
        S.dma("sp", gb1[:, 1, :], inp["ln1"][1:2, :].partition_broadcast(128), reads=[], writes=["gb11"], **ini)
        S.dma("sp", bg[:], inp["b_gate"][:, :], reads=[], writes=["bg"], **ini)
        S.dma("sp", brb[:], inp["b_router"][0:1, :].partition_broadcast(128), reads=[], writes=["brb"], **ini)
        S.dma("sp", rcon[:], inp["rconst"][:, :], reads=[], writes=["rcon"], **ini)
        S.dma("pool", ub[:], inp["rconst"][:, 0:256], reads=[], writes=["ub"], **ini)
        S.dma("sp", identf[:], inp["ident"][:, :], reads=[], writes=["identf"], **ini)
        S.add("pool", "memset", carry[:], 0.0, reads=[], writes=["carry"])
        iota = rcon[:, 256:288]
        ecap = rcon[:, 288:320]

        tpb = psum[0]
        tpv = tpb[:].bitcast(BF16)
        g_banks = [psum[1], psum[2]]
        brA, brBk = psum[3], psum[4]
        m_banks = [psum[5], psum[6]]
        rb = psum[7]
        gcnt = 0
        NG = NOWN // 2

        def load(G):
            for t in range(2):
                i = 2 * G + t
                kx = 2 * i + 1
                S.dma("sp", xt[t][:], xall[kx * 128:(kx + 1) * 128, :], reads=[], writes=["mx%d" % t])
                S.dma("sp", oab[t][:, 0:512], OA[i * 128:(i + 1) * 128, :], reads=[], writes=["oab%da" % t])
                S.dma("sp", oab[t][:, 512:1024], OB[i * 128:(i + 1) * 128, :], reads=[], writes=["oab%db" % t])

        hbs = [hb, sb("pm_hb2", [128, D], BF16)]

        def stage_a(G, part="ab"):
            g2 = G % 2
            hres = hres_all[2 * g2:2 * g2 + 2]
            hT, oT = hT_all[g2], oT_all[g2]
            if "a" in part:
                load(G)
                for t in range(2):
                    layer_norm_tile(S, xt[t], "mx%d" % t, xna, "mxna", hres[t], "hres%d_%d" % (g2, t), st6a, mva, "pmsa", gbin,
                                    ["gbin0", "gbin1"])
                    S.add("pool", "tensor_copy", hbs[t][:], hres[t][:], reads=["hres%d_%d" % (g2, t)], writes=["mhb%d" % t])
            if "b" not in part:
                return
            for t in range(2):
                hb = hbs[t]
                for k in range(8):
                    S.add("pe", "transpose", tpv[:, k * 128:(k + 1) * 128], hb[:, k * 128:(k + 1) * 128], ident[:],
                          reads=["mhb%d" % t, "ident"], writes=["mtp"])
                S.add("act", "copy", hT[:, :, t * 128:(t + 1) * 128], tpv.rearrange("p (k t) -> p k t", k=8),
                      reads=["mtp"], writes=["mhT%d_%d" % (g2, t)])
                for k in range(8):
                    S.add("pe", "transpose", tpv[:, k * 128:(k + 1) * 128], oab[t][:, k * 128:(k + 1) * 128], ident[:],
                          reads=["oab%da" % t, "oab%db" % t, "ident"], writes=["mtp"])
                S.add("act", "copy", oT[:, :, t * 128:(t + 1) * 128], tpv.rearrange("p (k t) -> p k t", k=8),
                      reads=["mtp"], writes=["moT%d_%d" % (g2, t)])

        env = (rr_all, h1b, h1T, xn, st6, mv, gb1, H1, g_banks, rb, identf, wr, brb, sm, idx8, maskb, carry, ub, iota, ecap,
               junk, pers, XS, bc_reg)
        stage_b2 = lambda G_, part: _pm_stage_b2(S, G_, env, part)
        st6a = sb("pm_st6a", [128, 2, 6], F32)
        xna = sb("pm_xna", [128, D], F32)
        mva = sb("pm_mva", [128, 8], F32)
        ln_consts(S, mva, "pmsa")
        stage_a(0)
        for G in range(NG):
            g2 = G % 2
            hres = hres_all[2 * g2:2 * g2 + 2]
            hT, oT = hT_all[g2], oT_all[g2]
            if G >= 1:
                stage_b2(G - 1, "a")
            if G + 1 < NG:
                stage_a(G + 1, "a")
            rhT = ["mhT%d_0" % g2, "mhT%d_1" % g2]
            roT = ["moT%d_0" % g2, "moT%d_1" % g2]
            for gc in range(16):
                gbk = g_banks[gcnt % 2]
                rg = "gps%d" % (gcnt % 2)
                gcnt += 1
                for kc in range(8):
                    S.add("pe", "matmul", gbk[:, 0:256], wg[:, kc, gc * 128:(gc + 1) * 128], hT[:, kc, :],
                          start=(kc == 0), stop=(kc == 7), reads=rhT + ["wg"], writes=[rg])
                S.add("act", "activation", gT[:, gc, :], gbk[:, 0:256], AF.Sigmoid, bias=bg[:, gc:gc + 1],
                      reads=[rg, "bg"], writes=["gT%d" % gc])
            if G + 1 < NG:
                stage_a(G + 1, "b")
            if G >= 1:
                stage_b2(G - 1, "b")
                stage_b2(G - 1, "c")
            for oc in range(8):
                for kc in range(4):
                    S.add("pe", "matmul", brA[:, 0:256], wba[:, kc, oc * 128:(oc + 1) * 128], oT[:, kc, :],
                          start=(kc == 0), stop=(kc == 3), reads=roT + ["wba"], writes=["brA"])
                for kc in range(4):
                    S.add("pe", "matmul", brBk[:, 0:256], wbb[:, kc, oc * 128:(oc + 1) * 128], oT[:, 4 + kc, :],
                          start=(kc == 0), stop=(kc == 3), reads=roT + ["wbb"], writes=["brB"])
                T1, T2 = t1[oc % 2], t2[oc % 2]
                S.add("dve", "tensor_tensor", T1[:], brA[:, 0:256], gT[:, oc, :], ALU.mult,
                      reads=["brA", "gT%d" % oc], writes=["mt1%d" % (oc % 2)])
                S.add("dve", "tensor_tensor", T2[:], brBk[:, 0:256], gT[:, 8 + oc, :], ALU.mult,
                      reads=["brB", "gT%d" % (8 + oc)], writes=["mt2%d" % (oc % 2)])
                S.add("pool", "tensor_tensor", mT[:, oc, :], T1[:], T2[:], ALU.add,
                      reads=["mt1%d" % (oc % 2), "mt2%d" % (oc % 2)], writes=["mT%d" % oc])
            rmT = ["mT%d" % oc for oc in range(8)]
            if G >= 1:
                stage_b2(G - 1, "d")
            for t in range(2):
                for half in range(2):
                    for kc in range(8):
                        S.add("pe", "matmul", m_banks[half][:, :], mT[:, kc, t * 128:(t + 1) * 128],
                              wo[:, kc, half * 512:(half + 1) * 512], start=(kc == 0), stop=(kc == 7),
                              reads=rmT + ["wo"], writes=["mps%d" % half])
                R = rr_all[2 * g2 + t]
                for half in range(2):
                    S.add("dve", "scalar_tensor_tensor", R[:, half * 512:(half + 1) * 512],
                          hres[t][:, half * 512:(half + 1) * 512], ALPHA, m_banks[half][:, :], ALU.mult, ALU.add,
                          reads=["hres%d_%d" % (g2, t), "mps%d" % half], writes=["mr%d_%d" % (g2, t)])
        for part in "abcd":
            stage_b2(NG - 1, part)
        if "DESTD" in pers:
            S.dma("sp", pers["DESTD"][:, :], pers["dest"][:].rearrange("p a b -> p (a b)"),
                  reads=["dest%d" % i for i in range(NOWN)], writes=["DESTD"])
            S.dma("sp", pers["GATED"][:, :], pers["gate"][:].rearrange("p a b -> p (a b)"),
                  reads=["gate%d" % i for i in range(NOWN)], writes=["GATED"])
        S.flush()


def _pm_stage_b2(S, G, env, part):
    (rr_all, h1b, h1T, xn, st6, mv, gb1, H1, g_banks, rb, identf, wr, brb, sm, idx8, maskb, carry, ub, iota, ecap, junk,
     pers, XS, bc_reg) = env
    g2 = G % 2
    for t in range(2):
        i = 2 * G + t
        R, HB = rr_all[2 * g2 + t], h1b[t]
        H = R
        rh = "mr%d_%d" % (g2, t)
        SM, IX, MK = sm[t], idx8[t], maskb[t]
        rs = "msm%d" % t
        lg, top8, idxf, e4 = SM[:, 0:32], SM[:, 32:40], SM[:, 40:44], SM[:, 44:48]
        nmax, den, rden = SM[:, 48:49], SM[:, 49:50], SM[:, 50:51]
        posf, ovf, dfull = SM[:, 52:84], SM[:, 84:116], SM[:, 116:148]
        c0 = 96 * t
        if part == "a":
            layer_norm_tile(S, R, rh, xn, "mxn", H, rh, st6, mv, "pms", gb1, ["gb10", "gb11"])
            S.dma("sp", H1[i * 128:(i + 1) * 128, :], H[:], reads=[rh], writes=["H1_%d" % i], key="h1st%d" % t)
            S.add("pool", "tensor_copy", HB[:], H[:], reads=[rh], writes=["mh1b%d" % t])
        elif part == "b":
            for k in range(8):
                bank = g_banks[k // 4]
                S.add("pe", "transpose", bank[:, (k % 4) * 128:(k % 4 + 1) * 128], H[:, k * 128:(k + 1) * 128],
                      identf[:], reads=[rh, "identf"], writes=["gps%d" % (k // 4)])
            S.add("act", "copy", h1T[:, t, 0:4, :], g_banks[0][:, :].rearrange("p (k t) -> p k t", k=4), reads=["gps0"],
                  writes=["h1Ta%d" % t])
            S.add("act", "copy", h1T[:, t, 4:8, :], g_banks[1][:, :].rearrange("p (k t) -> p k t", k=4), reads=["gps1"],
                  writes=["h1Tb%d" % t])
        elif part == "c":
            for kc in range(8):
                S.add("pe", "matmul", rb[:, c0:c0 + 32], h1T[:, t, kc, :], wr[:, kc, :], start=(kc == 0), stop=(kc == 7),
                      reads=["h1Ta%d" % t, "h1Tb%d" % t, "wr"], writes=["rb"])
            S.add("dve", "tensor_tensor", lg, rb[:, c0:c0 + 32], brb[:], ALU.add, reads=["rb", "brb"], writes=[rs])
            S.add("dve", "max", top8, lg, reads=[rs], writes=[rs])
            S.add("dve", "max_index", IX[:], top8, lg, reads=[rs], writes=[rs + "i"])
            S.add("dve", "tensor_copy", idxf, IX[:, 0:4], reads=[rs + "i"], writes=[rs])
            S.add("dve", "tensor_scalar", nmax, top8[:, 0:1], -1.0, None, ALU.mult, reads=[rs], writes=[rs])
            S.add("act", "activation", e4, top8[:, 0:4], AF.Exp, bias=nmax, accum_out=den, reads=[rs], writes=[rs])
            S.add("dve", "tensor_scalar", MK[:], lg, top8[:, 3:4], None, ALU.is_ge, reads=[rs], writes=[rs + "m"])
            S.add("dve", "reciprocal", rden, den, reads=[rs], writes=[rs])
            S.add("dve", "tensor_scalar", pers["gate"][:, i, :], e4, rden, None, ALU.mult, reads=[rs],
                  writes=["gate%d" % i])
        elif part == "d":
            S.add("pe", "matmul", rb[:, c0 + 32:c0 + 64], ub[:, 0:128], MK[:], start=True, stop=True,
                  reads=[rs + "m", "ub"], writes=["rb"])
            S.add("pe", "matmul", rb[:, c0 + 64:c0 + 96], ub[:, 128:256], MK[:], start=True, stop=True,
                  reads=[rs + "m", "ub"], writes=["rb"])
            S.add("dve", "tensor_tensor", posf, rb[:, c0 + 32:c0 + 64], carry[:], ALU.add, reads=["rb", "carry"],
                  writes=[rs])
            S.add("dve", "tensor_tensor", carry[:], rb[:, c0 + 64:c0 + 96], carry[:], ALU.add, reads=["rb", "carry"],
                  writes=["carry"])
            S.add("dve", "tensor_scalar", ovf, posf, float(E_CAP), 1.0e7, ALU.is_ge, ALU.mult, reads=[rs], writes=[rs])
            S.add("dve", "tensor_tensor", dfull, posf, ecap, ALU.add, reads=[rs, "rcon"], writes=[rs])
            S.add("dve", "tensor_tensor", dfull, dfull, ovf, ALU.add, reads=[rs], writes=[rs])
            oh3 = junk[:, 0:128].rearrange("p (k e) -> p k e", k=4)
            S.add("dve", "tensor_tensor", oh3, iota.unsqueeze(1).to_broadcast([128, 4, 32]),
                  idxf.unsqueeze(2).to_broadcast([128, 4, 32]), ALU.is_equal, reads=[rs, "rcon"], writes=["mjunk"])
            S.add("dve", "tensor_tensor", oh3, oh3, dfull.unsqueeze(1).to_broadcast([128, 4, 32]), ALU.mult,
                  reads=[rs, "mjunk"], writes=["mjunk"])
            S.add("dve", "tensor_reduce", SM[:, 152:156], oh3, AX.X, ALU.add, reads=["mjunk"], writes=[rs + "d"])
            S.add("dve", "tensor_copy", pers["dest"][:, i, :], SM[:, 152:156], reads=[rs + "d"], writes=["dest%d" % i])
            S.add("dve", "tensor_scalar", SM[:, 156:160], SM[:, 152:156], 1.0e6, None, ALU.is_lt, reads=[rs + "d"],
                  writes=[rs + "g"])
            S.add("dve", "tensor_tensor", pers["gate"][:, i, :], pers["gate"][:, i, :], SM[:, 156:160], ALU.mult,
                  reads=[rs + "g", "gate%d" % i], writes=["gate%d" % i])
            for k in range(4):
                S.add("pool", "indirect_dma_start", reads=["dest%d" % i, "mh1b%d" % t], writes=["XS_%d_%d" % (i, k)],
                      dma=True, key="xsc%d@sw" % t,
                      out=XS[:, :], out_offset=bass.IndirectOffsetOnAxis(ap=pers["dest"][:, i, k:k + 1], axis=0),
                      in_=HB[:], in_offset=None, bounds_check=bc_reg, oob_is_err=False)


def phaseX(nc, S, psum, ident, inp, XS, YS):
    NB = (E_CAP + 127) // 128
    ntl = [(0, min(512, E_CAP))] + ([(512, E_CAP)] if E_CAP > 512 else [])
    with ExitStack() as st:
        sb = lambda name, shape, dt: st.enter_context(nc.sbuf_tensor(name, list(shape), dt))
        w1 = [sb("px_w1%d" % i, [128, 8, 2048], BF16) for i in range(2)]
        w2 = [sb("px_w2%d" % i, [128, 8, D], BF16) for i in range(2)]
        xs = [sb("px_xs%d" % i, [128, NB, D], BF16) for i in range(2)]
        xT = [sb("px_xT%d" % i, [128, 8, E_CAP], BF16) for i in range(2)]
        aT = sb("px_aT", [128, 8, E_CAP], BF16)
        b1 = sb("px_b1", [128, NEXP, 16], F32)
        b2 = [sb("px_b2%d" % i, [128, D], F32) for i in range(2)]
        gs = [sb("px_g%d" % i, [128, 512], F32) for i in range(3)]
        sg = [sb("px_sg%d" % i, [128, 512], F32) for i in range(3)]
        us = [sb("px_u%d" % i, [128, 512], F32) for i in range(3)]
        ys = [sb("px_y%d" % i, [128, D], F32) for i in range(2)]
        S.dma("sp", b1[:].rearrange("p e c -> p (e c)"), inp["b_e1"][:, :], reads=[], writes=["b1"], key="initX", group=True)
        b1p = sb("px_b1p", [128, NEXP, 16], F32)
        S.add("dve", "tensor_scalar", b1p[:], b1[:], 7.0, None, ALU.add, reads=["b1"], writes=["b1p"])
        tpb = psum[0]
        tpv = tpb[:].bitcast(BF16)
        h_banks = [psum[1], psum[2], psum[3]]
        y_banks = [(psum[4], psum[5]), (psum[6], psum[7])]
        hc = 0
        yc = 0
        ec = 0

        pending = []

        def load(e, defer):
            b = e % 2
            w1v = inp["w_e1"][e].rearrange("(k p) c -> p k c", p=128)
            w2v = inp["w_e2"][e].rearrange("(k p) c -> p k c", p=128)
            lst = []
            for k in range(8):
                lst.append(lambda k=k: S.dma("pool", w1[b][:, k, :], w1v[:, k, :], reads=[], writes=["w1_%d_%d" % (b, k)],
                                             key="w1_%d" % b, group=True))
            for k in range(0, 8, 2):
                lst.append(lambda k=k: S.dma("pool", w2[b][:, k:k + 2, :], w2v[:, k:k + 2, :], reads=[],
                                             writes=["w2_%d_%d" % (b, k // 2)], key="w2_%d" % b, group=True))
            nfull = E_CAP // 128
            S.dma("sp", xs[b][:, 0:nfull, :], XS[e * E_CAP:e * E_CAP + nfull * 128, :].rearrange("(n p) d -> p n d", p=128),
                  reads=[], writes=["xs%d" % b])
            if E_CAP % 128:
                S.dma("sp", xs[b][0:E_CAP % 128, nfull, :], XS[e * E_CAP + nfull * 128:(e + 1) * E_CAP, :], reads=[],
                      writes=["xs%dr" % b])
            S.dma("sp", b2[b][:], inp["b_e2"][e:e + 1, :].partition_broadcast(128), reads=[], writes=["b2_%d" % b])
            if defer:
                pending.extend(lst)
            else:
                for f in lst:
                    f()

        load(0, False)
        for e in range(NEXP):
            b = e % 2
            if e + 1 < NEXP:
                load(e + 1, True)
            W1, W2, XSb, XT = w1[b], w2[b], xs[b], xT[b]
            rw1 = ["w1_%d_%d" % (b, k) for k in range(8)]
            rw2 = ["w2_%d_%d" % (b, k) for k in range(4)]

            def xpose(ee):
                bb = ee % 2
                for n in range(NB):
                    rows = min(128, E_CAP - n * 128)
                    for k in range(8):
                        S.add("pe", "transpose", tpv[:, k * 128:k * 128 + rows], xs[bb][0:rows, n, k * 128:(k + 1) * 128],
                              ident[0:rows, 0:rows], reads=["xs%d" % bb, "xs%dr" % bb, "ident"], writes=["xtp"])
                    S.add("act", "copy", xT[bb][:, :, n * 128:n * 128 + rows],
                          tpv.rearrange("p (k t) -> p k t", k=8)[:, :, 0:rows], reads=["xtp"], writes=["xT%d_%d" % (bb, n)])

            if e == 0:
                xpose(0)
            rxT = ["xT%d_%d" % (b, n) for n in range(NB)]
            for fc in range(8):
                for (n0, n1) in ntl:
                    N = n1 - n0
                    pg = h_banks[hc % 3]
                    rpg = "hps%d" % (hc % 3)
                    hc += 1
                    pu = h_banks[hc % 3]
                    rpu = "hps%d" % (hc % 3)
                    hc += 1
                    for kc in range(8):
                        S.add("pe", "matmul", pg[:, 0:N], W1[:, kc, fc * 128:(fc + 1) * 128], XT[:, kc, n0:n1],
                              start=(kc == 0), stop=(kc == 7), reads=rxT + [rw1[kc]], writes=[rpg])
                    for kc in range(8):
                        S.add("pe", "matmul", pu[:, 0:N], W1[:, kc, 1024 + fc * 128:1024 + (fc + 1) * 128], XT[:, kc, n0:n1],
                              start=(kc == 0), stop=(kc == 7), reads=rxT + [rw1[kc]], writes=[rpu])
                    q = ec % 3
                    ec += 1
                    Gs, Sg, Us = gs[q], sg[q], us[q]
                    S.add("dve", "tensor_scalar", Gs[:, 0:N], pg[:, 0:N], b1[:, e, fc:fc + 1], 7.0, ALU.add, ALU.min,
                          reads=[rpg, "b1"], writes=["xg%d" % q])
                    S.add("act", "activation", Us[:, 0:N], pu[:, 0:N], AF.Relu, bias=b1p[:, e, 8 + fc:9 + fc],
                          reads=[rpu, "b1p"], writes=["xu%d" % q])
                    S.add("act", "activation", Sg[:, 0:N], Gs[:, 0:N], AF.Gelu_apprx_sigmoid, reads=["xg%d" % q],
                          writes=["xsg%d" % q])
                    S.add("dve", "tensor_scalar", Us[:, 0:N], Us[:, 0:N], 14.0, -6.0, ALU.min, ALU.add,
                          reads=["xu%d" % q], writes=["xu%d" % q])
                    S.add("dve", "tensor_tensor", aT[:, fc, n0:n1], Sg[:, 0:N], Us[:, 0:N], ALU.mult,
                          reads=["xsg%d" % q, "xu%d" % q], writes=["aT%d_%d" % (fc, n0)])
                    if pending:
                        pending.pop(0)()
            while pending:
                pending.pop(0)()
            if e + 1 < NEXP:
                xpose(e + 1)
            raT = ["aT%d_%d" % (fc, n0) for fc in range(8) for (n0, _) in ntl]
            for n in range(NB):
                yb = y_banks[yc % 2]
                ry = "yps%d" % (yc % 2)
                Y = ys[yc % 2]
                rys = "ysb%d" % (yc % 2)
                yc += 1
                rows = min(128, E_CAP - n * 128)
                for half in range(2):
                    for kc in range(8):
                        S.add("pe", "matmul", yb[half][0:rows, :], aT[:, kc, n * 128:n * 128 + rows],
                              W2[:, kc, half * 512:(half + 1) * 512], start=(kc == 0), stop=(kc == 7),
                              reads=raT + [rw2[kc // 2]], writes=[ry + "_%d" % half])
                for half in range(2):
                    S.add("dve", "tensor_tensor", Y[0:rows, half * 512:(half + 1) * 512], yb[half][0:rows, :],
                          b2[b][0:rows, half * 512:(half + 1) * 512], ALU.add, reads=[ry + "_%d" % half, "b2_%d" % b],
                          writes=[rys])
                r0 = e * E_CAP + n * 128
                S.dma("sp", YS[r0:r0 + rows, :], Y[0:rows, :], reads=[rys], writes=["YS_%d_%d" % (e, n)], key=rys)
        S.flush()


def phaseF(nc, S, psum, inp, H1, YS, out, pers):
    with ExitStack() as st:
        sb = lambda name, shape, dt: st.enter_context(nc.sbuf_tensor(name, list(shape), dt))
        gb2 = sb("pf_gb2", [128, 2, D], F32)
        h1 = [sb("pf_h%d" % i, [128, D], F32) for i in range(2)]
        yk = [[sb("pf_y%d_%d" % (i, k), [128, D], F32) for k in range(4)] for i in range(2)]
        acc = [sb("pf_acc%d" % i, [128, D], F32) for i in range(2)]
        xn = sb("pf_xn", [128, D], F32)
        ot = [sb("pf_o%d" % i, [128, D], F32) for i in range(2)]
        st6 = sb("pf_st6", [128, 2, 6], F32)
        mv = sb("pf_mv", [128, 8], F32)
        ln_consts(S, mv, "pfs")
        bc_reg = nc.gpsimd.alloc_register("ys_bc")
        nc.gpsimd.reg_mov(bc_reg, NEXP * E_CAP - 1)
        S.dma("sp", gb2[:, 0, :], inp["ln2"][0:1, :].partition_broadcast(128), reads=[], writes=["gb20"], key="initF", group=True)
        S.dma("sp", gb2[:, 1, :], inp["ln2"][1:2, :].partition_broadcast(128), reads=[], writes=["gb21"], key="initF", group=True)
        def fetch(i):
            b = i % 2
            S.dma("sp", h1[b][:], H1[i * 128:(i + 1) * 128, :], reads=[], writes=["fh%d" % b])
            for k in range(4):
                S.add("pool", "indirect_dma_start", reads=[], writes=["fy%d_%d" % (b, k)], dma=True, key="fy%d_%d@sw" % (b, k),
                      out=yk[b][k][:], out_offset=None, in_=YS[:, :],
                      in_offset=bass.IndirectOffsetOnAxis(ap=pers["dest"][:, i, k:k + 1], axis=0),
                      bounds_check=bc_reg, oob_is_err=False)

        for b_ in range(2):
            for k in range(4):
                S.add("dve", "memset", yk[b_][k][:], 0.0, reads=[], writes=["fy%d_%d" % (b_, k)])
        fetch(0)
        for i in range(NOWN):
            b = i % 2
            if i + 1 < NOWN:
                fetch(i + 1)
            A = acc[b]
            ra = "facc%d" % b
            S.add("act", "activation", A[:], yk[b][0][:], AF.Copy, scale=pers["gate"][:, i, 0:1],
                  reads=["fy%d_0" % b], writes=[ra])
            S.add("dve", "scalar_tensor_tensor", A[:], h1[b][:], ALPHA, A[:], ALU.mult, ALU.add,
                  reads=[ra, "fh%d" % b], writes=[ra])
            for k in range(1, 4):
                S.add("dve", "scalar_tensor_tensor", A[:], yk[b][k][:], pers["gate"][:, i, k:k + 1], A[:], ALU.mult, ALU.add,
                      reads=["fy%d_%d" % (b, k), ra], writes=[ra])
            layer_norm_tile(S, A, ra, xn, "fxn", ot[b], "fo%d" % b, st6, mv, "pfs", gb2, ["gb20", "gb21"])
            S.dma("sp", out[i * 128:(i + 1) * 128, :], ot[b][:], reads=["fo%d" % b], writes=["out%d" % i], key="ost%d" % b)
        S.flush()


def kernel(**inputs):
    nc, S = build_program()
    in_maps = [_core_inputs(c, inputs) for c in range(8)]
    res = run_bass_kernel_spmd(nc, in_maps, core_ids=list(range(8)))
    outp = np.zeros((4, 64, 128, D), np.float32)
    for c in range(8):
        b, j = divmod(c, 2)
        outp[b, j::2] = np.asarray(res.results[c]["out"]).reshape(NOWN, 128, D)
    return outp.reshape(4, 8192, D)
```

```python
import numpy as np
from contextlib import ExitStack
import concourse.bass as bass
import concourse.mybir as mybir
from concourse.bass_utils import run_bass_kernel_spmd

F32 = mybir.dt.float32
BF16 = mybir.dt.bfloat16
I32 = mybir.dt.int32
U32 = mybir.dt.uint32
ALU = mybir.AluOpType
AF = mybir.ActivationFunctionType
AX = mybir.AxisListType

D = 1024
NBLK = 64
NOWN = 32
NTOK = NOWN * 128
ALPHA = 2.0 ** 0.25
EPS = 1e-5
NEG = -30000.0
LAM_INIT = 0.8 - 0.6
SLOPES = [2.0 ** (-8.0 * (i + 1) / 4) for i in range(4)]
A_WINDOW = [3, 9, 33, 10 ** 6]
E_CAP = 704
NEXP = 32

SAME_ENG_SYNC = True
PA_DBG = {}


class Op:
    __slots__ = ("eng", "name", "args", "kwargs", "reads", "writes", "dma", "key", "deps", "token")


class Sched:
    def __init__(self, nc, stack):
        self.nc = nc
        self.eng = {"pe": nc.tensor, "act": nc.scalar, "dve": nc.vector, "pool": nc.gpsimd, "sp": nc.sync}
        self.esem = {e: stack.enter_context(nc.semaphore("es_" + e)) for e in self.eng}
        self.ecount = {e: 0 for e in self.eng}
        self.dsem = {}
        self.scount = {}
        self.sem_pool = []
        self.all_sems = []
        self.sem_kind = {}
        self.stack = stack
        self.waited = {e: {} for e in self.eng}
        self.ops = []
        self.n_emitted = 0
        self.group_keys = set()

    def add(self, eng, name, *args, reads=(), writes=(), dma=False, key=None, **kwargs):
        op = Op()
        op.eng, op.name, op.args, op.kwargs = eng, name, args, kwargs
        op.reads, op.writes, op.dma, op.key = tuple(reads), tuple(writes), dma, key
        op.token = None
        self.ops.append(op)
        return op

    def dma(self, eng, out, in_, reads, writes, key=None, group=False, **kw):
        if key is None:
            key = writes[0]
        key = key + ("@sw" if eng == "pool" else "@hw")
        if group:
            self.group_keys.add(key)
        return self.add(eng, "dma_start", reads=reads, writes=writes, dma=True, key=key, out=out, in_=in_, **kw)

    def _dsem(self, key):
        if key not in self.dsem:
            kind = key[-3:]
            cand = [x for x in self.sem_pool if self.sem_kind[x] == kind]
            if cand:
                sem = cand[-1]
                self.sem_pool.remove(sem)
            else:
                sem = self.stack.enter_context(self.nc.semaphore("ds_%d" % len(self.all_sems)))
                self.all_sems.append(sem)
                self.scount[sem] = 0
                self.sem_kind[sem] = key[-3:]
            self.dsem[key] = sem
        return self.dsem[key]

    def flush(self):
        ops = self.ops
        self.ops = []
        n = len(ops)
        last_w = {}
        readers = {}
        for i, op in enumerate(ops):
            deps = set()
            for r in op.reads:
                if r in last_w:
                    deps.add(last_w[r])
            for w in op.writes:
                if w in last_w:
                    deps.add(last_w[w])
                rd = readers.get(w)
                if rd:
                    deps.update(rd.values())
            deps.discard(i)
            kept = []
            for j in deps:
                pj = ops[j]
                if (not pj.dma) and (not op.dma) and pj.eng == op.eng:
                    if op.eng == "pe" or not SAME_ENG_SYNC:
                        continue
                kept.append(j)
            op.deps = sorted(kept)
            for r in op.reads:
                d = readers.setdefault(r, {})
                d[("d%d" % i) if op.dma else op.eng] = i
            for w in op.writes:
                last_w[w] = i
                readers[w] = {}
        needs = [False] * n
        for op in ops:
            for j in op.deps:
                needs[j] = True
        last_on = {}
        for i, op in enumerate(ops):
            if not op.dma:
                last_on[op.eng] = i
        for i in last_on.values():
            needs[i] = True
        for i, op in enumerate(ops):
            E = op.eng
            e = self.eng[E]
            wt = self.waited[E]
            for j in op.deps:
                sem, val = ops[j].token
                if ops[j].dma and ops[j].key in self.group_keys:
                    val = self.scount[sem]
                if wt.get(sem, 0) < val:
                    e.wait_ge(sem, val)
                    wt[sem] = val
            try:
                ins = getattr(e, op.name)(*op.args, **op.kwargs)
            except Exception:
                print("EMIT FAIL", E, op.name, op.writes, [str(a)[:120] for a in op.args], {k: str(v)[:120] for k, v in op.kwargs.items()})
                raise
            if op.dma:
                sem = self._dsem(op.key)
                self.scount[sem] += 16
                ins.then_inc(sem, 16)
                op.token = (sem, self.scount[sem])
            elif needs[i]:
                self.ecount[E] += 1
                ins.then_inc(self.esem[E], 1)
                op.token = (self.esem[E], self.ecount[E])
        self.n_emitted += n
        for E, e in self.eng.items():
            wt = self.waited[E]
            for F in self.eng:
                if F != E and self.ecount[F] > wt.get(self.esem[F], 0):
                    e.wait_ge(self.esem[F], self.ecount[F])
                    wt[self.esem[F]] = self.ecount[F]
            for sem in self.all_sems:
                if self.scount[sem] > wt.get(sem, 0):
                    e.wait_ge(sem, self.scount[sem])
                    wt[sem] = self.scount[sem]
        self.sem_pool.extend(self.dsem.values())
        self.dsem = {}


def _dram(nc, name, shape, dt, dbg):
    return nc.dram_tensor(name, list(shape), dt, kind=("ExternalOutput" if dbg else "Internal")).ap()


def build_program(stop_after=None, dbg=()):
    nc = bass.Bass("TRN2", target_bir_lowering=False)
    inp = {}

    def din(name, shape, dt=F32):
        inp[name] = nc.dram_tensor(name, list(shape), dt, kind="ExternalInput").ap()
        return inp[name]

    xall = din("xall", [NBLK * 128, D])
    lnin = din("lnin", [2, D])
    w_in = din("w_in", [D, 5120])
    ident_in = din("ident", [128, 128])
    din("abias", [128, 2 * 4 * 64])
    din("adiag", [128, 4 * 2 * 128])
    din("lamv", [4, 64])
    din("subln", [1, 128])
    din("btab", [128, 8 * 5 * 128])
    din("parmask", [128, 1])
    out = nc.dram_tensor("out", [NTOK, D], F32, kind="ExternalOutput").ap()
    din("b_gate", [128, 16])
    din("w_ba", [512, D])
    din("w_bb", [512, D])
    din("w_out", [D, D])
    din("ln1", [2, D])
    din("w_router", [D, NEXP])
    din("b_router", [1, NEXP])
    din("rconst", [128, 128 + 128 + 32 + 32])
    H1 = _dram(nc, "H1", [NTOK, D], F32, "H1" in dbg)
    YS = _dram(nc, "YS", [NEXP * E_CAP, D], F32, "YS" in dbg)
    din("w_e1", [NEXP, D, 2048])
    din("w_e2", [NEXP, D, D])
    din("b_e1", [128, NEXP * 16])
    din("b_e2", [NEXP, D])
    din("ln2", [2, D])
    XS = _dram(nc, "XS", [NEXP * E_CAP, D], BF16, "XS" in dbg)
    OA = _dram(nc, "OA", [NTOK, 512], BF16, "OA" in dbg)
    OB = _dram(nc, "OB", [NTOK, 512], BF16, "OB" in dbg)

    KAT = _dram(nc, "KAT", [4, 128, NBLK * 128], BF16, "KAT" in dbg)
    KBT = _dram(nc, "KBT", [4, 128, NBLK * 128], BF16, "KBT" in dbg)
    QAT = _dram(nc, "QAT", [4, 128, NTOK], BF16, "QAT" in dbg)
    QBT = _dram(nc, "QBT", [4, 128, NTOK], BF16, "QBT" in dbg)
    VA = _dram(nc, "VA", [NBLK, 128, 4 * 129], BF16, "VA" in dbg)
    VB = _dram(nc, "VB", [NBLK, 128, 8 * 65], BF16, "VB" in dbg)

    with ExitStack() as stack:
        S = Sched(nc, stack)
        psum = [stack.enter_context(nc.psum_tensor("ps%d" % i, [128, 512], F32)) for i in range(8)]
        ident = stack.enter_context(nc.sbuf_tensor("identb", [128, 128], BF16))
        S.dma("pool", ident[:], ident_in[:, :], reads=[], writes=["ident"], key="init", group=True)

        if "skip0" not in dbg:
            phase0(nc, S, psum, ident, xall, lnin, w_in, KAT, KBT, QAT, QBT, VA, VB)
        if stop_after == 0:
            return nc, S
        if "skipA" not in dbg:
            phaseA(nc, S, psum, inp, KAT, QAT, VA, OA)
        if stop_after == 1:
            return nc, S
        if "skipB" not in dbg:
            phaseB(nc, S, psum, inp, KBT, QBT, VB, OB)
        if stop_after == 2:
            return nc, S
        pers = {
            "dest": stack.enter_context(nc.sbuf_tensor("dest_i", [128, NOWN, 4], I32)),
            "gate": stack.enter_context(nc.sbuf_tensor("gates4", [128, NOWN, 4], F32)),
        }
        if "DESTD" in dbg:
            pers["DESTD"] = _dram(nc, "DESTD", [128, NOWN * 4], I32, True)
            pers["GATED"] = _dram(nc, "GATED", [128, NOWN * 4], F32, True)
        if "skipM" not in dbg:
            phaseM(nc, S, psum, ident, inp, xall, lnin, w_in, OA, OB, H1, XS, pers)
        if stop_after == 3:
            return nc, S
        if "skipX" not in dbg:
            phaseX(nc, S, psum, ident, inp, XS, YS)
        if stop_after == 4:
            return nc, S
        phaseF(nc, S, psum, inp, H1, YS, out, pers)
    return nc, S


def phase0(nc, S, psum, ident, xall, lnin, w_in, KAT, KBT, QAT, QBT, VA, VB):
    with ExitStack() as st:
        sb = lambda name, shape, dt: st.enter_context(nc.sbuf_tensor(name, list(shape), dt))
        gb = sb("p0_gb", [128, 2, D], F32)
        wkv = sb("p0_w", [128, 8, 3072], BF16)
        xt = [sb("p0_x%d" % i, [128, D], F32) for i in range(3)]
        xn = [sb("p0_xn%d" % i, [128, D], F32) for i in range(2)]
        hb = [sb("p0_hb%d" % i, [128, D], BF16) for i in range(2)]
        hT = [sb("p0_hT%d" % i, [128, 8, 512], BF16) for i in range(2)]
        st6 = [sb("p0_st%d" % i, [128, 2, 6], F32) for i in range(2)]
        mv = [sb("p0_mv%d" % i, [128, 8], F32) for i in range(2)]
        for i in range(2):
            ln_consts(S, mv[i], "p0s%d" % i)
        kq = [sb("p0_kq%d" % i, [128, 16, 512], BF16) for i in range(2)]
        vas = [sb("p0_va%d" % i, [128, 4, 129], BF16) for i in range(2)]
        vbs = [sb("p0_vb%d" % i, [128, 8, 65], BF16) for i in range(2)]

        S.dma("sp", gb[:, 0, :], lnin[0:1, :].partition_broadcast(128), reads=[], writes=["gb0"], key="init", group=True)
        S.dma("sp", gb[:, 1, :], lnin[1:2, :].partition_broadcast(128), reads=[], writes=["gb1"], key="init", group=True)
        wv = w_in[:, 0:3072].rearrange("(k p) c -> p k c", p=128)
        for k in range(8):
            S.dma("pool", wkv[:, k, :], wv[:, k, :], reads=[], writes=["wkv%d" % k], key="init", group=True)
        for i in range(2):
            S.add("pool", "memset", vas[i][:, :, 128:129], 1.0, reads=[], writes=["vas1_%d" % i])
            S.add("pool", "memset", vbs[i][:, :, 64:65], 1.0, reads=[], writes=["vbs1_%d" % i])
        wk_all = ["wkv%d" % k for k in range(8)]

        tp_banks = [psum[0], psum[1]]
        kq_banks = [psum[2], psum[3], psum[4]]
        v_banks = [psum[5], psum[6], psum[7]]
        vcnt = 0
        kcnt = 0
        def load_x(kx):
            S.dma("sp", xt[kx % 3][:], xall[kx * 128:(kx + 1) * 128, :], reads=[], writes=["x%d" % (kx % 3)])

        def stage_a(kx):
            b2 = kx % 2
            X, XN, HB = xt[kx % 3], xn[b2], hb[b2]
            rx, rxn, rhb = "x%d" % (kx % 3), "xn%d" % b2, "hb%d" % b2
            layer_norm_tile(S, X, rx, XN, rxn, HB, rhb, st6[b2], mv[b2], "p0s%d" % b2, gb, ["gb0", "gb1"])

        load_x(0)
        load_x(1)
        stage_a(0)
        for kx in range(NBLK):
            b2 = kx % 2
            u, blk = divmod(kx, 4)
            g2 = u % 2
            XN, HB = xn[b2], hb[b2]
            rxn, rhb = "xn%d" % b2, "hb%d" % b2
            if kx + 2 < NBLK:
                load_x(kx + 2)
            if kx + 1 < NBLK:
                stage_a(kx + 1)
            tpb = tp_banks[b2]
            tpv = tpb[:].bitcast(BF16)
            rtp = "tp%d" % b2
            for k in range(8):
                S.add("pe", "transpose", tpv[:, k * 128:(k + 1) * 128], HB[:, k * 128:(k + 1) * 128], ident[:],
                      reads=[rhb, "ident"], writes=[rtp])
            rhT = "hT%d_%d" % (g2, blk)
            S.add("act", "copy", hT[g2][:, :, blk * 128:(blk + 1) * 128],
                  tpv.rearrange("p (k t) -> p k t", k=8), reads=[rtp], writes=[rhT])
            for which, (c0, stg, rs, dst, nh, dv) in enumerate(
                    [(1024, vas[b2], "vas_%d" % b2, VA, 4, 128), (2560, vbs[b2], "vbs_%d" % b2, VB, 8, 64)]):
                vb = v_banks[vcnt % 3]
                rv = "vps%d" % (vcnt % 3)
                vcnt += 1
                for kc in range(8):
                    S.add("pe", "matmul", vb[:], hT[g2][:, kc, blk * 128:(blk + 1) * 128], wkv[:, kc, c0:c0 + 512],
                          start=(kc == 0), stop=(kc == 7), reads=[rhT, "wkv%d" % kc], writes=[rv])
                eng = "act" if which == 0 else "dve"
                if eng == "act":
                    S.add("act", "copy", stg[:, :, 0:dv], vb[:].rearrange("p (h d) -> p h d", h=nh),
                          reads=[rv], writes=[rs])
                else:
                    S.add("dve", "tensor_copy", stg[:, :, 0:dv], vb[:].rearrange("p (h d) -> p h d", h=nh),
                          reads=[rv], writes=[rs])
                S.dma("sp", dst[kx], stg[:].rearrange("p h d -> p (h d)"),
                      reads=[rs, ("vas1_%d" if which == 0 else "vbs1_%d") % b2], writes=["dram_v%d_%d" % (which, kx)],
                      key="vst%d_%d" % (which, b2))
            if blk == 3:
                KQ = kq[g2]
                rgrp = ["hT%d_%d" % (g2, t) for t in range(4)]
                for ci in range(16):
                    kind, m = divmod(ci, 4)
                    c0 = [512, 2048, 0, 1536][kind] + 128 * m
                    pb = kq_banks[kcnt % 3]
                    rp = "kqps%d" % (kcnt % 3)
                    kcnt += 1
                    isq = kind >= 2
                    N = 256 if isq else 512
                    for kc in range(8):
                        if isq:
                            rhs = hT[g2][:, kc, :].rearrange("p (a b t) -> p a b t", a=2, b=2)[:, :, 1, :]
                            o = pb[:, 0:256].rearrange("p (a t) -> p a t", a=2)
                        else:
                            rhs = hT[g2][:, kc, :]
                            o = pb[:, :]
                        S.add("pe", "matmul", o, wkv[:, kc, c0:c0 + 128], rhs, start=(kc == 0), stop=(kc == 7),
                              reads=rgrp + ["wkv%d" % kc], writes=[rp])
                    rk = "kq%d_%d" % (g2, ci)
                    if isq:
                        S.add("act", "activation", KQ[:, ci, 0:N], pb[:, 0:N], AF.Copy, scale=0.125,
                              reads=[rp], writes=[rk])
                    else:
                        S.add("dve", "tensor_copy", KQ[:, ci, 0:N], pb[:, 0:N], reads=[rp], writes=[rk])
                    dst = [KAT, KBT, QAT, QBT][kind]
                    S.dma("sp" if ci % 2 == 0 else "pool", dst[m][:, u * N:(u + 1) * N], KQ[:, ci, 0:N], reads=[rk],
                          writes=["dram_kq%d_%d_%d" % (kind, m, u)], key="kqst%d_%d" % (g2, ci))
        S.flush()


def layer_norm_tile(S, X, rx, XN, rxn, OUT, rout, st6, mv, rs, gb, rgb):
    for c in range(2):
        S.add("dve", "bn_stats", st6[:, c, :], X[:, c * 512:(c + 1) * 512], reads=[rx], writes=[rs + "a"])
    S.add("dve", "bn_aggr", mv[:, 0:2], st6[:].rearrange("p a b -> p (a b)"), reads=[rs + "a"], writes=[rs + "b"])
    S.add("dve", "scalar_tensor_tensor", XN[:], X[:], mv[:, 0:1], gb[:, 0, :], ALU.subtract, ALU.mult,
          reads=[rx, rs + "b", rgb[0]], writes=[rxn])
    S.add("pool", "tensor_scalar", mv[:, 2:3], mv[:, 1:2], EPS, None, ALU.add, reads=[rs + "b"], writes=[rs + "c"])
    S.add("pool", "tensor_tensor", mv[:, 3:4], mv[:, 2:3], mv[:, 4:5], ALU.pow, reads=[rs + "c", rs + "k"], writes=[rs + "d"])
    S.add("dve", "scalar_tensor_tensor", OUT[:], XN[:], mv[:, 3:4], gb[:, 1, :], ALU.mult, ALU.add,
          reads=[rxn, rs + "d", rgb[1]], writes=[rout])


def ln_consts(S, mv, rs):
    S.add("pool", "memset", mv[:, 4:5], -0.5, reads=[], writes=[rs + "k"])

def phaseA(nc, S, psum, inp, KAT, QAT, VA, OA):
    with ExitStack() as st:
        sb = lambda name, shape, dt: st.enter_context(nc.sbuf_tensor(name, list(shape), dt))
        vall = sb("pa_v", [128, NBLK, 4 * 129], BF16)
        kt = [sb("pa_k%d" % i, [128, NBLK * 128], BF16) for i in range(2)]
        qt = [sb("pa_q%d" % i, [128, 2, NTOK], BF16) for i in range(2)]
        oa = sb("pa_oa", [128, NOWN, 512], BF16)
        abias = sb("pa_ab", [128, 2, 4, 64], F32)
        adiag = sb("pa_ad", [128, 4, 256], F32)
        lamt = sb("pa_lam", [128, 4, 64], F32)
        lams = sb("pa_lams", [128, 8], F32)
        subw = sb("pa_sw", [128, 128], F32)
        NPT = 4
        pt = [sb("pa_pt%d" % i, [128, 512], BF16) for i in range(NPT)]
        dtmp = [sb("pa_dt%d" % i, [128, 512], F32) for i in range(2)]
        fin = [sb("pa_fin%d" % i, [128, 8], F32) for i in range(2)]
        t0 = [sb("pa_t0%d" % i, [128, 128], F32) for i in range(2)]
        av = [sb("pa_a%d" % i, [128, 128], F32) for i in range(2)]
        junk = sb("pa_junk", [128, 128], F32)
        for f_ in fin:
            S.add("pool", "memset", f_[:, 6:7], -0.5, reads=[], writes=["fink"])
        zt = sb("pa_zt", [1, 512], BF16)
        S.add("pool", "memset", zt[:], 0.0, reads=[], writes=["zt"])

        for i in range(2):
            S.add("pool", "memset", qt[i][0:64, 1, :], 0.0, reads=[], writes=["qtz%d" % i])
            S.add("pool", "memset", qt[i][64:128, 0, :], 0.0, reads=[], writes=["qtz%d" % i])
        for c in range(4):
            S.dma("sp" if c % 2 == 0 else "pool", vall[:, c * 16:(c + 1) * 16, :],
                  VA[c * 16:(c + 1) * 16].rearrange("k p d -> p k d"), reads=[], writes=["vall%d" % c],
                  key="initA", group=True)
        S.dma("sp", abias[:].rearrange("p a b c -> p (a b c)"), inp["abias"][:, :], reads=[], writes=["abias"],
              key="initA", group=True)
        S.dma("sp", adiag[:].rearrange("p a b -> p (a b)"), inp["adiag"][:, :], reads=[], writes=["adiag"],
              key="initA", group=True)
        for v in range(4):
            S.dma("sp", lamt[:, v, :], inp["lamv"][v:v + 1, :].partition_broadcast(128), reads=[], writes=["lamt"],
                  key="initA", group=True)
        S.dma("sp", subw[:], inp["subln"][0:1, :].partition_broadcast(128), reads=[], writes=["subw"],
              key="initA", group=True)
        for v in range(2):
            S.add("dve", "tensor_tensor", lamt[:, 2 * v, :], lamt[:, 2 * v, :], lamt[:, 2 * v + 1, :], ALU.mult,
                  reads=["lamt"], writes=["lamt"])
            S.add("dve", "reduce_sum", lams[:, v:v + 1], lamt[:, 2 * v, :], AX.X, reads=["lamt"], writes=["lams"])
        S.add("act", "activation", lams[:, 2:4], lams[:, 0:2], AF.Exp, reads=["lams"], writes=["lams"])
        S.add("dve", "tensor_tensor", lams[:, 4:5], lams[:, 3:4], lams[:, 2:3], ALU.subtract,
              reads=["lams"], writes=["lams"])
        S.add("dve", "tensor_scalar", lams[:, 4:5], lams[:, 4:5], -LAM_INIT, None, ALU.add,
              reads=["lams"], writes=["lams"])
        S.add("dve", "tensor_scalar", subw[:], subw[:], 1.0 - LAM_INIT, None, ALU.mult, reads=["subw"], writes=["subw"])

        s_banks = [psum[0], psum[1], psum[2], psum[3]]
        o_banks = [psum[4], psum[5], psum[6], psum[7]]
        LOOK = 2
        steps = []
        for h in range(PA_DBG.get("heads", 4)):
            for ia in range(0, PA_DBG.get("qblocks", NOWN), 2):
                rmax = [min(A_WINDOW[h], 2 * (ia + s_) + 1) for s_ in range(2)]
                for rel in range(max(rmax), -1, -1):
                    slots = []
                    for s_ in range(2):
                        i = ia + s_
                        if rel <= rmax[s_]:
                            slots.append((s_, i, 2 * i + 1 - rel, rel == rmax[s_]))
                    steps.append((h, rel, slots))
        loaded = set()
        deferred = []
        pcnt = {}

        def head_load(h):
            KT, QT = kt[h % 2], qt[h % 2]
            rk, rq = "kt%d" % (h % 2), "qt%d" % (h % 2)
            for c in range(4):
                S.dma("sp" if c % 2 == 0 else "pool", KT[:, c * 2048:(c + 1) * 2048], KAT[h][:, c * 2048:(c + 1) * 2048],
                      reads=[], writes=[rk + "_%d" % c], key=rk, group=True)
            S.dma("sp", QT[0:64, 0, :], QAT[h][0:64, :], reads=[], writes=[rq + "a"])
            S.dma("pool", QT[64:128, 1, :], QAT[h][64:128, :], reads=[], writes=[rq + "b"])

        def emit_s(sidx):
            h, rel, slots = steps[sidx]
            if h not in loaded:
                loaded.add(h)
                head_load(h)
            KT, QT = kt[h % 2], qt[h % 2]
            rk, rq = "kt%d" % (h % 2), "qt%d" % (h % 2)
            sbk = s_banks[sidx % 4]
            rs = "sps%d" % (sidx % 4)
            P = pt[sidx % NPT]
            rp = "pt%d" % (sidx % NPT)
            for (s_, i, kx, first) in slots:
                for c in range(2):
                    col = s_ * 256 + c * 128
                    S.add("pe", "matmul", sbk[:, col:col + 128], KT[:, kx * 128:(kx + 1) * 128],
                          QT[:, c, i * 128:(i + 1) * 128], start=True, stop=True,
                          reads=[rk + "_%d" % (kx // 16), rq + "a", rq + "b", "qtz%d" % (h % 2)], writes=[rs])
            lo = slots[0][0] * 256
            hi = slots[-1][0] * 256 + 256
            if rel == 0:
                DT = dtmp[sidx % 2]
                rd = "dtmp%d" % (sidx % 2)
                n2 = (hi - lo) // 256
                S.add("dve", "tensor_tensor", DT[:, lo:hi].rearrange("p (a b) -> p a b", a=n2),
                      sbk[:, lo:hi].rearrange("p (a b) -> p a b", a=n2),
                      adiag[:, h, :].unsqueeze(1).to_broadcast([128, n2, 256]), ALU.add, reads=[rs, "adiag"], writes=[rd])
                S.add("act", "activation", P[:, lo:hi], DT[:, lo:hi], AF.Exp, reads=[rd], writes=[rp])
            elif any(kx == 0 for (_, _, kx, _) in slots):
                for (s_, i, kx, first) in slots:
                    z = 1 if kx == 0 else 0
                    S.add("act", "activation", P[:, s_ * 256:s_ * 256 + 256], sbk[:, s_ * 256:s_ * 256 + 256], AF.Exp,
                          bias=abias[:, z, h, rel:rel + 1], reads=[rs, "abias"], writes=[rp])
            else:
                S.add("act", "activation", P[:, lo:hi], sbk[:, lo:hi], AF.Exp, bias=abias[:, 0, h, rel:rel + 1],
                      reads=[rs, "abias"], writes=[rp])

        def emit_av(sidx):
            h, rel, slots = steps[sidx]
            P = pt[sidx % NPT]
            rp = "pt%d" % (sidx % NPT)
            for (s_, i, kx, first) in slots:
                key = (h, i)
                if key not in pcnt:
                    pcnt[key] = len(pcnt)
                qn = pcnt[key]
                ob = o_banks[qn % 4]
                ro = "ops%d" % (qn % 4)
                if first:
                    S.add("pe", "matmul", ob[:, 0:258], zt[0:1, 0:128], zt[0:1, 0:258], start=True, stop=False,
                          reads=["zt"], writes=[ro])
                for c in range(2):
                    col = s_ * 256 + c * 128
                    S.add("pe", "matmul", ob[:, c * 129:(c + 1) * 129], P[:, col:col + 128],
                          vall[:, kx, h * 129:(h + 1) * 129], start=False, stop=(rel == 0 and c == 1),
                          reads=[rp, "vall%d" % (kx // 16)], writes=[ro])
                if rel == 0:
                    deferred.append((sidx + 3, h, i, qn))

        def emit_fin(h, i, qn):
            ob = o_banks[qn % 4]
            ro = "ops%d" % (qn % 4)
            f2 = qn % 2
            F, T0, A = fin[f2], t0[f2], av[f2]
            rf = "fin%d" % f2
            S.add("dve", "reciprocal", F[:, 0:2], ob[:, 0:258].rearrange("p (c d) -> p c d", c=2)[:, :, 128],
                  reads=[ro], writes=[rf])
            S.add("dve", "tensor_tensor", F[:, 2:3], F[:, 1:2], lams[:, 4:5], ALU.mult, reads=[rf, "lams"], writes=[rf])
            S.add("dve", "tensor_scalar", T0[:], ob[:, 0:128], F[:, 0:1], None, ALU.mult, reads=[ro, rf], writes=[rf + "t"])
            S.add("dve", "scalar_tensor_tensor", A[:], ob[:, 129:257], F[:, 2:3], T0[:], ALU.mult, ALU.add,
                  reads=[ro, rf, rf + "t"], writes=[rf + "a"])
            S.add("dve", "scalar_tensor_tensor", junk[:], A[:], 1.0, A[:], ALU.mult, ALU.mult, accum_out=F[:, 3:4],
                  reads=[rf + "a"], writes=["junk", rf + "s"])
            S.add("dve", "tensor_scalar", F[:, 3:4], F[:, 3:4], 1.0 / 128, EPS, ALU.mult, ALU.add,
                  reads=[rf + "s"], writes=[rf + "s"])
            S.add("pool", "tensor_tensor", F[:, 5:6], F[:, 3:4], F[:, 6:7], ALU.pow, reads=[rf + "s", "fink"], writes=[rf + "r"])
            S.add("dve", "scalar_tensor_tensor", oa[:, i, h * 128:(h + 1) * 128], A[:], F[:, 5:6], subw[:],
                  ALU.mult, ALU.mult, reads=[rf + "a", rf + "r", "subw"], writes=["oa%d" % i])

        ns = len(steps)
        for sidx in range(ns + LOOK + 4):
            if sidx < ns:
                emit_s(sidx)
            if 0 <= sidx - LOOK < ns:
                emit_av(sidx - LOOK)
            while deferred and deferred[0][0] <= sidx:
                _, h_, i_, qn_ = deferred.pop(0)
                if not PA_DBG.get("nofin"):
                    emit_fin(h_, i_, qn_)
        assert not deferred
        for c in range(4):
            S.dma("sp" if c % 2 == 0 else "pool", OA[c * 1024:(c + 1) * 1024, :].rearrange("(i p) d -> p i d", p=128),
                  oa[:, c * 8:(c + 1) * 8, :], reads=["oa%d" % i for i in range(c * 8, c * 8 + 8)],
                  writes=["OA%d" % c], key="oast")
        S.flush()


def _const_tables(j, rel_bias):
    bk = np.arange(128, dtype=np.float32)
    parmask = np.full((128, 1), 0.0 if j == 1 else NEG, np.float32)
    abias = np.zeros((128, 2, 4, 64), np.float32)
    adiag = np.zeros((128, 4, 2, 128), np.float32)
    for h in range(4):
        sl = np.float32(SLOPES[h])
        for rel in range(64):
            abias[:, 0, h, rel] = sl * (bk - 128.0 * rel)
            abias[:, 1, h, rel] = sl * (bk - 128.0 * rel) + parmask[0, 0]
        bq = bk[None, :]
        bkk = bk[:, None]
        t = -sl * np.abs(bq - bkk) + sl * bq
        t = np.where((bkk // 64) > (bq // 64), NEG, t)
        adiag[:, h, 0, :] = t
        adiag[:, h, 1, :] = t
    kk = np.arange(128)[:, None, None]
    tb = np.arange(5)[None, :, None]
    q = np.arange(128)[None, None, :]
    rel = (tb - 4) * 128 + kk - q
    kch = 2 * (tb - 4) + kk // 64
    qch = q // 64
    valid = (kch - qch >= -8) & (kch - qch <= 0)
    idx = np.clip(rel, -128, 128) + 128
    btab = np.zeros((128, 8, 5, 128), np.float32)
    for hb in range(8):
        btab[:, hb] = np.where(valid, rel_bias[hb][idx], NEG)
    return parmask, abias.reshape(128, -1), adiag.reshape(128, -1), btab.reshape(128, -1)


def _core_inputs(core, inputs):
    b, j = divmod(core, 2)
    xb = np.asarray(inputs["x"][b], np.float32)
    if j == 0:
        xall = np.concatenate([np.zeros((128, D), np.float32), xb[:63 * 128]], 0)
    else:
        xall = xb
    parmask, abias, adiag, btab = _const_tables(j, np.asarray(inputs["rel_bias"][0], np.float32))
    m = {
        "xall": np.ascontiguousarray(xall),
        "lnin": np.stack([inputs["ln_in_g"], inputs["ln_in_b"]]).astype(np.float32),
        "w_in": np.ascontiguousarray(inputs["w_in"][0]),
        "ident": np.eye(128, dtype=np.float32),
        "abias": abias, "adiag": adiag, "btab": btab, "parmask": parmask,
        "lamv": np.stack([inputs["lambda_q1"][0], inputs["lambda_k1"][0], inputs["lambda_q2"][0],
                          inputs["lambda_k2"][0]]).astype(np.float32),
        "subln": np.asarray(inputs["subln_w"], np.float32).reshape(1, 128),
        "b_gate": np.ascontiguousarray(np.asarray(inputs["b_gate"][0], np.float32).reshape(16, 128).T),
        "w_ba": np.ascontiguousarray(inputs["w_branch_a"][0]), "w_bb": np.ascontiguousarray(inputs["w_branch_b"][0]),
        "w_out": np.ascontiguousarray(inputs["w_out"][0]),
        "ln1": np.stack([inputs["ln1_g"][0], inputs["ln1_b"][0]]).astype(np.float32),
        "w_router": np.ascontiguousarray(inputs["w_router"][0]),
        "b_router": np.asarray(inputs["b_router"][0], np.float32).reshape(1, NEXP),
        "rconst": _rconst(),
        "w_e1": np.asarray(inputs["w_exp_in"][0]), "w_e2": np.asarray(inputs["w_exp_out"][0]),
        "b_e1": np.ascontiguousarray(np.asarray(inputs["b_exp_in"][0], np.float32).reshape(NEXP, 16, 128)
                                     .transpose(2, 0, 1).reshape(128, NEXP * 16)),
        "b_e2": np.asarray(inputs["b_exp_out"][0], np.float32),
        "ln2": np.stack([inputs["ln2_g"][0], inputs["ln2_b"][0]]).astype(np.float32),
    }
    return m


def phaseB(nc, S, psum, inp, KBT, QBT, VB, OB):
    with ExitStack() as st:
        sb = lambda name, shape, dt: st.enter_context(nc.sbuf_tensor(name, list(shape), dt))
        vall = sb("pb_v", [128, NBLK, 8 * 65], BF16)
        kt = sb("pb_k", [128, 4, NBLK * 128], BF16)
        btab = sb("pb_bt", [128, 8, 5, 128], F32)
        pm = sb("pb_pm", [128, 1], F32)
        qp = [sb("pb_q%d" % i, [128, 8, 128], BF16) for i in range(2)]
        tmp = [sb("pb_t%d" % i, [128, 5, 128], F32) for i in range(2)]
        pt = [sb("pb_p%d" % i, [128, 5, 128], BF16) for i in range(2)]
        rc = [sb("pb_r%d" % i, [128, 8], F32) for i in range(2)]
        ob = [sb("pb_o%d" % i, [128, 8, 64], BF16) for i in range(2)]
        for c in range(4):
            S.dma("sp" if c % 2 == 0 else "pool", vall[:, c * 16:(c + 1) * 16, :],
                  VB[c * 16:(c + 1) * 16].rearrange("k p d -> p k d"), reads=[], writes=["vball"], key="initB", group=True)
            S.dma("pool" if c % 2 == 0 else "sp", kt[:, c, :], KBT[c], reads=[], writes=["kball"], key="initB", group=True)
        S.dma("sp", btab[:].rearrange("p a b c -> p (a b c)"), inp["btab"][:, :], reads=[], writes=["btab"],
              key="initB", group=True)
        S.dma("sp", pm[:], inp["parmask"][:, :], reads=[], writes=["pm"], key="initB", group=True)
        for i in range(2):
            for hb in range(8):
                lo = 64 * (1 - hb % 2)
                S.add("pool", "memset", qp[i][lo:lo + 64, hb, :], 0.0, reads=[], writes=["qbz%d" % i])
        s_banks = [(psum[0], psum[1]), (psum[2], psum[3])]
        o_banks = [(psum[4], psum[5]), (psum[6], psum[7])]
        steps = [(i, hb) for i in range(NOWN) for hb in range(8)]

        def q_load(i):
            Q = qp[i % 2]
            rq = "qb%d" % (i % 2)
            for m in range(4):
                for half in range(2):
                    S.dma("sp" if half == 0 else "pool", Q[64 * half:64 * half + 64, 2 * m + half, :],
                          QBT[m][64 * half:64 * half + 64, i * 128:(i + 1) * 128], reads=[], writes=[rq + "_%d" % (2 * m + half)],
                          key=rq, group=True)

        def emit_s(sidx):
            i, hb = steps[sidx]
            if hb == 0 and i + 1 < NOWN:
                q_load(i + 1)
            kxq = 2 * i + 1
            Q = qp[i % 2]
            rq = "qb%d" % (i % 2)
            tbs = [tb for tb in range(5) if kxq - 4 + tb >= 0]
            m = hb // 2
            sA, sB = s_banks[sidx % 2]
            rs = "sbps%d" % (sidx % 2)
            T, P = tmp[sidx % 2], pt[sidx % 2]
            rt, rp = "bt%d" % (sidx % 2), "bp%d" % (sidx % 2)
            for tb in tbs:
                kx = kxq - 4 + tb
                o = sA[:, tb * 128:(tb + 1) * 128] if tb < 4 else sB[:, 0:128]
                S.add("pe", "matmul", o, kt[:, m, kx * 128:(kx + 1) * 128], Q[:, hb, :], start=True, stop=True,
                      reads=["kball", rq + "_%d" % hb, "qbz%d" % (i % 2)], writes=[rs + ("a" if tb < 4 else "b")])
            lo = tbs[0]
            if lo < 4:
                S.add("dve", "tensor_tensor", T[:, lo:4, :], sA[:, lo * 128:512].rearrange("p (a b) -> p a b", b=128),
                      btab[:, hb, lo:4, :], ALU.add, reads=[rs + "a", "btab"], writes=[rt + "a"])
            S.add("dve", "tensor_tensor", T[:, 4, :], sB[:, 0:128], btab[:, hb, 4, :], ALU.add,
                  reads=[rs + "b", "btab"], writes=[rt + "b"])
            segs = []
            z = [tb for tb in tbs if kxq - 4 + tb == 0]
            if z:
                segs.append((z[0], z[0] + 1, True))
                if z[0] + 1 < 5:
                    segs.append((z[0] + 1, 5, False))
            else:
                segs.append((lo, 5, False))
            for (a_, b_, msk) in segs:
                kw = dict(bias=pm[:, 0:1]) if msk else {}
                S.add("act", "activation", P[:, a_:b_, :], T[:, a_:b_, :], AF.Exp, reads=[rt + "a", rt + "b", "pm"],
                      writes=[rp], **kw)

        def emit_av(sidx):
            i, hb = steps[sidx]
            kxq = 2 * i + 1
            tbs = [tb for tb in range(5) if kxq - 4 + tb >= 0]
            P = pt[sidx % 2]
            rp = "bp%d" % (sidx % 2)
            obk = o_banks[i % 2]
            ro = "obps%d" % (i % 2)
            ot = obk[hb // 4]
            oc = (hb % 4) * 65
            for tb in tbs:
                kx = kxq - 4 + tb
                S.add("pe", "matmul", ot[:, oc:oc + 65], P[:, tb, :], vall[:, kx, hb * 65:(hb + 1) * 65],
                      start=(tb == tbs[0]), stop=(tb == 4), reads=[rp, "vball"], writes=[ro + "_%d" % (hb // 4)])

        def emit_fin(i):
            obk = o_banks[i % 2]
            ro = "obps%d" % (i % 2)
            R, O = rc[i % 2], ob[i % 2]
            rr, rob = "brc%d" % (i % 2), "bob%d" % (i % 2)
            for g in range(2):
                ov = obk[g][:, 0:260].rearrange("p (h d) -> p h d", h=4)
                S.add("dve", "reciprocal", R[:, 4 * g:4 * g + 4], ov[:, :, 64], reads=[ro + "_%d" % g], writes=[rr])
                S.add("dve", "tensor_tensor", O[:, 4 * g:4 * g + 4, :], ov[:, :, 0:64],
                      R[:, 4 * g:4 * g + 4].unsqueeze(2).to_broadcast([128, 4, 64]), ALU.mult,
                      reads=[ro + "_%d" % g, rr], writes=[rob])
            S.dma("sp", OB[i * 128:(i + 1) * 128, :], O[:].rearrange("p h d -> p (h d)"), reads=[rob], writes=["OB%d" % i],
                  key="obst%d" % (i % 2))

        q_load(0)
        ns = len(steps)
        fin_at = {}
        for sidx in range(ns + 4):
            if sidx < ns:
                emit_s(sidx)
            if 0 <= sidx - 1 < ns:
                emit_av(sidx - 1)
                i_, hb_ = steps[sidx - 1]
                if hb_ == 7:
                    fin_at[sidx + 2] = i_
            if sidx in fin_at:
                emit_fin(fin_at.pop(sidx))
        assert not fin_at
        S.flush()


def _rconst():
    t = np.arange(128)
    U = (t[:, None] < t[None, :]).astype(np.float32)
    ones = np.ones((128, 128), np.float32)
    iota = np.tile(np.arange(32, dtype=np.float32)[None, :], (128, 1))
    ecap = iota * E_CAP
    return np.concatenate([U, ones, iota, ecap], 1)


def phaseM(nc, S, psum, ident, inp, xall, lnin, w_in, OA, OB, H1, XS, pers):
    with ExitStack() as st:
        sb = lambda name, shape, dt: st.enter_context(nc.sbuf_tensor(name, list(shape), dt))
        wg = sb("pm_wg", [128, 8, 2048], BF16)
        wba = sb("pm_wba", [128, 4, D], BF16)
        wbb = sb("pm_wbb", [128, 4, D], BF16)
        wo = sb("pm_wo", [128, 8, D], BF16)
        wr = sb("pm_wr", [128, 8, NEXP], F32)
        gbin = sb("pm_gbin", [128, 2, D], F32)
        gb1 = sb("pm_gb1", [128, 2, D], F32)
        bg = sb("pm_bg", [128, 16], F32)
        brb = sb("pm_brb", [128, NEXP], F32)
        rcon = sb("pm_rc", [128, 320], F32)
        ub = sb("pm_ub", [128, 256], BF16)
        identf = sb("pm_idf", [128, 128], F32)
        carry = sb("pm_carry", [128, NEXP], F32)
        xt = [sb("pm_x%d" % i, [128, D], F32) for i in range(2)]
        xn = sb("pm_xn", [128, D], F32)
        hres_all = [sb("pm_hr%d" % i, [128, D], F32) for i in range(4)]
        hb = sb("pm_hb", [128, D], BF16)
        hT_all = [sb("pm_hT%d" % i, [128, 8, 256], BF16) for i in range(2)]
        oab = [sb("pm_oab%d" % i, [128, D], BF16) for i in range(2)]
        oT_all = [sb("pm_oT%d" % i, [128, 8, 256], BF16) for i in range(2)]
        gT = sb("pm_gT", [128, 16, 256], F32)
        mT = sb("pm_mT", [128, 8, 256], BF16)
        t1 = [sb("pm_t1%d" % i, [128, 256], F32) for i in range(2)]
        t2 = [sb("pm_t2%d" % i, [128, 256], F32) for i in range(2)]
        rr_all = [sb("pm_r%d" % i, [128, D], F32) for i in range(4)]
        h1b = [sb("pm_h1b%d" % i, [128, D], BF16) for i in range(2)]
        h1T = sb("pm_h1T", [128, 2, 8, 128], F32)
        st6 = sb("pm_st6", [128, 2, 6], F32)
        mv = sb("pm_mv", [128, 8], F32)
        ln_consts(S, mv, "pms")
        sm = [sb("pm_sm%d" % i, [128, 160], F32) for i in range(2)]
        idx8 = [sb("pm_ix%d" % i, [128, 8], U32) for i in range(2)]
        maskb = [sb("pm_mk%d" % i, [128, NEXP], BF16) for i in range(2)]
        junk = sb("pm_junk", [128, 4 * NEXP], F32)

        S.group_keys.update(["xsc0@sw", "xsc1@sw"])
        bc_reg = nc.gpsimd.alloc_register("xs_bc")
        nc.gpsimd.reg_mov(bc_reg, NEXP * E_CAP - 1)
        ini = dict(key="initM", group=True)
        wgv = w_in[:, 3072:5120].rearrange("(k p) c -> p k c", p=128)
        for k in range(8):
            S.dma("pool", wg[:, k, :], wgv[:, k, :], reads=[], writes=["wg"], **ini)
        S.dma("pool", wba[:], inp["w_ba"].rearrange("(k p) c -> p k c", p=128), reads=[], writes=["wba"], **ini)
        S.dma("pool", wbb[:], inp["w_bb"].rearrange("(k p) c -> p k c", p=128), reads=[], writes=["wbb"], **ini)
        S.dma("pool", wo[:], inp["w_out"].rearrange("(k p) c -> p k c", p=128), reads=[], writes=["wo"], **ini)
        S.dma("sp", wr[:], inp["w_router"].rearrange("(k p) c -> p k c", p=128), reads=[], writes=["wr"], **ini)
        S.dma("sp", gbin[:, 0, :], lnin[0:1, :].partition_broadcast(128), reads=[], writes=["gbin0"], **ini)
        S.dma("sp", gbin[:, 1, :], lnin[1:2, :].partition_broadcast(128), reads=[], writes=["gbin1"], **ini)
        S.dma("sp", gb1[:, 0, :], inp["ln1"][0:1, :].partition_broadcast(128), reads=[], writes=["gb10"], **ini)
        S.dma("sp", gb1[:, 1, :], inp["ln1"][1:2, :].partition_broadcast(128), reads=[], writes=["gb11"], **ini)
        S.dma("sp", bg[:], inp["b_gate"][:, :], reads=[], writes=["bg"], **ini)
        S.dma("sp", brb[:], inp["b_router"][0:1, :].partition_broadcast(128), reads=[], writes=["brb"], **ini)
        S.dma("sp", rcon[:], inp["rconst"][:, :], reads=[], writes=["rcon"], **ini)
        S.dma("pool", ub[:], inp["rconst"][:, 0:256], reads=[], writes=["ub"], **ini)
        S.dma("sp", identf[:], inp["ident"][:, :], reads=[], writes=["identf"], **ini)
        S.add("pool", "memset", carry[:], 0.0, reads=[], writes=["carry"])
        iota = rcon[:, 256:288]
        ecap = rcon[:, 288:320]

        tpb = psum[0]
        tpv = tpb[:].bitcast(BF16)
        g_banks = [psum[1], psum[2]]
        brA, brBk = psum[3], psum[4]
        m_banks = [psum[5], psum[6]]
        rb = psum[7]
        gcnt = 0
        NG = NOWN // 2

        def load(G):
            for t in range(2):
                i = 2 * G + t
                kx = 2 * i + 1
                S.dma("sp", xt[t][:], xall[kx * 128:(kx + 1) * 128, :], reads=[], writes=["mx%d" % t])
                S.dma("sp", oab[t][:, 0:512], OA[i * 128:(i + 1) * 128, :], reads=[], writes=["oab%da" % t])
                S.dma("sp", oab[t][:, 512:1024], OB[i * 128:(i + 1) * 128, :], reads=[], writes=["oab%db" % t])

        hbs = [hb, sb("pm_hb2", [128, D], BF16)]

        def stage_a(G, part="ab"):
            g2 = G % 2
            hres = hres_all[2 * g2:2 * g2 + 2]
            hT, oT = hT_all[g2], oT_all[g2]
            if "a" in part:
                for t in range(2):
                    layer_norm_tile(S, xt[t], "mx%d" % t, xna, "mxna", hres[t], "hres%d_%d" % (g2, t), st6a, mva, "pmsa", gbin,
                                    ["gbin0", "gbin1"])
                    S.add("pool", "tensor_copy", hbs[t][:], hres[t][:], reads=["hres%d_%d" % (g2, t)], writes=["mhb%d" % t])
            if "b" not in part:
                return
            for t in range(2):
                hb = hbs[t]
                for k in range(8):
                    S.add("pe", "transpose", tpv[:, k * 128:(k + 1) * 128], hb[:, k * 128:(k + 1) * 128], ident[:],
                          reads=["mhb%d" % t, "ident"], writes=["mtp"])
                S.add("act", "copy", hT[:, :, t * 128:(t + 1) * 128], tpv.rearrange("p (k t) -> p k t", k=8),
                      reads=["mtp"], writes=["mhT%d_%d" % (g2, t)])
                for k in range(8):
                    S.add("pe", "transpose", tpv[:, k * 128:(k + 1) * 128], oab[t][:, k * 128:(k + 1) * 128], ident[:],
                          reads=["oab%da" % t, "oab%db" % t, "ident"], writes=["mtp"])
                S.add("act", "copy", oT[:, :, t * 128:(t + 1) * 128], tpv.rearrange("p (k t) -> p k t", k=8),
                      reads=["mtp"], writes=["moT%d_%d" % (g2, t)])

        env = (rr_all, h1b, h1T, xn, st6, mv, gb1, H1, g_banks, rb, identf, wr, brb, sm, idx8, maskb, carry, ub, iota, ecap,
               junk, pers, XS, bc_reg)
        stage_b2 = lambda G_, part: _pm_stage_b2(S, G_, env, part)
        st6a = sb("pm_st6a", [128, 2, 6], F32)
        xna = sb("pm_xna", [128, D], F32)
        mva = sb("pm_mva", [128, 8], F32)
        ln_consts(S, mva, "pmsa")
        load(0)
        stage_a(0)
        for G in range(NG):
            g2 = G % 2
            hres = hres_all[2 * g2:2 * g2 + 2]
            hT, oT = hT_all[g2], oT_all[g2]
            if G + 1 < NG:
                load(G + 1)
            if G >= 1:
                stage_b2(G - 1, "a")
            if G + 1 < NG:
                stage_a(G + 1, "a")
            rhT = ["mhT%d_0" % g2, "mhT%d_1" % g2]
            roT = ["moT%d_0" % g2, "moT%d_1" % g2]
            for gc in range(16):
                gbk = g_banks[gcnt % 2]
                rg = "gps%d" % (gcnt % 2)
                gcnt += 1
                for kc in range(8):
                    S.add("pe", "matmul", gbk[:, 0:256], wg[:, kc, gc * 128:(gc + 1) * 128], hT[:, kc, :],
                          start=(kc == 0), stop=(kc == 7), reads=rhT + ["wg"], writes=[rg])
                S.add("act", "activation", gT[:, gc, :], gbk[:, 0:256], AF.Sigmoid, bias=bg[:, gc:gc + 1],
                      reads=[rg, "bg"], writes=["gT%d" % gc])
            if G + 1 < NG:
                stage_a(G + 1, "b")
            if G >= 1:
                stage_b2(G - 1, "b")
                stage_b2(G - 1, "c")
            for oc in range(8):
                for kc in range(4):
                    S.add("pe", "matmul", brA[:, 0:256], wba[:, kc, oc * 128:(oc + 1) * 128], oT[:, kc, :],
                          start=(kc == 0), stop=(kc == 3), reads=roT + ["wba"], writes=["brA"])
                for kc in range(4):
                    S.add("pe", "matmul", brBk[:, 0:256], wbb[:, kc, oc * 128:(oc + 1) * 128], oT[:, 4 + kc, :],
                          start=(kc == 0), stop=(kc == 3), reads=roT + ["wbb"], writes=["brB"])
                T1, T2 = t1[oc % 2], t2[oc % 2]
                S.add("dve", "tensor_tensor", T1[:], brA[:, 0:256], gT[:, oc, :], ALU.mult,
                      reads=["brA", "gT%d" % oc], writes=["mt1%d" % (oc % 2)])
                S.add("dve", "tensor_tensor", T2[:], brBk[:, 0:256], gT[:, 8 + oc, :], ALU.mult,
                      reads=["brB", "gT%d" % (8 + oc)], writes=["mt2%d" % (oc % 2)])
                S.add("pool", "tensor_tensor", mT[:, oc, :], T1[:], T2[:], ALU.add,
                      reads=["mt1%d" % (oc % 2), "mt2%d" % (oc % 2)], writes=["mT%d" % oc])
            rmT = ["mT%d" % oc for oc in range(8)]
            if G >= 1:
                stage_b2(G - 1, "d")
            for t in range(2):
                for half in range(2):
                    for kc in range(8):
                        S.add("pe", "matmul", m_banks[half][:, :], mT[:, kc, t * 128:(t + 1) * 128],
                              wo[:, kc, half * 512:(half + 1) * 512], start=(kc == 0), stop=(kc == 7),
                              reads=rmT + ["wo"], writes=["mps%d" % half])
                R = rr_all[2 * g2 + t]
                for half in range(2):
                    S.add("dve", "scalar_tensor_tensor", R[:, half * 512:(half + 1) * 512],
                          hres[t][:, half * 512:(half + 1) * 512], ALPHA, m_banks[half][:, :], ALU.mult, ALU.add,
                          reads=["hres%d_%d" % (g2, t), "mps%d" % half], writes=["mr%d_%d" % (g2, t)])
        for part in "abcd":
            stage_b2(NG - 1, part)
        if "DESTD" in pers:
            S.dma("sp", pers["DESTD"][:, :], pers["dest"][:].rearrange("p a b -> p (a b)"),
                  reads=["dest%d" % i for i in range(NOWN)], writes=["DESTD"])
            S.dma("sp", pers["GATED"][:, :], pers["gate"][:].rearrange("p a b -> p (a b)"),
                  reads=["gate%d" % i for i in range(NOWN)], writes=["GATED"])
        S.flush()


def _pm_stage_b2(S, G, env, part):
    (rr_all, h1b, h1T, xn, st6, mv, gb1, H1, g_banks, rb, identf, wr, brb, sm, idx8, maskb, carry, ub, iota, ecap, junk,
     pers, XS, bc_reg) = env
    g2 = G % 2
    for t in range(2):
        i = 2 * G + t
        R, HB = rr_all[2 * g2 + t], h1b[t]
        H = R
        rh = "mr%d_%d" % (g2, t)
        SM, IX, MK = sm[t], idx8[t], maskb[t]
        rs = "msm%d" % t
        lg, top8, idxf, e4 = SM[:, 0:32], SM[:, 32:40], SM[:, 40:44], SM[:, 44:48]
        nmax, den, rden = SM[:, 48:49], SM[:, 49:50], SM[:, 50:51]
        posf, ovf, dfull = SM[:, 52:84], SM[:, 84:116], SM[:, 116:148]
        c0 = 96 * t
        if part == "a":
            layer_norm_tile(S, R, rh, xn, "mxn", H, rh, st6, mv, "pms", gb1, ["gb10", "gb11"])
            S.dma("sp", H1[i * 128:(i + 1) * 128, :], H[:], reads=[rh], writes=["H1_%d" % i], key="h1st%d" % t)
            S.add("pool", "tensor_copy", HB[:], H[:], reads=[rh], writes=["mh1b%d" % t])
        elif part == "b":
            for k in range(8):
                bank = g_banks[k // 4]
                S.add("pe", "transpose", bank[:, (k % 4) * 128:(k % 4 + 1) * 128], H[:, k * 128:(k + 1) * 128],
                      identf[:], reads=[rh, "identf"], writes=["gps%d" % (k // 4)])
            S.add("act", "copy", h1T[:, t, 0:4, :], g_banks[0][:, :].rearrange("p (k t) -> p k t", k=4), reads=["gps0"],
                  writes=["h1Ta%d" % t])
            S.add("act", "copy", h1T[:, t, 4:8, :], g_banks[1][:, :].rearrange("p (k t) -> p k t", k=4), reads=["gps1"],
                  writes=["h1Tb%d" % t])
        elif part == "c":
            for kc in range(8):
                S.add("pe", "matmul", rb[:, c0:c0 + 32], h1T[:, t, kc, :], wr[:, kc, :], start=(kc == 0), stop=(kc == 7),
                      reads=["h1Ta%d" % t, "h1Tb%d" % t, "wr"], writes=["rb"])
            S.add("dve", "tensor_tensor", lg, rb[:, c0:c0 + 32], brb[:], ALU.add, reads=["rb", "brb"], writes=[rs])
            S.add("dve", "max", top8, lg, reads=[rs], writes=[rs])
            S.add("dve", "max_index", IX[:], top8, lg, reads=[rs], writes=[rs + "i"])
            S.add("dve", "tensor_copy", idxf, IX[:, 0:4], reads=[rs + "i"], writes=[rs])
            S.add("dve", "tensor_scalar", nmax, top8[:, 0:1], -1.0, None, ALU.mult, reads=[rs], writes=[rs])
            S.add("act", "activation", e4, top8[:, 0:4], AF.Exp, bias=nmax, accum_out=den, reads=[rs], writes=[rs])
            S.add("dve", "tensor_scalar", MK[:], lg, top8[:, 3:4], None, ALU.is_ge, reads=[rs], writes=[rs + "m"])
            S.add("dve", "reciprocal", rden, den, reads=[rs], writes=[rs])
            S.add("dve", "tensor_scalar", pers["gate"][:, i, :], e4, rden, None, ALU.mult, reads=[rs],
                  writes=["gate%d" % i])
        elif part == "d":
            S.add("pe", "matmul", rb[:, c0 + 32:c0 + 64], ub[:, 0:128], MK[:], start=True, stop=True,
                  reads=[rs + "m", "ub"], writes=["rb"])
            S.add("pe", "matmul", rb[:, c0 + 64:c0 + 96], ub[:, 128:256], MK[:], start=True, stop=True,
                  reads=[rs + "m", "ub"], writes=["rb"])
            S.add("dve", "tensor_tensor", posf, rb[:, c0 + 32:c0 + 64], carry[:], ALU.add, reads=["rb", "carry"],
                  writes=[rs])
            S.add("dve", "tensor_tensor", carry[:], rb[:, c0 + 64:c0 + 96], carry[:], ALU.add, reads=["rb", "carry"],
                  writes=["carry"])
            S.add("dve", "tensor_scalar", ovf, posf, float(E_CAP), 1.0e7, ALU.is_ge, ALU.mult, reads=[rs], writes=[rs])
            S.add("dve", "tensor_tensor", dfull, posf, ecap, ALU.add, reads=[rs, "rcon"], writes=[rs])
            S.add("dve", "tensor_tensor", dfull, dfull, ovf, ALU.add, reads=[rs], writes=[rs])
            oh3 = junk[:, 0:128].rearrange("p (k e) -> p k e", k=4)
            S.add("dve", "tensor_tensor", oh3, iota.unsqueeze(1).to_broadcast([128, 4, 32]),
                  idxf.unsqueeze(2).to_broadcast([128, 4, 32]), ALU.is_equal, reads=[rs, "rcon"], writes=["mjunk"])
            S.add("dve", "tensor_tensor", oh3, oh3, dfull.unsqueeze(1).to_broadcast([128, 4, 32]), ALU.mult,
                  reads=[rs, "mjunk"], writes=["mjunk"])
            S.add("dve", "tensor_reduce", SM[:, 152:156], oh3, AX.X, ALU.add, reads=["mjunk"], writes=[rs + "d"])
            S.add("dve", "tensor_copy", pers["dest"][:, i, :], SM[:, 152:156], reads=[rs + "d"], writes=["dest%d" % i])
            S.add("dve", "tensor_scalar", SM[:, 156:160], SM[:, 152:156], 1.0e6, None, ALU.is_lt, reads=[rs + "d"],
                  writes=[rs + "g"])
            S.add("dve", "tensor_tensor", pers["gate"][:, i, :], pers["gate"][:, i, :], SM[:, 156:160], ALU.mult,
                  reads=[rs + "g", "gate%d" % i], writes=["gate%d" % i])
            for k in range(4):
                S.add("pool", "indirect_dma_start", reads=["dest%d" % i, "mh1b%d" % t], writes=["XS_%d_%d" % (i, k)],
                      dma=True, key="xsc%d@sw" % t,
                      out=XS[:, :], out_offset=bass.IndirectOffsetOnAxis(ap=pers["dest"][:, i, k:k + 1], axis=0),
                      in_=HB[:], in_offset=None, bounds_check=bc_reg, oob_is_err=False)


def phaseX(nc, S, psum, ident, inp, XS, YS):
    NB = (E_CAP + 127) // 128
    ntl = [(0, min(512, E_CAP))] + ([(512, E_CAP)] if E_CAP > 512 else [])
    with ExitStack() as st:
        sb = lambda name, shape, dt: st.enter_context(nc.sbuf_tensor(name, list(shape), dt))
        w1 = [sb("px_w1%d" % i, [128, 8, 2048], BF16) for i in range(2)]
        w2 = [sb("px_w2%d" % i, [128, 8, D], BF16) for i in range(2)]
        xs = [sb("px_xs%d" % i, [128, NB, D], BF16) for i in range(2)]
        xT = [sb("px_xT%d" % i, [128, 8, E_CAP], BF16) for i in range(2)]
        aT = sb("px_aT", [128, 8, E_CAP], BF16)
        b1 = sb("px_b1", [128, NEXP, 16], F32)
        b2 = [sb("px_b2%d" % i, [128, D], F32) for i in range(2)]
        gs = [sb("px_g%d" % i, [128, 512], F32) for i in range(3)]
        sg = [sb("px_sg%d" % i, [128, 512], F32) for i in range(3)]
        us = [sb("px_u%d" % i, [128, 512], F32) for i in range(3)]
        ys = [sb("px_y%d" % i, [128, D], F32) for i in range(2)]
        S.dma("sp", b1[:].rearrange("p e c -> p (e c)"), inp["b_e1"][:, :], reads=[], writes=["b1"], key="initX", group=True)
        b1p = sb("px_b1p", [128, NEXP, 16], F32)
        S.add("dve", "tensor_scalar", b1p[:], b1[:], 7.0, None, ALU.add, reads=["b1"], writes=["b1p"])
        tpb = psum[0]
        tpv = tpb[:].bitcast(BF16)
        h_banks = [psum[1], psum[2], psum[3]]
        y_banks = [(psum[4], psum[5]), (psum[6], psum[7])]
        hc = 0
        yc = 0
        ec = 0

        pending = []

        def load(e, defer):
            b = e % 2
            w1v = inp["w_e1"][e].rearrange("(k p) c -> p k c", p=128)
            w2v = inp["w_e2"][e].rearrange("(k p) c -> p k c", p=128)
            lst = []
            for k in range(8):
                lst.append(lambda k=k: S.dma("pool", w1[b][:, k, :], w1v[:, k, :], reads=[], writes=["w1_%d_%d" % (b, k)],
                                             key="w1_%d" % b, group=True))
            for k in range(0, 8, 2):
                lst.append(lambda k=k: S.dma("pool", w2[b][:, k:k + 2, :], w2v[:, k:k + 2, :], reads=[],
                                             writes=["w2_%d_%d" % (b, k // 2)], key="w2_%d" % b, group=True))
            nfull = E_CAP // 128
            S.dma("sp", xs[b][:, 0:nfull, :], XS[e * E_CAP:e * E_CAP + nfull * 128, :].rearrange("(n p) d -> p n d", p=128),
                  reads=[], writes=["xs%d" % b])
            if E_CAP % 128:
                S.dma("sp", xs[b][0:E_CAP % 128, nfull, :], XS[e * E_CAP + nfull * 128:(e + 1) * E_CAP, :], reads=[],
                      writes=["xs%dr" % b])
            S.dma("sp", b2[b][:], inp["b_e2"][e:e + 1, :].partition_broadcast(128), reads=[], writes=["b2_%d" % b])
            if defer:
                pending.extend(lst)
            else:
                for f in lst:
                    f()

        load(0, False)
        for e in range(NEXP):
            b = e % 2
            if e + 1 < NEXP:
                load(e + 1, True)
            W1, W2, XSb, XT = w1[b], w2[b], xs[b], xT[b]
            rw1 = ["w1_%d_%d" % (b, k) for k in range(8)]
            rw2 = ["w2_%d_%d" % (b, k) for k in range(4)]

            def xpose(ee):
                bb = ee % 2
                for n in range(NB):
                    rows = min(128, E_CAP - n * 128)
                    for k in range(8):
                        S.add("pe", "transpose", tpv[:, k * 128:k * 128 + rows], xs[bb][0:rows, n, k * 128:(k + 1) * 128],
                              ident[0:rows, 0:rows], reads=["xs%d" % bb, "xs%dr" % bb, "ident"], writes=["xtp"])
                    S.add("act", "copy", xT[bb][:, :, n * 128:n * 128 + rows],
                          tpv.rearrange("p (k t) -> p k t", k=8)[:, :, 0:rows], reads=["xtp"], writes=["xT%d_%d" % (bb, n)])

            if e == 0:
                xpose(0)
            rxT = ["xT%d_%d" % (b, n) for n in range(NB)]
            for fc in range(8):
                for (n0, n1) in ntl:
                    N = n1 - n0
                    pg = h_banks[hc % 3]
                    rpg = "hps%d" % (hc % 3)
                    hc += 1
                    pu = h_banks[hc % 3]
                    rpu = "hps%d" % (hc % 3)
                    hc += 1
                    for kc in range(8):
                        S.add("pe", "matmul", pg[:, 0:N], W1[:, kc, fc * 128:(fc + 1) * 128], XT[:, kc, n0:n1],
                              start=(kc == 0), stop=(kc == 7), reads=rxT + [rw1[kc]], writes=[rpg])
                    for kc in range(8):
                        S.add("pe", "matmul", pu[:, 0:N], W1[:, kc, 1024 + fc * 128:1024 + (fc + 1) * 128], XT[:, kc, n0:n1],
                              start=(kc == 0), stop=(kc == 7), reads=rxT + [rw1[kc]], writes=[rpu])
                    q = ec % 3
                    ec += 1
                    Gs, Sg, Us = gs[q], sg[q], us[q]
                    S.add("dve", "tensor_scalar", Gs[:, 0:N], pg[:, 0:N], b1[:, e, fc:fc + 1], 7.0, ALU.add, ALU.min,
                          reads=[rpg, "b1"], writes=["xg%d" % q])
                    S.add("act", "activation", Us[:, 0:N], pu[:, 0:N], AF.Relu, bias=b1p[:, e, 8 + fc:9 + fc],
                          reads=[rpu, "b1p"], writes=["xu%d" % q])
                    S.add("act", "activation", Sg[:, 0:N], Gs[:, 0:N], AF.Gelu_apprx_sigmoid, reads=["xg%d" % q],
                          writes=["xsg%d" % q])
                    S.add("dve", "tensor_scalar", Us[:, 0:N], Us[:, 0:N], 14.0, -6.0, ALU.min, ALU.add,
                          reads=["xu%d" % q], writes=["xu%d" % q])
                    S.add("dve", "tensor_tensor", aT[:, fc, n0:n1], Sg[:, 0:N], Us[:, 0:N], ALU.mult,
                          reads=["xsg%d" % q, "xu%d" % q], writes=["aT%d_%d" % (fc, n0)])
                    if pending:
                        pending.pop(0)()
            while pending:
                pending.pop(0)()
            if e + 1 < NEXP:
                xpose(e + 1)
            raT = ["aT%d_%d" % (fc, n0) for fc in range(8) for (n0, _) in ntl]
            for n in range(NB):
                yb = y_banks[yc % 2]
                ry = "yps%d" % (yc % 2)
                Y = ys[yc % 2]
                rys = "ysb%d" % (yc % 2)
                yc += 1
                rows = min(128, E_CAP - n * 128)
                for half in range(2):
                    for kc in range(8):
                        S.add("pe", "matmul", yb[half][0:rows, :], aT[:, kc, n * 128:n * 128 + rows],
                              W2[:, kc, half * 512:(half + 1) * 512], start=(kc == 0), stop=(kc == 7),
                              reads=raT + [rw2[kc // 2]], writes=[ry + "_%d" % half])
                for half in range(2):
                    S.add("dve", "tensor_tensor", Y[0:rows, half * 512:(half + 1) * 512], yb[half][0:rows, :],
                          b2[b][0:rows, half * 512:(half + 1) * 512], ALU.add, reads=[ry + "_%d" % half, "b2_%d" % b],
                          writes=[rys])
                r0 = e * E_CAP + n * 128
                S.dma("sp", YS[r0:r0 + rows, :], Y[0:rows, :], reads=[rys], writes=["YS_%d_%d" % (e, n)], key=rys)
        S.flush()


def phaseF(nc, S, psum, inp, H1, YS, out, pers):
    with ExitStack() as st:
        sb = lambda name, shape, dt: st.enter_context(nc.sbuf_tensor(name, list(shape), dt))
        gb2 = sb("pf_gb2", [128, 2, D], F32)
        h1 = [sb("pf_h%d" % i, [128, D], F32) for i in range(2)]
        yk = [[sb("pf_y%d_%d" % (i, k), [128, D], F32) for k in range(4)] for i in range(2)]
        acc = [sb("pf_acc%d" % i, [128, D], F32) for i in range(2)]
        xn = sb("pf_xn", [128, D], F32)
        ot = [sb("pf_o%d" % i, [128, D], F32) for i in range(2)]
        st6 = sb("pf_st6", [128, 2, 6], F32)
        mv = sb("pf_mv", [128, 8], F32)
        ln_consts(S, mv, "pfs")
        bc_reg = nc.gpsimd.alloc_register("ys_bc")
        nc.gpsimd.reg_mov(bc_reg, NEXP * E_CAP - 1)
        S.dma("sp", gb2[:, 0, :], inp["ln2"][0:1, :].partition_broadcast(128), reads=[], writes=["gb20"], key="initF", group=True)
        S.dma("sp", gb2[:, 1, :], inp["ln2"][1:2, :].partition_broadcast(128), reads=[], writes=["gb21"], key="initF", group=True)
        def fetch(i):
            b = i % 2
            S.dma("sp", h1[b][:], H1[i * 128:(i + 1) * 128, :], reads=[], writes=["fh%d" % b])
            for k in range(4):
                S.add("pool", "indirect_dma_start", reads=[], writes=["fy%d_%d" % (b, k)], dma=True, key="fy%d_%d@sw" % (b, k),
                      out=yk[b][k][:], out_offset=None, in_=YS[:, :],
                      in_offset=bass.IndirectOffsetOnAxis(ap=pers["dest"][:, i, k:k + 1], axis=0),
                      bounds_check=bc_reg, oob_is_err=False)

        for b_ in range(2):
            for k in range(4):
                S.add("dve", "memset", yk[b_][k][:], 0.0, reads=[], writes=["fy%d_%d" % (b_, k)])
        fetch(0)
        for i in range(NOWN):
            b = i % 2
            if i + 1 < NOWN:
                fetch(i + 1)
            A = acc[b]
            ra = "facc%d" % b
            S.add("act", "activation", A[:], yk[b][0][:], AF.Copy, scale=pers["gate"][:, i, 0:1],
                  reads=["fy%d_0" % b], writes=[ra])
            S.add("dve", "scalar_tensor_tensor", A[:], h1[b][:], ALPHA, A[:], ALU.mult, ALU.add,
                  reads=[ra, "fh%d" % b], writes=[ra])
            for k in range(1, 4):
                S.add("dve", "scalar_tensor_tensor", A[:], yk[b][k][:], pers["gate"][:, i, k:k + 1], A[:], ALU.mult, ALU.add,
                      reads=["fy%d_%d" % (b, k), ra], writes=[ra])
            layer_norm_tile(S, A, ra, xn, "fxn", ot[b], "fo%d" % b, st6, mv, "pfs", gb2, ["gb20", "gb21"])
            S.dma("sp", out[i * 128:(i + 1) * 128, :], ot[b][:], reads=["fo%d" % b], writes=["out%d" % i], key="ost%d" % b)
        S.flush()


def kernel(**inputs):
    nc, S = build_program()
    in_maps = [_core_inputs(c, inputs) for c in range(8)]
    res = run_bass_kernel_spmd(nc, in_maps, core_ids=list(range(8)))
    outp = np.zeros((4, 64, 128, D), np.float32)
    for c in range(8):
        b, j = divmod(c, 2)
        outp[b, j::2] = np.asarray(res.results[c]["out"]).reshape(NOWN, 128, D)
    return outp.reshape(4, 8192, D)
```
